# Optimizing a Trainium2 kernel written in Bass

```python
import math
import jax, jax.numpy as jnp
from jax import lax
import numpy as np

D_MODEL = 4096
BATCH = 2
SEQ = 4096
DEPTH = 1

NSA_HEADS = 16
NSA_GROUPS = 2
NSA_HPG = NSA_HEADS // NSA_GROUPS
HEAD_DIM = 128
ROT_DIM = HEAD_DIM // 4
ROPE_THETA = 500000.0
CMP_LEN = 32
CMP_STRIDE = 16
CMP_HIDDEN = 256
SLC_LEN = 64
SLC_TOPK = 16
WINDOW = 512
Q_BLOCK = 128
NSA_WIDTH = NSA_HEADS * HEAD_DIM
KV_WIDTH = NSA_GROUPS * HEAD_DIM
DN_HEADS = 16
DN_DK = 128
DN_DV = 128
DN_WIDTH = DN_HEADS * DN_DV
CONV_WIDTH = 4
DN_CHUNK = 64
EPS = 1e-6

IN_WIDTHS = (
    NSA_WIDTH,
    6 * KV_WIDTH,
    3 * NSA_HEADS,
    NSA_WIDTH,
    3 * DN_WIDTH,
    DN_HEADS,
    DN_HEADS,
    DN_WIDTH,
    2 * D_MODEL,
)
D_IN = sum(IN_WIDTHS)

kernel_name = "hybrid_nsa_gated_deltanet_block"


def rms_norm(x, gain):
    x32 = x.astype(jnp.float32)
    y = x32 * lax.rsqrt(jnp.mean(x32 * x32, axis=-1, keepdims=True) + EPS)
    return (y * gain.astype(jnp.float32)).astype(x.dtype)


def l2_norm(x):
    x32 = x.astype(jnp.float32)
    return (x32 * lax.rsqrt(jnp.sum(x32 * x32, axis=-1, keepdims=True) + EPS)).astype(x.dtype)


def partial_rope(x, pos):
    half = ROT_DIM // 2
    inv_freq = ROPE_THETA ** (-jnp.arange(half, dtype=jnp.float32) / half)
    ang = pos.astype(jnp.float32)[..., None, None] * inv_freq
    cos, sin = jnp.cos(ang), jnp.sin(ang)
    xr = x[..., :ROT_DIM].astype(jnp.float32)
    x1, x2 = xr[..., :half], xr[..., half:]
    rot = jnp.concatenate([x1 * cos - x2 * sin, x2 * cos + x1 * sin], axis=-1).astype(x.dtype)
    return jnp.concatenate([rot, x[..., ROT_DIM:]], axis=-1)


def masked_softmax(s, mask):
    s = jnp.where(mask, s.astype(jnp.float32), -jnp.inf)
    m = jnp.max(s, axis=-1, keepdims=True)
    m = jnp.where(jnp.isfinite(m), m, 0.0)
    p = jnp.exp(s - m)
    return p / jnp.maximum(jnp.sum(p, axis=-1, keepdims=True), 1e-30)


def compress_blocks(kv, pos_emb, w1, w2):
    B, S, G, DH = kv.shape
    n_cmp = (S - CMP_LEN) // CMP_STRIDE + 1
    idx = np.arange(n_cmp)[:, None] * CMP_STRIDE + np.arange(CMP_LEN)[None, :]
    blocks = kv[:, idx] + pos_emb[None, None, :, None, :]
    blocks = blocks.transpose(0, 1, 3, 2, 4).reshape(B, n_cmp, G, CMP_LEN * DH)
    return jax.nn.silu(blocks @ w1) @ w2


def nsa_attention(q, k_cmp, v_cmp, k_slc, v_slc, k_win, v_win, gates):
    B, S, H, DH = q.shape
    G, P = NSA_GROUPS, NSA_HPG
    n_cmp = k_cmp.shape[1]
    nb = S // SLC_LEN
    topk = min(SLC_TOPK, nb)
    cmp_start = np.arange(n_cmp) * CMP_STRIDE
    cmp_end = jnp.asarray(cmp_start + CMP_LEN - 1, dtype=jnp.int32)
    slc_start = np.arange(nb) * SLC_LEN
    overlap = jnp.asarray(((cmp_start[:, None] <= slc_start[None, :] + SLC_LEN - 1)
                           & (cmp_start[:, None] + CMP_LEN - 1 >= slc_start[None, :])).astype(np.float32))
    ks_blocks = k_slc.reshape(B, nb, SLC_LEN, G, DH).transpose(0, 3, 1, 2, 4)
    vs_blocks = v_slc.reshape(B, nb, SLC_LEN, G, DH).transpose(0, 3, 1, 2, 4)
    kw_pad = jnp.pad(k_win, ((0, 0), (WINDOW, 0), (0, 0), (0, 0)))
    vw_pad = jnp.pad(v_win, ((0, 0), (WINDOW, 0), (0, 0), (0, 0)))
    bi = jnp.arange(B)[:, None, None, None]
    gi = jnp.arange(G)[None, :, None, None]
    jblk = jnp.arange(nb)
    scale = HEAD_DIM ** -0.5
    dt = q.dtype

    def block_fn(qs):
        t = qs + jnp.arange(Q_BLOCK)
        qb = lax.dynamic_slice_in_dim(q, qs, Q_BLOCK, 1)
        qb = qb.reshape(B, Q_BLOCK, G, P, DH).transpose(0, 2, 3, 1, 4) * scale
        s_c = jnp.einsum('bgptd,bcgd->bgptc', qb, k_cmp)
        p_c = masked_softmax(s_c, cmp_end[None, :] <= t[:, None])
        o_c = jnp.einsum('bgptc,bcgd->bgptd', p_c.astype(dt), v_cmp)
        imp = jnp.einsum('bgptc,cj->bgtj', p_c, overlap)
        tb = t // SLC_LEN
        valid = jblk[None, :] * SLC_LEN <= t[:, None]
        forced = (jblk[None, :] == 0) | (jblk[None, :] == tb[:, None]) | (jblk[None, :] == tb[:, None] - 1)
        score = jnp.where(forced, 1e9, jnp.where(valid, imp, -jnp.inf))
        _, sel = lax.top_k(score, topk)
        ks = ks_blocks[bi, gi, sel]
        vs = vs_blocks[bi, gi, sel]
        tok = sel[..., None] * SLC_LEN + jnp.arange(SLC_LEN)
        mask_s = (tok <= t[None, None, :, None, None]).reshape(B, G, 1, Q_BLOCK, topk * SLC_LEN)
        s_s = jnp.einsum('bgptd,bgtkld->bgptkl', qb, ks).reshape(B, G, P, Q_BLOCK, topk * SLC_LEN)
        p_s = masked_softmax(s_s, mask_s).reshape(B, G, P, Q_BLOCK, topk, SLC_LEN)
        o_s = jnp.einsum('bgptkl,bgtkld->bgptd', p_s.astype(dt), vs)
        kw = lax.dynamic_slice_in_dim(kw_pad, qs, WINDOW + Q_BLOCK, 1)
        vw = lax.dynamic_slice_in_dim(vw_pad, qs, WINDOW + Q_BLOCK, 1)
        s_idx = qs - WINDOW + jnp.arange(WINDOW + Q_BLOCK)
        mask_w = (s_idx[None, :] >= 0) & (s_idx[None, :] <= t[:, None]) & (t[:, None] - s_idx[None, :] < WINDOW)
        s_w = jnp.einsum('bgptd,bsgd->bgpts', qb, kw)
        p_w = masked_softmax(s_w, mask_w)
        o_w = jnp.einsum('bgpts,bsgd->bgptd', p_w.astype(dt), vw)
        gb = lax.dynamic_slice_in_dim(gates, qs, Q_BLOCK, 1)
        gb = gb.reshape(B, Q_BLOCK, 3, G, P).transpose(2, 0, 3, 4, 1)[..., None]
        o = gb[0] * o_c + gb[1] * o_s + gb[2] * o_w
        return o.transpose(0, 3, 1, 2, 4).reshape(B, Q_BLOCK, H * DH)

    outs = lax.map(block_fn, jnp.arange(S // Q_BLOCK, dtype=jnp.int32) * Q_BLOCK)
    return outs.transpose(1, 0, 2, 3).reshape(B, S, H * DH)


def causal_conv(x, w):
    C = x.shape[-1]
    return lax.conv_general_dilated(x, w[:, None, :].astype(x.dtype), window_strides=(1,),
                                    padding=[(CONV_WIDTH - 1, 0)],
                                    dimension_numbers=('NWC', 'WIO', 'NWC'),
                                    feature_group_count=C)


def gated_delta_rule(q, k, v, g, beta):
    B, S, H, DK = q.shape
    DV = v.shape[-1]
    C = DN_CHUNK
    N = S // C

    def chunk(x):
        return x.reshape(B, N, C, H, x.shape[-1]).transpose(1, 0, 3, 2, 4)

    qc, kc, vc = chunk(q), chunk(k), chunk(v)
    gc = g.reshape(B, N, C, H).transpose(1, 0, 3, 2)
    bc = beta.reshape(B, N, C, H).transpose(1, 0, 3, 2)
    decay = jnp.cumsum(gc, axis=-1)
    tril = jnp.tril(jnp.ones((C, C), dtype=bool))
    strict = jnp.tril(jnp.ones((C, C), dtype=bool), -1)
    lmat = jnp.exp(jnp.where(tril, decay[..., :, None] - decay[..., None, :], -jnp.inf))
    kb = kc * bc[..., None]
    vb = vc * bc[..., None]
    a = jnp.where(strict, jnp.einsum('nbhid,nbhjd->nbhij', kb, kc) * lmat, 0.0)
    eye = jnp.eye(C, dtype=jnp.float32)
    tinv = lax.linalg.triangular_solve(a + eye, jnp.broadcast_to(eye, a.shape), left_side=True,
                                       lower=True, unit_diagonal=True)
    u = tinv @ vb
    w = tinv @ (kb * jnp.exp(decay)[..., None])
    attn = jnp.where(tril, jnp.einsum('nbhid,nbhjd->nbhij', qc, kc) * lmat, 0.0)
    q_dec = qc * jnp.exp(decay)[..., None]
    k_dec = kc * jnp.exp(decay[..., -1:] - decay)[..., None]
    g_last = jnp.exp(decay[..., -1])

    def step(state, xs):
        u_i, w_i, attn_i, qd_i, kd_i, gl_i = xs
        v_new = u_i - w_i @ state
        o_i = qd_i @ state + attn_i @ v_new
        state = state * gl_i[..., None, None] + jnp.swapaxes(kd_i, -1, -2) @ v_new
        return state, o_i

    s0 = jnp.zeros((B, H, DK, DV), dtype=jnp.float32)
    _, o = lax.scan(step, s0, (u, w, attn, q_dec, k_dec, g_last))
    return o.transpose(1, 0, 3, 2, 4).reshape(B, S, H, DV)


def setup_inputs(seed: int = 0) -> dict:
    key = jax.random.key(seed)
    ks = jax.random.split(key, 24)
    f32 = jnp.float32
    L, D = DEPTH, D_MODEL

    def nrm(k, shape, scale):
        return jax.random.normal(k, shape, f32) * scale

    dt = jnp.exp(jax.random.uniform(ks[13], (L, DN_HEADS), f32, math.log(1e-3), math.log(1e-1)))
    return {
        "x": nrm(ks[0], (BATCH, SEQ, D), 1.0),
        "c": nrm(ks[1], (BATCH, D), 1.0),
        "positions": jnp.tile(jnp.arange(SEQ, dtype=jnp.int32)[None, :], (BATCH, 1)),
        "w_ada": nrm(ks[2], (L, D, 3 * D), D ** -0.5),
        "b_ada": nrm(ks[3], (L, 3 * D), 0.01),
        "norm_gain": 1.0 + nrm(ks[4], (L, D), 0.02),
        "w_in": nrm(ks[5], (L, D, D_IN), D ** -0.5),
        "cmp_pos_k": nrm(ks[6], (L, CMP_LEN, HEAD_DIM), 0.02),
        "cmp_pos_v": nrm(ks[7], (L, CMP_LEN, HEAD_DIM), 0.02),
        "w_cmp_k1": nrm(ks[8], (L, CMP_LEN * HEAD_DIM, CMP_HIDDEN), (CMP_LEN * HEAD_DIM) ** -0.5),
        "w_cmp_k2": nrm(ks[9], (L, CMP_HIDDEN, HEAD_DIM), CMP_HIDDEN ** -0.5),
        "w_cmp_v1": nrm(ks[10], (L, CMP_LEN * HEAD_DIM, CMP_HIDDEN), (CMP_LEN * HEAD_DIM) ** -0.5),
        "w_cmp_v2": nrm(ks[11], (L, CMP_HIDDEN, HEAD_DIM), CMP_HIDDEN ** -0.5),
        "conv_w": nrm(ks[12], (L, CONV_WIDTH, 3 * DN_WIDTH), CONV_WIDTH ** -0.5),
        "dt_bias": jnp.log(jnp.expm1(dt)),
        "a_log": jnp.log(jax.random.uniform(ks[14], (L, DN_HEADS), f32, 1.0, 16.0)),
        "dn_norm_gain": 1.0 + nrm(ks[15], (L, DN_DV), 0.02),
        "w_proj_a": nrm(ks[16], (L, NSA_WIDTH, D), NSA_WIDTH ** -0.5),
        "w_proj_b": nrm(ks[17], (L, DN_WIDTH, D), DN_WIDTH ** -0.5),
        "w_out": nrm(ks[18], (L, D, D), D ** -0.5),
        "final_gain": 1.0 + nrm(ks[19], (D,), 0.02),
    }


def reference(x, c, positions, w_ada, b_ada, norm_gain, w_in, cmp_pos_k, cmp_pos_v,
              w_cmp_k1, w_cmp_k2, w_cmp_v1, w_cmp_v2, conv_w, dt_bias, a_log, dn_norm_gain,
              w_proj_a, w_proj_b, w_out, final_gain):
    B, S, D = x.shape
    split_at = np.cumsum(IN_WIDTHS)[:-1].tolist()
    n_cmp = (S - CMP_LEN) // CMP_STRIDE + 1
    cmp_end_idx = np.arange(n_cmp) * CMP_STRIDE + CMP_LEN - 1
    for l in range(DEPTH):
        mod = c @ w_ada[l] + b_ada[l]
        shift, scale, gate = jnp.split(mod, 3, axis=-1)
        h = rms_norm(x, norm_gain[l]) * (1.0 + scale[:, None, :]) + shift[:, None, :]
        proj = h @ w_in[l]
        (nsa_q, nsa_kv, nsa_g, nsa_z, dn_qkv, dn_a, dn_b, dn_z, merge_g) = jnp.split(proj, split_at, axis=-1)

        q = partial_rope(nsa_q.reshape(B, S, NSA_HEADS, HEAD_DIM), positions)
        k_c, v_c, k_s, v_s, k_w, v_w = [t.reshape(B, S, NSA_GROUPS, HEAD_DIM) for t in jnp.split(nsa_kv, 6, axis=-1)]
        k_s = partial_rope(k_s, positions)
        k_w = partial_rope(k_w, positions)
        kc = compress_blocks(k_c, cmp_pos_k[l], w_cmp_k1[l], w_cmp_k2[l])
        vc = compress_blocks(v_c, cmp_pos_v[l], w_cmp_v1[l], w_cmp_v2[l])
        kc = partial_rope(kc, positions[:, cmp_end_idx])
        branch_gates = jax.nn.sigmoid(nsa_g.reshape(B, S, 3, NSA_HEADS))
        o_a = nsa_attention(q, kc, vc, k_s, v_s, k_w, v_w, branch_gates) * jax.nn.silu(nsa_z)
        out_a = o_a @ w_proj_a[l]

        qkv = jax.nn.silu(causal_conv(dn_qkv, conv_w[l]))
        dq, dk, dv = jnp.split(qkv, 3, axis=-1)
        dq = l2_norm(dq.reshape(B, S, DN_HEADS, DN_DK)) * (DN_DK ** -0.5)
        dk = l2_norm(dk.reshape(B, S, DN_HEADS, DN_DK))
        dv = dv.reshape(B, S, DN_HEADS, DN_DV)
        g = -jnp.exp(a_log[l].astype(jnp.float32)) * jax.nn.softplus(dn_a.astype(jnp.float32) + dt_bias[l].astype(jnp.float32))
        beta = jax.nn.sigmoid(dn_b.astype(jnp.float32))
        o_b = gated_delta_rule(dq.astype(jnp.float32), dk.astype(jnp.float32), dv.astype(jnp.float32), g, beta)
        o_b = rms_norm(o_b.astype(x.dtype), dn_norm_gain[l]).reshape(B, S, DN_WIDTH) * jax.nn.silu(dn_z)
        out_b = o_b @ w_proj_b[l]

        gate_a, gate_b = jnp.split(jax.nn.sigmoid(merge_g), 2, axis=-1)
        mixed = (gate_a * out_a + gate_b * out_b) @ w_out[l]
        x = x + gate[:, None, :] * mixed
    return rms_norm(x, final_gain)
```

```python
import numpy as np
from contextlib import ExitStack
import concourse.bass as bass
import concourse.mybir as mybir
from concourse.bass_utils import run_bass_kernel_spmd

F32 = mybir.dt.float32
BF16 = mybir.dt.bfloat16
I32 = mybir.dt.int32
AF = mybir.ActivationFunctionType
ALU = mybir.AluOpType
AX = mybir.AxisListType

D = 4096
S = 4096
NKC = 32
NCHUNK = 50
NSMALL = 20
EPS = 1e-6
GROUPS = [[0, 1, 2, 3], [4, 5, 6, 7]]

DEBUG = None
STOP_AFTER = None


class Buf:
    __slots__ = ("name", "w", "r", "excl")

    def __init__(self, name):
        self.name = name
        self.w = None
        self.r = {}
        self.excl = False


class K:
    ENG = ("pe", "act", "dve", "pool", "sp")

    def __init__(self, nc, stack):
        self.nc = nc
        self.e = {"pe": nc.tensor, "act": nc.scalar, "dve": nc.vector, "pool": nc.gpsimd, "sp": nc.sync}
        self.stack = stack
        self.sems = {}
        self.cnt = {}
        for en in self.ENG:
            self.sems[en] = stack.enter_context(nc.semaphore("s_" + en))
            self.cnt[en] = 0
        self.ring = [[stack.enter_context(nc.semaphore("d%d" % i)), 0] for i in range(40)]
        self.ring_i = 0
        self.cc_sem = stack.enter_context(nc.semaphore("cc"))
        self.cc_cnt = 0
        self.waited = {en: {} for en in self.ENG}
        self.nbuf = 0

    def buf(self, name=None):
        self.nbuf += 1
        return Buf(name or "b%d" % self.nbuf)

    def bufs(self, n, name="b"):
        return [self.buf("%s%d" % (name, i)) for i in range(n)]

    def _wait(self, eng, sem, val):
        key = id(sem)
        if self.waited[eng].get(key, -1) >= val:
            return
        self.waited[eng][key] = val
        self.e[eng].wait_ge(sem, val)

    def _deps(self, eng, R, W, is_dma):
        need = {}

        def add(ev):
            if ev is None:
                return
            sem, val, src = ev
            if src == "pe" and eng == "pe" and not is_dma:
                return
            k = id(sem)
            if k not in need or need[k][1] < val:
                need[k] = (sem, val)

        for b in R:
            add(b.w)
            if b.excl:
                for ev in b.r.values():
                    add(ev)
        for b in W:
            add(b.w)
            for ev in b.r.values():
                add(ev)
        for sem, val in need.values():
            self._wait(eng, sem, val)

    def _record(self, ev, R, W):
        for b in R:
            if b.excl:
                b.w = ev
                b.r = {}
                continue
            k = (ev[2], id(ev[0]))
            b.r[k] = ev
        for b in W:
            b.w = ev
            b.r = {}

    def op(self, eng, meth, R=(), W=(), **kw):
        self._deps(eng, R, W, False)
        ins = getattr(self.e[eng], meth)(**kw)
        self.cnt[eng] += 1
        ins.then_inc(self.sems[eng], 1)
        ev = (self.sems[eng], self.cnt[eng], eng)
        self._record(ev, R, W)
        return ev

    def dma(self, eng, out, in_, R=(), W=(), **kw):
        self._deps(eng, R, W, True)
        slot = self.ring[self.ring_i]
        self.ring_i = (self.ring_i + 1) % len(self.ring)
        if slot[1] > 0:
            self._wait(eng, slot[0], slot[1])
        ins = self.e[eng].dma_start(out=out, in_=in_, **kw)
        slot[1] += 16
        ins.then_inc(slot[0], 16)
        ev = (slot[0], slot[1], "dma")
        self._record(ev, R, W)
        return ev

    def allgather(self, in_t, out_t, R=(), W=()):
        self._deps("pool", R, W, True)
        ins = self.nc.gpsimd.collective_compute(
            "AllGather", ALU.bypass, replica_groups=GROUPS,
            ins=[in_t.ap().opt()], outs=[out_t.ap().opt()])
        self.cc_cnt += 1
        ins.then_inc(self.cc_sem)
        ev = (self.cc_sem, self.cc_cnt, "dma")
        self._record(ev, R, W)
        return ev

    def barrier(self):
        for en in self.ENG:
            for src in self.ENG:
                if self.cnt[src] > 0 and src != en:
                    self._wait(en, self.sems[src], self.cnt[src])
            for slot in self.ring:
                if slot[1] > 0:
                    self._wait(en, slot[0], slot[1])
            if self.cc_cnt:
                self._wait(en, self.cc_sem, self.cc_cnt)
        for en in self.ENG:
            if self.cnt[en] > 0 and en != "pe":
                self._wait(en, self.sems[en], self.cnt[en])

    def finish(self, out_bufs):
        self.barrier()


SCALE = 128.0 ** -0.5
TWO_PI = 2.0 * np.pi


def build_program():
    nc = bass.Bass("TRN2", target_bir_lowering=False)
    dbg = DEBUG or set()

    def din(name, shape, dt=F32):
        return nc.dram_tensor(name, list(shape), dt, kind="ExternalInput")

    dbg_pairs = []

    def dscr(name, shape, dt=F32):
        if name in dbg and name in ("projT", "small", "dnq", "ybuf"):
            t = nc.dram_tensor(name + "_i", list(shape), dt)
            o = nc.dram_tensor(name, list(shape), dt, kind="ExternalOutput")
            dbg_pairs.append((t, o))
            return t
        if name in dbg:
            return nc.dram_tensor(name, list(shape), dt, kind="ExternalOutput")
        return nc.dram_tensor(name, list(shape), dt)

    x_t = din("x", [S, D])
    xc_t = din("xcols", [S, 1024])
    c_t = din("c_pk", [128, NKC])
    wada_t = din("w_ada", [D, 3072])
    bada_t = din("b_ada", [1, 3072])
    ngain_t = din("ngain_pk", [128, NKC])
    gsel_t = din("gsel", [96, 8])
    win_t = din("w_in", [NCHUNK, 128, NKC * 128])
    wsm_t = din("w_small", [128, NKC * NSMALL])
    pos_t = din("pos", [1, S], I32)
    w1k_t = din("w1k", [D, 256])
    w1v_t = din("w1v", [D, 256])
    w2k_t = din("w2k", [256, 128])
    w2v_t = din("w2v", [256, 128])
    cposk_t = din("cposkT", [128, 32])
    cposv_t = din("cposvT", [128, 32])
    convw_t = din("convw", [128, 12 * 4])
    dnc_t = din("dnc", [64, 8 + 128])
    wpa_t = din("wpa", [8, 128, 16 * 128])
    wpb_t = din("wpb", [8, 128, 16 * 128])
    wo_t = din("wo", [8, 128, 32 * 128])
    fg_t = din("fgain", [128, 1024])
    ident_t = din("ident", [128, 128])
    c64_t = din("c64", [64, 256])
    c128_t = din("c128", [128, 256 + 128 + 1 + 128 + 128])
    rot_t = din("rotT", [32, 32])
    vmask_t = din("vmask", [32, 128, 64])
    fbias_t = din("fbias", [32, 128, 64])
    out_t = nc.dram_tensor("out", [S, 1024], F32, kind="ExternalOutput")

    modb_t = nc.dram_tensor("mod_b", [1, 3072], F32)
    modf_t = nc.dram_tensor("mod_f", [4, 3072], F32)
    projT_t = dscr("projT", [NCHUNK * 128, S])
    small_t = dscr("small", [S, NSMALL])
    dnq_t = dscr("dnq", [3 * 512, S])
    CW = min(1024, S)
    NCW = S // CW
    oa_b = [nc.dram_tensor("oa_b%d" % i, [512, CW], BF16) for i in range(NCW)]
    oa_f = [nc.dram_tensor("oa_f%d" % i, [2048, CW], BF16) for i in range(NCW)]
    ob_b = [nc.dram_tensor("ob_b%d" % i, [512, CW], BF16) for i in range(NCW)]
    ob_f = [nc.dram_tensor("ob_f%d" % i, [2048, CW], BF16) for i in range(NCW)]
    mg_b = [nc.dram_tensor("mg_b%d" % i, [1024, 512], BF16) for i in range(S // 512)]
    mg_f = [nc.dram_tensor("mg_f%d" % i, [4096, 512], BF16) for i in range(S // 512)]
    ybuf_t = dscr("ybuf", [S, 1024])
    ss_b = nc.dram_tensor("ss_b", [128, S // 128], F32)
    ss_f = nc.dram_tensor("ss_f", [512, S // 128], F32)
    oa_dbg = dscr("oa_dbg", [512, S]) if "oa_dbg" in dbg else None
    ob_dbg = dscr("ob_dbg", [512, S]) if "ob_dbg" in dbg else None
    qr_dbg = dscr("qr_dbg", [128, S]) if "qr_dbg" in dbg else None
    kc_dbg = dscr("kc_dbg", [128, 512]) if "kc_dbg" in dbg else None
    mg_dbg = dscr("mg_dbg", [1024, S]) if "mg_dbg" in dbg else None

    es = ExitStack()
    with es:
        k = K(nc, es)
        blk = es.enter_context(nc.Block())
        cur = [None]
        b_out = k.buf("out")

        def sb(name, shape, dt=F32):
            return cur[0].enter_context(nc.sbuf_tensor("sb_" + name, list(shape), dt))

        def ps(name, shape, dt=F32):
            return cur[0].enter_context(nc.psum_tensor("ps_" + name, list(shape), dt))

        def finish():
            k.barrier()
            for t, o in dbg_pairs:
                n = t.ap().shape[0]
                step = 128 if n % 128 == 0 else n
                for r0 in range(0, n, step):
                    k.dma("sp", o.ap()[r0:r0 + step, :], t.ap()[r0:r0 + step, :])
            k.barrier()

        stg = {}

        def stg_alloc(n=2, size=4096):
            stg["t"] = [sb("stg%d_%d" % (k.nbuf, i), [128, size]) for i in range(n)]
            stg["b"] = k.bufs(n, "stg")
            stg["i"] = 0

        def cast_load(dst, src, W, inner=None):
            i = stg["i"] % len(stg["t"])
            stg["i"] += 1
            n = 1
            for d_ in dst.shape[1:]:
                n *= d_
            sv = stg["t"][i][:, 0:n]
            if inner is not None:
                sv = sv.rearrange("p (a b) -> p a b", b=inner)
            k.dma("sp", sv, src, W=[stg["b"][i]])
            k.op("pool", "tensor_copy", R=[stg["b"][i]], W=W, out=dst, in_=sv)

        def mm(out, lhsT, rhs, R, W, start=True, stop=True):
            return k.op("pe", "matmul", R=R, W=W, out=out, lhsT=lhsT, rhs=rhs, start=start, stop=stop)

        def tr(out, in_, idn, R, W):
            return k.op("pe", "transpose", R=R, W=W, out=out, in_=in_, identity=idn)

        def act(out, in_, func, R, W, **kw):
            return k.op("act", "activation", R=R, W=W, out=out, in_=in_, func=func, **kw)

        def ts(out, in0, s1, s2, op0, op1, R, W, eng="dve", **kw):
            if op1 is None:
                return k.op(eng, "tensor_scalar", R=R, W=W, out=out, in0=in0, scalar1=s1, scalar2=None, op0=op0, **kw)
            return k.op(eng, "tensor_scalar", R=R, W=W, out=out, in0=in0, scalar1=s1, scalar2=s2, op0=op0, op1=op1, **kw)

        def tt(out, in0, in1, op, R, W, eng="dve"):
            return k.op(eng, "tensor_tensor", R=R, W=W, out=out, in0=in0, in1=in1, op=op)

        def stt(out, in0, scalar, in1, op0, op1, R, W, **kw):
            return k.op("dve", "scalar_tensor_tensor", R=R, W=W, out=out, in0=in0, scalar=scalar, in1=in1,
                        op0=op0, op1=op1, **kw)

        def cp(out, in_, R, W, eng="dve"):
            if eng == "act":
                return k.op("act", "activation", R=R, W=W, out=out, in_=in_, func=AF.Copy)
            return k.op(eng, "tensor_copy", R=R, W=W, out=out, in_=in_)

        @blk.sync
        def _(sync_eng):
            es_c = ExitStack()
            cur[0] = es_c
            ident = sb("ident", [128, 128])
            identb = sb("identb", [128, 128], BF16)
            ones = sb("ones", [128, 128])
            c64 = sb("c64", [64, 256])
            c128 = sb("c128", [128, 641])
            tri_b = sb("tri_b", [128, 128], BF16)
            triw_b = sb("triw_b", [128, 128], BF16)
            ones_b = sb("ones_b", [128, 512], BF16)
            rotT = sb("rotT", [32, 32])
            a_sb = sb("a_sb", [128, NKC])
            shift_sb = sb("shift_sb", [128, NKC])
            gate_sb = sb("gate_sb", [128, 8])
            b_cst = k.buf("cst")
            k.dma("sp", ident[:], ident_t.ap()[:, :], W=[b_cst])
            k.dma("sp", c64[:], c64_t.ap()[:, :], W=[b_cst])
            k.dma("sp", c128[:], c128_t.ap()[:, :], W=[b_cst])
            k.dma("sp", rotT[:], rot_t.ap()[:, :], W=[b_cst])
            cp(identb[:], ident[:], [b_cst], [b_cst])
            k.op("dve", "memset", W=[b_cst], ap=ones[:], constant=1.0)
            k.op("dve", "memset", W=[b_cst], ap=ones_b[:], constant=1.0)
            cp(tri_b[:], c128[:, 385:513], [b_cst], [b_cst])
            cp(triw_b[:], c128[:, 513:641], [b_cst], [b_cst])
            tri_le = c64[:, 0:64]
            lowst = c64[:, 64:128]
            lowd = c64[:, 128:192]
            lowo = c64[:, 192:256]
            cval = c128[:, 0:256]
            ovl = c128[:, 256:384].rearrange("p (a b) -> p a b", b=64)
            invf = c128[:, 384:385]
            b_mod = k.buf("modvecs")
            k.barrier()

            es_p = ExitStack()
            cur[0] = es_p
            c_sb = sb("c_sb", [128, NKC])
            bada_sb = sb("bada_sb", [1, 3072])
            mod_sb = sb("mod_sb", [1, 3072])
            wa = [sb("wa%d" % i, [128, NKC, 256]) for i in range(2)]
            b_wa = k.bufs(2, "wa")
            pm = [ps("pm%d" % i, [128, 512]) for i in range(2)]
            b_pm = k.bufs(2, "pm")
            b_c = k.buf("c")
            b_modsb = k.buf("modsb")
            k.dma("sp", c_sb[:], c_t.ap()[:, :], W=[b_c])
            k.dma("sp", bada_sb[:], bada_t.ap()[:, :], W=[b_c])
            wada_v = wada_t.ap().rearrange("(kc p) n -> p kc n", p=128)
            for ct in range(12):
                i = ct % 2
                k.dma("sp", wa[i][:], wada_v[:, :, ct * 256:(ct + 1) * 256], W=[b_wa[i]])
                for kc in range(NKC):
                    mm(pm[i][0:1, 0:256], c_sb[:, kc:kc + 1], wa[i][:, kc, :], [b_c, b_wa[i]], [b_pm[i]],
                       start=(kc == 0), stop=(kc == NKC - 1))
                tt(mod_sb[0:1, ct * 256:(ct + 1) * 256], pm[i][0:1, 0:256],
                   bada_sb[0:1, ct * 256:(ct + 1) * 256], ALU.add, [b_pm[i], b_c], [b_modsb])
            b_modb = k.buf("modb")
            b_modf = k.buf("modf")
            k.dma("sp", modb_t.ap()[:, :], mod_sb[:], R=[b_modsb], W=[b_modb])
            k.allgather(modb_t, modf_t, R=[b_modb], W=[b_modf])
            rows = sb("modrows", [96, 128])
            b_rows = k.buf("rows")
            modf_rows = modf_t.ap().rearrange("r (a p) -> (r a) p", p=128)
            k.dma("sp", rows[:], modf_rows, R=[b_modf], W=[b_rows])
            tr(pm[0][:, 0:96], rows[:], ident[0:96, 0:96], [b_rows], [b_pm[0]])
            ng_sb = sb("ng_sb", [128, NKC])
            k.dma("sp", ng_sb[:], ngain_t.ap()[:, :], W=[b_c])
            cp(shift_sb[:], pm[0][:, 0:32], [b_pm[0]], [b_mod])
            stt(a_sb[:], pm[0][:, 32:64], 1.0, ng_sb[:], ALU.add, ALU.mult, [b_pm[0], b_c], [b_mod])
            gsel = sb("gsel", [96, 8])
            k.dma("sp", gsel[:], gsel_t.ap()[:, :], W=[b_c])
            mm(pm[1][:, 0:8], rows[:], gsel[:], [b_rows, b_c], [b_pm[1]])
            cp(gate_sb[:], pm[1][:, 0:8], [b_pm[1]], [b_mod])
            k.barrier()
            es_p.close()

            es_p = ExitStack()
            cur[0] = es_p
            TT = 512
            xs = [sb("xs%d" % i, [128, D]) for i in range(4)]
            b_xs = k.bufs(4, "xs")
            junk = sb("junk", [128, D], BF16)
            b_junk = k.buf("junk")
            hT = sb("hT", [128, NKC, TT], BF16)
            b_hT = k.buf("hT")
            wb = [sb("wb%d" % i, [128, NKC, 128], BF16) for i in range(3)]
            b_wb = k.bufs(3, "wb")
            ob = [sb("ob%d" % i, [128, TT]) for i in range(2)]
            b_ob = k.bufs(2, "ob")
            wsm = sb("wsm", [128, NKC, NSMALL], BF16)
            b_wsm = k.buf("wsm")
            osm = sb("osm", [128, 4, NSMALL])
            b_osm = k.buf("osm")
            st = sb("stats", [128, 8])
            b_st = k.buf("st")
            ptr = [ps("ptr%d" % i, [128, 512]) for i in range(2)]
            b_ptr = k.bufs(2, "ptr")
            pp = [ps("pp%d" % i, [128, 512]) for i in range(2)]
            b_pp = k.bufs(2, "pp")
            psm = ps("psm", [128, 512])
            b_psm = k.buf("psm")
            b_projT = k.buf("projT")
            b_small = k.buf("small")
            stg_alloc(2)
            cast_load(wsm[:].rearrange("p a b -> p (a b)"), wsm_t.ap()[:, :], [b_wsm])
            n_tt = S // TT

            def chunk_func(ch):
                if 14 <= ch < 18 or 30 <= ch < 34:
                    return AF.Silu
                if ch >= 34:
                    return AF.Sigmoid
                return AF.Identity
            epi = 0
            for tt_i in range(n_tt):
                t0 = tt_i * TT
                for sub in range(4):
                    k.dma("sp", xs[sub][:], x_t.ap()[t0 + sub * 128:t0 + (sub + 1) * 128, :], W=[b_xs[sub]])
                    act(junk[:], xs[sub][:], AF.Square, [b_xs[sub]], [b_junk, b_st], accum_out=st[:, sub:sub + 1])
                ts(st[:, 0:4], st[:, 0:4], 1.0 / D, EPS, ALU.mult, ALU.add, [b_st], [b_st])
                act(st[:, 0:4], st[:, 0:4], AF.Sqrt, [b_st], [b_st])
                k.op("dve", "reciprocal", R=[b_st], W=[b_st], out=st[:, 4:8], in_=st[:, 0:4])
                for sub in range(4):
                    ts(xs[sub][:], xs[sub][:], st[:, 4 + sub:5 + sub], None, ALU.mult, None,
                       [b_xs[sub], b_st], [b_xs[sub]])
                for kc in range(NKC):
                    i = kc % 2
                    for sub in range(4):
                        tr(ptr[i][:, sub * 128:(sub + 1) * 128], xs[sub][:, kc * 128:(kc + 1) * 128], ident[:],
                           [b_xs[sub]], [b_ptr[i]])
                    act(hT[:, kc, :], ptr[i][:], AF.Identity, [b_ptr[i]], [b_hT],
                        scale=a_sb[:, kc:kc + 1], bias=shift_sb[:, kc:kc + 1])
                for sub in range(4):
                    for kc in range(NKC):
                        mm(psm[:, sub * 32:sub * 32 + NSMALL], hT[:, kc, sub * 128:(sub + 1) * 128], wsm[:, kc, :],
                           [b_hT, b_wsm], [b_psm], start=(kc == 0), stop=(kc == NKC - 1))
                cp(osm[:], psm[:, 0:128].rearrange("p (a b) -> p a b", b=32)[:, :, 0:NSMALL], [b_psm], [b_osm])
                k.dma("sp", small_t.ap()[t0:t0 + TT, :].rearrange("(a p) n -> p a n", p=128), osm[:],
                      R=[b_osm], W=[])
                for ch in range(NCHUNK):
                    wi = epi % 3
                    pi = epi % 2
                    epi += 1
                    cast_load(wb[wi][:].rearrange("p a b -> p (a b)"), win_t.ap()[ch, :, :], [b_wb[wi]])
                    for kc in range(NKC):
                        mm(pp[pi][:], wb[wi][:, kc, :], hT[:, kc, :], [b_hT, b_wb[wi]], [b_pp[pi]],
                           start=(kc == 0), stop=(kc == NKC - 1))
                    act(ob[pi][:], pp[pi][:], chunk_func(ch), [b_pp[pi]], [b_ob[pi]])
                    k.dma("sp", projT_t.ap()[ch * 128:(ch + 1) * 128, t0:t0 + TT], ob[pi][:],
                          R=[b_ob[pi]], W=[])
            k.barrier()
            es_p.close()
            if STOP_AFTER == "proj":
                finish()
                return

            es_n = ExitStack()
            cur[0] = es_n
            qT = sb("qT", [128, 8, S], BF16)
            kTs = sb("kTs", [128, S], BF16)
            kTw = sb("kTw", [128, S], BF16)
            Vs = sb("Vs", [128, S // 128, 128], BF16)
            Vw = sb("Vw", [128, S // 128, 128], BF16)
            kcmpT = sb("kcmpT", [128, 256], BF16)
            vcmp = sb("vcmp", [128, 2, 128], BF16)
            es_q = ExitStack()
            cur[0] = es_q
            kcT = sb("kcT", [128, S], BF16)
            vcT = sb("vcT", [128, S], BF16)
            cosT = sb("cosT", [32, S])
            sinT = sb("sinT", [32, S])
            HS = S // 2
            es_r = ExitStack()
            cur[0] = es_r
            posi = sb("posi", [32, HS], I32)
            ang = sb("ang", [32, HS])
            rtmp = sb("rtmp", [32, HS])
            rtmp2 = sb("rtmp2", [32, HS])
            rtmpi = sb("rtmpi", [32, HS], I32)
            b_r = k.buf("rope")
            RW = dict(R=[b_r], W=[b_r])
            for hf in range(2):
                hs = slice(hf * HS, (hf + 1) * HS)
                k.dma("sp", posi[:], pos_t.ap()[0:1, hs].to_broadcast([32, HS]), W=[b_r])
                cp(ang[:], posi[:], [b_r], [b_r])
                ts(ang[:], ang[:], invf[0:32, :], None, ALU.mult, None, [b_r], [b_r])
                for tab, off in ((sinT, 0.0), (cosT, float(np.pi / 2))):
                    ts(rtmp[:], ang[:], 1.0 / TWO_PI, off / TWO_PI, ALU.mult, ALU.add, [b_r], [b_r])
                    cp(rtmpi[:], rtmp[:], [b_r], [b_r])
                    cp(rtmp[:], rtmpi[:], [b_r], [b_r])
                    stt(rtmp[:], rtmp[:], -TWO_PI, ang[:], ALU.mult, ALU.add, [b_r], [b_r])
                    if off:
                        ts(rtmp[:], rtmp[:], off, None, ALU.add, None, [b_r], [b_r])
                    ts(rtmp2[:], rtmp[:], float(np.pi), -TWO_PI, ALU.is_gt, ALU.mult, [b_r], [b_r])
                    tt(rtmp[:], rtmp[:], rtmp2[:], ALU.add, [b_r], [b_r])
                    ts(rtmp2[:], rtmp[:], -float(np.pi), TWO_PI, ALU.is_lt, ALU.mult, [b_r], [b_r])
                    tt(rtmp[:], rtmp[:], rtmp2[:], ALU.add, [b_r], [b_r])
                    ts(rtmp[:], rtmp[:], float(np.pi), -float(np.pi), ALU.min, ALU.max, [b_r], [b_r])
                    act(tab[:, hs], rtmp[:], AF.Sin, [b_r], [b_r])
            k.barrier()
            es_r.close()
            cur[0] = es_q
            lb = [sb("lb%d" % i, [128, 512]) for i in range(3)]
            b_lb = k.bufs(3, "lb")
            rt1 = [sb("rt1_%d" % i, [32, 512]) for i in range(2)]
            rt2 = [sb("rt2_%d" % i, [32, 512]) for i in range(2)]
            b_rt = k.bufs(2, "rt")
            pr = [ps("pr%d" % i, [128, 512]) for i in range(2)]
            b_pr = k.bufs(2, "pr")
            b_dst = k.buf("nsadst")
            b_qdbg = k.buf("qdbg")
            li = 0
            plan = [(h, "rope", qT[:, h, :]) for h in range(8)]
            plan += [(8, "cast", kcT[:]), (9, "cast", vcT[:]), (10, "rope", kTs[:]), (11, "vT", Vs),
                     (12, "rope", kTw[:]), (13, "vT", Vw)]
            for ch, kind, dst in plan:
                for tt_i in range(S // 512):
                    t0 = tt_i * 512
                    a = li % 3
                    r = li % 2
                    li += 1
                    k.dma("sp", lb[a][:], projT_t.ap()[ch * 128:(ch + 1) * 128, t0:t0 + 512], W=[b_lb[a]])
                    if kind == "cast":
                        cp(dst[:, t0:t0 + 512], lb[a][:], [b_lb[a]], [b_dst], eng="act")
                    elif kind == "rope":
                        mm(pr[r][0:32, :], rotT[:, :], lb[a][0:32, :], [b_lb[a]], [b_pr[r]])
                        cp(dst[:, t0:t0 + 512], lb[a][:], [b_lb[a]], [b_dst], eng="act")
                        tt(rt1[r][:], lb[a][0:32, :], cosT[:, t0:t0 + 512], ALU.mult, [b_lb[a]], [b_rt[r]])
                        tt(rt2[r][:], pr[r][0:32, :], sinT[:, t0:t0 + 512], ALU.mult, [b_pr[r]], [b_rt[r]])
                        tt(dst[0:32, t0:t0 + 512], rt1[r][:], rt2[r][:], ALU.add, [b_rt[r]], [b_dst])
                    else:
                        for sub in range(4):
                            tr(pr[r][:, sub * 128:(sub + 1) * 128], lb[a][:, sub * 128:(sub + 1) * 128], ident[:],
                               [b_lb[a]], [b_pr[r]])
                        cp(dst[:, tt_i * 4:(tt_i + 1) * 4, :], pr[r][:].rearrange("p (a b) -> p a b", b=128),
                           [b_pr[r]], [b_dst])
            if qr_dbg is not None:
                qf = sb("qf_dbg", [128, S])
                cp(qf[:], qT[:, 0, :], [b_dst], [b_qdbg])
                k.dma("sp", qr_dbg.ap()[:, :], qf[:], R=[b_qdbg], W=[])
            k.barrier()

            NCMP = (S - 32) // 16 + 1
            stg_alloc(1, 2048)
            w1 = sb("w1", [128, 32, 256], BF16)
            w2 = sb("w2", [128, 2, 128], BF16)
            posT = sb("posT", [128, 32])
            posTb = sb("posTb", [128, 32], BF16)
            hid = sb("hid", [128, 2, 256], BF16)
            pbs = sb("pbs", [128, 2])
            kf = sb("kf", [128, 256])
            b_w = k.buf("cmpw")
            b_hid = k.buf("hid")
            b_kf = k.buf("kf")
            b_cmp = k.buf("cmpout")
            ph = pr[0]
            pb_ = pr[1]
            b_ph, b_pb = b_pr
            k.op("dve", "memset", W=[b_hid], ap=hid[:], constant=0.0)
            k.op("dve", "memset", W=[b_cmp], ap=kcmpT[:], constant=0.0)
            for which in range(2):
                w1_t = (w1k_t, w1v_t)[which]
                w2_t = (w2k_t, w2v_t)[which]
                cps_t = (cposk_t, cposv_t)[which]
                src = (kcT, vcT)[which]
                srcv = src[:].rearrange("p (n s) -> p n s", s=16)
                w1v = w1_t.ap().rearrange("(l d) h -> d l h", d=128)
                for a in range(4):
                    cast_load(w1[:, 8 * a:8 * a + 8, :], w1v[:, 8 * a:8 * a + 8, :], [b_w], inner=256)
                cast_load(w2[:], w2_t.ap().rearrange("(hc p) d -> p hc d", p=128), [b_w], inner=128)
                k.dma("sp", posT[:], cps_t.ap()[:, :], W=[b_w])
                cp(posTb[:], posT[:], [b_w], [b_w])
                for hc in range(2):
                    for l in range(32):
                        mm(pb_[:, 0:1], w1[:, l, hc * 128:(hc + 1) * 128], posTb[:, l:l + 1], [b_w], [b_pb],
                           start=(l == 0), stop=(l == 31))
                    cp(pbs[:, hc:hc + 1], pb_[:, 0:1], [b_pb], [b_hid])
                    for l in range(32):
                        rhs = srcv[:, 0:NCMP, l] if l < 16 else srcv[:, 1:NCMP + 1, l - 16]
                        mm(ph[:, 0:NCMP], w1[:, l, hc * 128:(hc + 1) * 128], rhs, [b_w], [b_ph],
                           start=(l == 0), stop=(l == 31))
                    act(hid[:, hc, 0:NCMP], ph[:, 0:NCMP], AF.Silu, [b_ph, b_hid], [b_hid], bias=pbs[:, hc:hc + 1])
                if which == 0:
                    for hc in range(2):
                        mm(ph[:, 0:NCMP], w2[:, hc, :], hid[:, hc, 0:NCMP], [b_w, b_hid], [b_ph],
                           start=(hc == 0), stop=(hc == 1))
                    cp(kf[:, 0:NCMP], ph[:, 0:NCMP], [b_ph], [b_kf])
                    mm(pb_[0:32, 0:NCMP], rotT[:, :], kf[0:32, 0:NCMP], [b_kf], [b_pb])
                    cosv = cosT[:].rearrange("p (n s) -> p n s", s=16)[:, 1:NCMP + 1, 15]
                    sinv = sinT[:].rearrange("p (n s) -> p n s", s=16)[:, 1:NCMP + 1, 15]
                    cp(kcmpT[:, 0:NCMP], kf[:, 0:NCMP], [b_kf], [b_cmp], eng="act")
                    tt(rt1[0][:, 0:NCMP], kf[0:32, 0:NCMP], cosv, ALU.mult, [b_kf], [b_rt[0]])
                    tt(rt2[0][:, 0:NCMP], pb_[0:32, 0:NCMP], sinv, ALU.mult, [b_pb], [b_rt[0]])
                    tt(kcmpT[0:32, 0:NCMP], rt1[0][:, 0:NCMP], rt2[0][:, 0:NCMP], ALU.add, [b_rt[0]], [b_cmp])
                    if kc_dbg is not None:
                        kfd = sb("kfd", [128, 512])
                        k.op("dve", "memset", W=[b_kf], ap=kfd[:], constant=0.0)
                        cp(kfd[:, 0:256], kcmpT[:], [b_cmp], [b_kf])
                else:
                    for nt in range(2):
                        for hc in range(2):
                            mm(ph[:, 0:128], hid[:, hc, nt * 128:(nt + 1) * 128], w2[:, hc, :], [b_w, b_hid], [b_ph],
                               start=(hc == 0), stop=(hc == 1))
                        cp(vcmp[:, nt, :], ph[:, 0:128], [b_ph], [b_cmp])
                        if kc_dbg is not None:
                            cp(kfd[:, 256 + nt * 128:256 + (nt + 1) * 128], ph[:, 0:128], [b_ph], [b_kf])
            if kc_dbg is not None:
                k.dma("sp", kc_dbg.ap()[:, :], kfd[:], R=[b_kf], W=[])
            k.barrier()
            es_q.close()

            es_a = ExitStack()
            cur[0] = es_a
            pS = [ps("pS%d" % i, [128, 512]) for i in range(2)]
            b_pS = k.bufs(2, "pS")
            pT = [ps("pT%d" % i, [128, 512], BF16) for i in range(2)]
            b_pT = k.bufs(2, "pT")
            pO = [ps("pO%d" % i, [128, 512]) for i in range(2)]
            b_pO = k.bufs(2, "pO")
            pI = ps("pI", [128, 512])
            b_pI = k.buf("pI")
            pX = ps("pX", [128, 512])
            b_pX = k.buf("pX")
            Pf = [sb("Pf%d" % i, [128, 512], BF16) for i in range(2)]
            b_Pf = k.bufs(2, "Pf")
            PTs = [sb("PTs%d" % i, [128, 512], BF16) for i in range(2)]
            b_PTs = k.bufs(2, "PTs")
            acc = sb("acc", [128, 256])
            b_acc = k.buf("acc")
            accT = sb("accT", [128, 2, 128])
            b_accT = k.buf("accT")
            cm = sb("cm", [128, 256], BF16)
            b_cm = k.buf("cm")
            vm = sb("vm", [128, 64])
            fb = sb("fb", [128, 64])
            b_vf = k.buf("vf")
            score = sb("score", [128, 64])
            sc2 = sb("sc2", [128, 64])
            m8 = sb("m8", [128, 8])
            m8b = sb("m8b", [128, 8])
            sel = sb("sel", [128, 64], BF16)
            b_sel = k.buf("sel")
            gts = sb("gts", [128, 12])
            b_gts = k.buf("gts")
            oacc = [sb("oacc%d" % i, [128, 128]) for i in range(4)]
            b_oacc = k.bufs(4, "oacc")
            zt = sb("zt", [128, 4, 128])
            b_zt = k.buf("zt")
            oT = sb("oT", [128, 4, 128], BF16)
            b_oT = k.buf("oT")
            oTf = sb("oTf", [128, 4, 128]) if oa_dbg is not None else None
            stt_ = sb("stt", [128, 256])
            b_stc = k.bufs(256, "stc")
            stc_i = [0]
            mxt = [sb("mxt%d" % i, [128, 16]) for i in range(4)]
            b_mxt = k.bufs(4, "mxt")
            mxt_i = [0]
            b_oab = k.buf("oab")
            tog = {"s": 0, "o": 0}

            def stat():
                c = stc_i[0] % 256
                stc_i[0] += 1
                return stt_[:, c:c + 1], b_stc[c]

            def finish_branch(h, o, rs_ap, b_rs, gcol, first):
                ri, b_ri = stat()
                ts(ri, rs_ap, 1e-30, None, ALU.max, None, [b_rs], [b_ri])
                k.op("dve", "reciprocal", R=[b_ri], W=[b_ri], out=ri, in_=ri)
                cf, b_cf = stat()
                tt(cf, ri, gts[:, gcol:gcol + 1], ALU.mult, [b_ri, b_gts], [b_cf])
                if first:
                    ts(oacc[h][:], pO[o][:, 0:128], cf, None, ALU.mult, None, [b_pO[o], b_cf], [b_oacc[h]])
                else:
                    stt(oacc[h][:], pO[o][:, 0:128], cf, oacc[h][:], ALU.mult, ALU.add,
                        [b_pO[o], b_cf, b_oacc[h]], [b_oacc[h]])
                return ri, b_ri

            def branch(qi, h, blocks, kT_, V_, use_sel, wfirst, gcol):
                t0 = qi * 128
                tiles = [blocks[a:a + 4] for a in range(0, len(blocks), 4)]
                mi = mxt_i[0] % 4
                mxt_i[0] += 1
                for ti, tl in enumerate(tiles):
                    w = 128 * len(tl)
                    k0 = tl[0] * 128
                    s = tog["s"]
                    tog["s"] ^= 1
                    mm(pS[s][:, 0:w], qT[:, h, t0:t0 + 128], kT_[:, k0:k0 + w], [], [b_pS[s]])
                    k.op("dve", "reduce_max", R=[b_pS[s]], W=[b_mxt[mi]], out=mxt[mi][:, ti:ti + 1],
                         in_=pS[s][:, 0:w], axis=AX.X)
                nm, b_nm = stat()
                k.op("dve", "reduce_max", R=[b_mxt[mi]], W=[b_nm], out=nm, in_=mxt[mi][:, 0:len(tiles)], axis=AX.X)
                ts(nm, nm, -SCALE, None, ALU.mult, None, [b_nm], [b_nm])
                o = tog["o"]
                tog["o"] ^= 1
                nblk = len(blocks)
                bdone = 0
                for ti, tl in enumerate(tiles):
                    w = 128 * len(tl)
                    k0 = tl[0] * 128
                    s = tog["s"]
                    tog["s"] ^= 1
                    mm(pS[s][:, 0:w], qT[:, h, t0:t0 + 128], kT_[:, k0:k0 + w], [], [b_pS[s]])
                    act(Pf[s][:, 0:w], pS[s][:, 0:w], AF.Exp, [b_pS[s], b_nm], [b_Pf[s]], scale=SCALE, bias=nm)
                    for bi, blk_ in enumerate(tl):
                        sl = slice(bi * 128, (bi + 1) * 128)
                        if blk_ == qi:
                            tt(Pf[s][:, sl], Pf[s][:, sl], tri_b[:], ALU.mult, [b_Pf[s]], [b_Pf[s]])
                        elif wfirst is not None and blk_ == wfirst:
                            tt(Pf[s][:, sl], Pf[s][:, sl], triw_b[:], ALU.mult, [b_Pf[s]], [b_Pf[s]])
                    nb2 = 2 * len(tl)
                    if use_sel:
                        in1 = sel[:, 2 * tl[0]:2 * tl[0] + nb2].unsqueeze(2).to_broadcast([128, nb2, 64])
                        pv_ = Pf[s][:, 0:w].rearrange("p (a b) -> p a b", b=64)
                        stt(pv_, pv_, 1.0, in1, ALU.mult, ALU.mult, [b_Pf[s], b_sel], [b_Pf[s], b_mxt[mi]],
                            accum_out=mxt[mi][:, 8 + ti:9 + ti])
                    else:
                        stt(Pf[s][:, 0:w], Pf[s][:, 0:w], 1.0, ones_b[:, 0:w], ALU.mult, ALU.mult,
                            [b_Pf[s]], [b_Pf[s], b_mxt[mi]], accum_out=mxt[mi][:, 8 + ti:9 + ti])
                    for bi in range(len(tl)):
                        sl = slice(bi * 128, (bi + 1) * 128)
                        tr(pT[s][:, sl], Pf[s][:, sl], identb[:], [b_Pf[s]], [b_pT[s]])
                    cp(PTs[s][:, 0:w], pT[s][:, 0:w], [b_pT[s]], [b_PTs[s]], eng="act")
                    for bi, blk_ in enumerate(tl):
                        sl = slice(bi * 128, (bi + 1) * 128)
                        mm(pO[o][:, 0:128], PTs[s][:, sl], V_[:, blk_, :], [b_PTs[s]], [b_pO[o]],
                           start=(bdone == 0), stop=(bdone == nblk - 1))
                        bdone += 1
                rs, b_rs = stat()
                k.op("dve", "reduce_sum", R=[b_mxt[mi]], W=[b_rs], out=rs, in_=mxt[mi][:, 8:8 + len(tiles)], axis=AX.X)
                finish_branch(h, o, rs, b_rs, gcol, False)

            for qi in range(S // 128):
                t0 = qi * 128
                k.dma("sp", gts[:], small_t.ap()[t0:t0 + 128, 0:12], W=[b_gts])
                act(gts[:], gts[:], AF.Sigmoid, [b_gts], [b_gts])
                k.dma("sp", vm[:], vmask_t.ap()[qi, :, :], W=[b_vf])
                k.dma("sp", fb[:], fbias_t.ap()[qi, :, :], W=[b_vf])
                k.dma("sp", zt[:], projT_t.ap()[14 * 128:18 * 128, t0:t0 + 128].rearrange("(h p) t -> p h t", p=128),
                      W=[b_zt])
                ts(cm[:], cval, float(128 * qi), None, ALU.is_le, None, [], [b_cm])
                for h in range(8):
                    s = tog["s"]
                    tog["s"] ^= 1
                    mm(pS[s][:, 0:256], qT[:, h, t0:t0 + 128], kcmpT[:, :], [], [b_pS[s]])
                    nm, b_nm = stat()
                    k.op("dve", "reduce_max", R=[b_pS[s]], W=[b_nm], out=nm, in_=pS[s][:, 0:256], axis=AX.X)
                    ts(nm, nm, -SCALE, None, ALU.mult, None, [b_nm], [b_nm])
                    act(Pf[s][:, 0:256], pS[s][:, 0:256], AF.Exp, [b_pS[s], b_nm], [b_Pf[s]], scale=SCALE, bias=nm)
                    rs, b_rs = stat()
                    stt(Pf[s][:, 0:256], Pf[s][:, 0:256], 1.0, cm[:], ALU.mult, ALU.mult, [b_Pf[s], b_cm],
                        [b_Pf[s], b_rs], accum_out=rs)
                    if h < 4:
                        for ct in range(2):
                            sl = slice(ct * 128, (ct + 1) * 128)
                            tr(pT[s][:, sl], Pf[s][:, sl], identb[:], [b_Pf[s]], [b_pT[s]])
                        cp(PTs[s][:, 0:256], pT[s][:, 0:256], [b_pT[s]], [b_PTs[s]], eng="act")
                        o = tog["o"]
                        tog["o"] ^= 1
                        for ct in range(2):
                            sl = slice(ct * 128, (ct + 1) * 128)
                            mm(pO[o][:, 0:128], PTs[s][:, sl], vcmp[:, ct, :], [b_PTs[s]], [b_pO[o]],
                               start=(ct == 0), stop=(ct == 1))
                        ri, b_ri = finish_branch(h, o, rs, b_rs, h, True)
                    else:
                        ri, b_ri = stat()
                        ts(ri, rs, 1e-30, None, ALU.max, None, [b_rs], [b_ri])
                        k.op("dve", "reciprocal", R=[b_ri], W=[b_ri], out=ri, in_=ri)
                    if h == 0:
                        ts(acc[:], Pf[s][:, 0:256], ri, None, ALU.mult, None, [b_Pf[s], b_ri], [b_acc])
                    else:
                        stt(acc[:], Pf[s][:, 0:256], ri, acc[:], ALU.mult, ALU.add, [b_Pf[s], b_ri, b_acc], [b_acc])
                for ct in range(2):
                    sl = slice(ct * 128, (ct + 1) * 128)
                    tr(pX[:, sl], acc[:, sl], ident[:], [b_acc], [b_pX])
                cp(accT[:], pX[:, 0:256].rearrange("p (a b) -> p a b", b=128), [b_pX], [b_accT])
                for ct in range(2):
                    mm(pI[:, 0:64], accT[:, ct, :], ovl[:, ct, :], [b_accT], [b_pI], start=(ct == 0), stop=(ct == 1))
                tt(score[:], pI[:, 0:64], vm[:], ALU.mult, [b_pI, b_vf], [b_sel])
                tt(score[:], score[:], fb[:], ALU.add, [b_sel, b_vf], [b_sel])
                k.op("dve", "max", R=[b_sel], W=[b_sel], out=m8[:], in_=score[:])
                k.op("dve", "match_replace", R=[b_sel], W=[b_sel], out=sc2[:], in_to_replace=m8[:],
                     in_values=score[:], imm_value=-2.0)
                k.op("dve", "max", R=[b_sel], W=[b_sel], out=m8b[:], in_=sc2[:])
                ts(sel[:], score[:], m8b[:, 7:8], None, ALU.is_ge, None, [b_sel], [b_sel])
                for h in range(4):
                    branch(qi, h, list(range(0, qi + 1)), kTs, Vs, True, None, 4 + h)
                    branch(qi, h, list(range(max(0, qi - 4), qi + 1)), kTw, Vw, False,
                           (qi - 4) if qi >= 4 else None, 8 + h)
                for h in range(4):
                    tr(pX[:, h * 128:(h + 1) * 128], oacc[h][:], ident[:], [b_oacc[h]], [b_pX])
                tt(oT[:], pX[:].rearrange("p (a b) -> p a b", b=128), zt[:], ALU.mult, [b_pX, b_zt], [b_oT])
                k.dma("sp", oa_b[t0 // CW].ap().rearrange("(h p) t -> p h t", p=128)[:, :, t0 % CW:t0 % CW + 128], oT[:],
                      R=[b_oT], W=[])
                if oa_dbg is not None:
                    tt(oTf[:], pX[:].rearrange("p (a b) -> p a b", b=128), zt[:], ALU.mult, [b_pX, b_zt], [b_oT])
                    k.dma("sp", oa_dbg.ap().rearrange("(h p) t -> p h t", p=128)[:, :, t0:t0 + 128], oTf[:],
                          R=[b_oT], W=[])
            k.barrier()
            es_a.close()
            es_n.close()
            if STOP_AFTER == "nsa":
                finish()
                return

            es_d = ExitStack()
            cur[0] = es_d
            cw = sb("cw", [128, 12, 4])
            dnc = sb("dnc", [64, 136])
            b_dc = k.buf("dnconst")
            k.dma("sp", cw[:].rearrange("p a b -> p (a b)"), convw_t.ap()[:, :], W=[b_dc])
            k.dma("sp", dnc[:], dnc_t.ap()[:, :], W=[b_dc])
            g_sb = sb("g_sb", [64, S // 64, 4])
            beta_sb = sb("beta_sb", [64, S // 64, 4])
            gb_raw = sb("gb_raw", [64, S // 64, 8])
            nea = sb("nea", [64, 4])
            es_dp = ExitStack()
            cur[0] = es_dp
            xb = [sb("xb%d" % i, [128, 515]) for i in range(2)]
            b_xb = k.bufs(2, "xb")
            yb = [sb("yb%d" % i, [128, 512]) for i in range(2)]
            b_yb = k.bufs(2, "yb")
            sq = sb("sq", [128, 512])
            rn = sb("rn", [128, 512])
            b_sq = k.buf("sq")
            pss = ps("pss", [128, 512])
            b_pss = k.buf("pss")
            b_dnq = k.buf("dnq")
            li = 0
            for ci in range(12):
                ch = 18 + ci
                for tt_i in range(S // 512):
                    t0 = tt_i * 512
                    a = li % 2
                    li += 1
                    if tt_i == 0:
                        k.op("dve", "memset", W=[b_xb[a]], ap=xb[a][:, 0:3], constant=0.0)
                        k.dma("sp", xb[a][:, 3:515], projT_t.ap()[ch * 128:(ch + 1) * 128, 0:512], W=[b_xb[a]])
                    else:
                        k.dma("sp", xb[a][:, 0:515], projT_t.ap()[ch * 128:(ch + 1) * 128, t0 - 3:t0 + 512],
                              W=[b_xb[a]])
                    ts(yb[a][:], xb[a][:, 3:515], cw[:, ci, 3:4], None, ALU.mult, None, [b_xb[a], b_dc], [b_yb[a]])
                    for i in (2, 1, 0):
                        stt(yb[a][:], xb[a][:, i:i + 512], cw[:, ci, i:i + 1], yb[a][:], ALU.mult, ALU.add,
                            [b_xb[a], b_dc, b_yb[a]], [b_yb[a]])
                    act(yb[a][:], yb[a][:], AF.Silu, [b_yb[a]], [b_yb[a]])
                    if ci < 8:
                        act(sq[:], yb[a][:], AF.Square, [b_yb[a]], [b_sq])
                        mm(pss[:], ones[:, :], sq[:], [b_sq], [b_pss])
                        ts(rn[:], pss[:], EPS, None, ALU.add, None, [b_pss], [b_sq])
                        act(rn[:], rn[:], AF.Sqrt, [b_sq], [b_sq])
                        k.op("dve", "reciprocal", R=[b_sq], W=[b_sq], out=rn[:], in_=rn[:])
                        if ci < 4:
                            stt(yb[a][:], yb[a][:], SCALE, rn[:], ALU.mult, ALU.mult, [b_yb[a], b_sq], [b_yb[a]])
                        else:
                            tt(yb[a][:], yb[a][:], rn[:], ALU.mult, [b_yb[a], b_sq], [b_yb[a]])
                    k.dma("sp", dnq_t.ap()[ci * 128:(ci + 1) * 128, t0:t0 + 512], yb[a][:], R=[b_yb[a]], W=[])
            b_g = k.buf("g")
            k.dma("sp", gb_raw[:], small_t.ap().rearrange("(c p) n -> p c n", p=64)[:, :, 12:20], W=[b_g])
            tt(g_sb[:], gb_raw[:, :, 0:4], dnc[:, 0:4].unsqueeze(1).to_broadcast([64, S // 64, 4]), ALU.add,
               [b_g, b_dc], [b_g])
            act(g_sb[:], g_sb[:], AF.Exp, [b_g], [b_g])
            ts(g_sb[:], g_sb[:], 1.0, None, ALU.add, None, [b_g], [b_g])
            act(g_sb[:], g_sb[:], AF.Ln, [b_g], [b_g])
            act(nea[:], dnc[:, 4:8], AF.Exp, [b_dc], [b_g])
            ts(nea[:], nea[:], -1.0, None, ALU.mult, None, [b_g], [b_g])
            tt(g_sb[:], g_sb[:], nea[:].unsqueeze(1).to_broadcast([64, S // 64, 4]), ALU.mult, [b_g], [b_g])
            act(beta_sb[:], gb_raw[:, :, 4:8], AF.Sigmoid, [b_g], [b_g])
            k.barrier()
            es_dp.close()
            cur[0] = es_d
            St = [sb("St%d" % i, [128, 128]) for i in range(4)]
            b_St = k.bufs(4, "St")
            for i in range(4):
                k.op("dve", "memset", W=[b_St[i]], ap=St[i][:], constant=0.0)
            qc = [sb("qc%d" % i, [128, 4, 64]) for i in range(2)]
            kc_ = [sb("kc%d" % i, [128, 4, 64]) for i in range(2)]
            vc_ = [sb("vc%d" % i, [128, 4, 64]) for i in range(2)]
            dzt = [sb("dzt%d" % i, [128, 4, 64]) for i in range(2)]
            b_ld = k.bufs(2, "dnld")
            obT = [sb("obT%d" % i, [128, 4, 64], BF16) for i in range(2)]
            b_obT = k.bufs(2, "obT")
            obTf = [sb("obTf%d" % i, [128, 4, 64]) for i in range(2)] if ob_dbg is not None else None
            NTMP = 144
            tmps = [sb("tmp%d" % i, [128, 128]) for i in range(NTMP)]
            b_tmps = k.bufs(NTMP, "tmp")
            tmp_i = [0]
            banks = [ps("dnp%d" % i, [128, 512]) for i in range(7)]
            b_banks = k.bufs(7, "dnbank")
            for b_ in b_banks:
                b_.excl = True
            b_slots = [b_banks[i // 4] for i in range(28)]
            slot_i = [0]
            b_obb = k.buf("obb")

            def tmp():
                i = tmp_i[0] % NTMP
                tmp_i[0] += 1
                return tmps[i], b_tmps[i]

            def pslot():
                i = slot_i[0] % 28
                slot_i[0] += 1
                return banks[i // 4][:, (i % 4) * 128:(i % 4 + 1) * 128], b_slots[i]

            I64 = ident[0:64, 0:64]
            H = slice(0, 64)
            dnq_v = dnq_t.ap().rearrange("(m h p) t -> m p h t", m=3, p=128)
            for c in range(S // 64):
                a = c % 2
                cs = slice(c * 64, (c + 1) * 64)
                k.dma("sp", qc[a][:], dnq_v[0][:, :, cs], W=[b_ld[a]])
                k.dma("sp", kc_[a][:], dnq_v[1][:, :, cs], W=[b_ld[a]])
                k.dma("sp", vc_[a][:], dnq_v[2][:, :, cs], W=[b_ld[a]])
                k.dma("sp", dzt[a][:], projT_t.ap()[30 * 128:34 * 128, cs].rearrange("(h p) t -> p h t", p=128),
                      W=[b_ld[a]])
                for hh in range(4):
                    L = [b_ld[a]]
                    gc = g_sb[:, c, hh:hh + 1]
                    bc = beta_sb[:, c, hh:hh + 1]
                    kT_h = kc_[a][:, hh, :]
                    qT_h = qc[a][:, hh, :]
                    vT_h = vc_[a][:, hh, :]
                    Gm, bGm = tmp()
                    ts(Gm[H, 0:64], lowst, gc, None, ALU.mult, None, [], [bGm])
                    cp(Gm[H, 64:65], gc, [], [bGm])
                    pDT, bDT = pslot()
                    mm(pDT[H, 0:64], Gm[H, 0:64], tri_le, [bGm], [bDT])
                    pDN, bDN = pslot()
                    mm(pDN[H, 0:65], tri_le, Gm[H, 0:65], [bGm], [bDN])
                    pgl, bgl = pslot()
                    mm(pgl[:, 0:1], ones[H, :], Gm[H, 64:65], [bGm], [bgl])
                    ET, bET = tmp()
                    act(ET[H, 0:64], pDT[H, 0:64], AF.Exp, [bDT], [bET])
                    tt(ET[H, 0:64], ET[H, 0:64], tri_le, ALU.mult, [bET], [bET])
                    EN, bEN = tmp()
                    act(EN[H, 0:64], pDN[H, 0:64], AF.Exp, [bDN], [bEN])
                    ENo, bENo = tmp()
                    tt(ENo[H, 0:64], EN[H, 0:64], lowo, ALU.mult, [bEN], [bENo])
                    tt(EN[H, 0:64], EN[H, 0:64], lowd, ALU.mult, [bEN], [bEN])
                    sc, bsc = tmp()
                    act(sc[H, 0:1], pDN[H, 64:65], AF.Exp, [bDN], [bsc])
                    cp(sc[H, 6:7], pDN[H, 64:65], [bDN], [bsc])
                    tt(sc[H, 5:6], pgl[H, 0:1], sc[H, 6:7], ALU.subtract, [bgl, bsc], [bsc])
                    act(sc[H, 1:2], sc[H, 5:6], AF.Exp, [bsc], [bsc])
                    tt(sc[H, 2:3], sc[H, 0:1], bc, ALU.mult, [bsc], [bsc])
                    ts(sc[H, 3:4], bc, -1.0, None, ALU.mult, None, [bsc], [bsc])
                    act(sc[:, 4:5], pgl[:, 0:1], AF.Exp, [bgl, bsc], [bsc])
                    pk, bpk = pslot()
                    tr(pk[H, :], kT_h, ident[:], L, [bpk])
                    kbd, bkbd = tmp()
                    ts(kbd[H, :], pk[H, :], sc[H, 2:3], None, ALU.mult, None, [bpk, bsc], [bkbd])
                    kd, bkd = tmp()
                    ts(kd[H, :], pk[H, :], sc[H, 1:2], None, ALU.mult, None, [bpk, bsc], [bkd])
                    pv, bpv = pslot()
                    tr(pv[H, :], vT_h, ident[:], L, [bpv])
                    vb, bvb = tmp()
                    ts(vb[H, :], pv[H, :], bc, None, ALU.mult, None, [bpv], [bvb])
                    pG, bpG = pslot()
                    mm(pG[H, 0:64], kT_h, kT_h, L, [bpG])
                    B, bB = tmp()
                    stt(B[H, 0:64], pG[H, 0:64], sc[H, 3:4], EN[H, 0:64], ALU.mult, ALU.mult, [bpG, bsc, bEN], [bB])
                    Bo, bBo = tmp()
                    stt(Bo[H, 0:64], pG[H, 0:64], sc[H, 3:4], ENo[H, 0:64], ALU.mult, ALU.mult, [bpG, bsc, bENo], [bBo])
                    pA, bpA = pslot()
                    mm(pA[H, 0:64], kT_h, qT_h, L, [bpA])
                    At, bAt = tmp()
                    tt(At[H, 0:64], pA[H, 0:64], ET[H, 0:64], ALU.mult, [bpA, bET], [bAt])
                    pC, bpC = pslot()
                    tr(pC[H, 0:64], B[H, 0:64], I64, [bB], [bpC])
                    C, bC = tmp()
                    cp(C[H, 0:64], pC[H, 0:64], [bpC], [bC], eng="act")
                    X, bX = tmp()
                    tt(X[H, 0:64], pC[H, 0:64], I64, ALU.add, [bpC], [bX])
                    Bp, bBp, Cp, bCp = B, bB, C, bC
                    for lev in range(1, 5):
                        pB2, bpB2 = pslot()
                        mm(pB2[H, 0:64], Cp[H, 0:64], Bp[H, 0:64], [bCp, bBp], [bpB2])
                        nB, bnB = tmp()
                        cp(nB[H, 0:64], pB2[H, 0:64], [bpB2], [bnB], eng="act")
                        if lev < 4:
                            pC2, bpC2 = pslot()
                            mm(pC2[H, 0:64], Bp[H, 0:64], Cp[H, 0:64], [bCp, bBp], [bpC2])
                            nC, bnC = tmp()
                            cp(nC[H, 0:64], pC2[H, 0:64], [bpC2], [bnC])
                        pX2, bpX2 = pslot()
                        mm(pX2[H, 0:64], nB[H, 0:64], X[H, 0:64], [bnB, bX], [bpX2])
                        nX, bnX = tmp()
                        tt(nX[H, 0:64], pX2[H, 0:64], X[H, 0:64], ALU.add, [bpX2, bX], [bnX])
                        Bp, bBp, X, bX = nB, bnB, nX, bnX
                        if lev < 4:
                            Cp, bCp = nC, bnC
                    pM1, bpM1 = pslot()
                    mm(pM1[H, 0:64], Bo[H, 0:64], X[H, 0:64], [bBo, bX], [bpM1])
                    M1, bM1 = tmp()
                    cp(M1[H, 0:64], pM1[H, 0:64], [bpM1], [bM1], eng="act")
                    pTd, bpTd = pslot()
                    tr(pTd[H, 0:64], X[H, 0:64], I64, [bX], [bpTd])
                    Td, bTd = tmp()
                    cp(Td[H, 0:64], pTd[H, 0:64], [bpTd], [bTd])
                    pM2, bpM2 = pslot()
                    mm(pM2[H, 0:64], Td[H, 0:64], M1[H, 0:64], [bTd, bM1], [bpM2])
                    Xf, bXf = tmp()
                    tt(Xf[H, 0:64], pM2[H, 0:64], X[H, 0:64], ALU.add, [bpM2, bX], [bXf])
                    X, bX = Xf, bXf
                    pu, bpu = pslot()
                    mm(pu[H, :], X[H, 0:64], vb[H, :], [bX, bvb], [bpu])
                    u, bu = tmp()
                    cp(u[H, :], pu[H, :], [bpu], [bu], eng="act")
                    pw, bpw = pslot()
                    mm(pw[:, 0:64], kbd[H, :], X[H, 0:64], [bX, bkbd], [bpw])
                    wT, bwT = tmp()
                    cp(wT[:, 0:64], pw[:, 0:64], [bpw], [bwT])
                    ppv, bppv = pslot()
                    mm(ppv[H, :], wT[:, 0:64], St[hh][:], [bwT, b_St[hh]], [bppv])
                    vn, bvn = tmp()
                    tt(vn[H, :], u[H, :], ppv[H, :], ALU.subtract, [bu, bppv], [bvn])
                    po1, bpo1 = pslot()
                    mm(po1[H, :], qT_h, St[hh][:], L + [b_St[hh]], [bpo1])
                    o1, bo1 = tmp()
                    ts(o1[H, :], po1[H, :], sc[H, 0:1], None, ALU.mult, None, [bpo1, bsc], [bo1])
                    po2, bpo2 = pslot()
                    mm(po2[H, :], At[H, 0:64], vn[H, :], [bAt, bvn], [bpo2])
                    o_, bo = tmp()
                    tt(o_[H, :], po2[H, :], o1[H, :], ALU.add, [bpo2, bo1], [bo])
                    pSn, bpSn = pslot()
                    mm(pSn[:, :], kd[H, :], vn[H, :], [bkd, bvn], [bpSn])
                    stt(St[hh][:], St[hh][:], sc[:, 4:5], pSn[:, :], ALU.mult, ALU.add,
                        [b_St[hh], bsc, bpSn], [b_St[hh]])
                    jk, bjk = tmp()
                    act(jk[H, :], o_[H, :], AF.Square, [bo], [bjk, bsc], accum_out=sc[H, 7:8])
                    ts(sc[H, 7:8], sc[H, 7:8], 1.0 / 128, EPS, ALU.mult, ALU.add, [bsc], [bsc])
                    act(sc[H, 7:8], sc[H, 7:8], AF.Sqrt, [bsc], [bsc])
                    k.op("dve", "reciprocal", R=[bsc], W=[bsc], out=sc[H, 7:8], in_=sc[H, 7:8])
                    on, bon = tmp()
                    stt(on[H, :], o_[H, :], sc[H, 7:8], dnc[:, 8:136], ALU.mult, ALU.mult, [bo, bsc], [bon])
                    pot, bpot = pslot()
                    tr(pot[:, 0:64], on[H, :], I64, [bon], [bpot])
                    tt(obT[a][:, hh, :], pot[:, 0:64], dzt[a][:, hh, :], ALU.mult, [bpot] + L, [b_obT[a]])
                    if ob_dbg is not None:
                        tt(obTf[a][:, hh, :], pot[:, 0:64], dzt[a][:, hh, :], ALU.mult, [bpot] + L, [b_obT[a]])
                k.dma("sp", ob_b[(c * 64) // CW].ap().rearrange("(h p) t -> p h t", p=128)[:, :, (c * 64) % CW:(c * 64) % CW + 64],
                      obT[a][:], R=[b_obT[a]], W=[])
                if ob_dbg is not None:
                    k.dma("sp", ob_dbg.ap().rearrange("(h p) t -> p h t", p=128)[:, :, cs], obTf[a][:],
                          R=[b_obT[a]], W=[])
            k.barrier()
            es_d.close()
            if STOP_AFTER == "dn":
                finish()
                return

            b_g1 = k.buf("gath1")
            for i in range(NCW):
                k.allgather(oa_b[i], oa_f[i])
                k.allgather(ob_b[i], ob_f[i])
            k.barrier()
            es_2 = ExitStack()
            cur[0] = es_2
            wpa = sb("wpa", [128, 8, 16, 128], BF16)
            wpb = sb("wpb", [128, 8, 16, 128], BF16)
            b_wp = k.buf("wp")
            stg_alloc(2)
            for ch in range(8):
                cast_load(wpa[:, ch, :, :].rearrange("p a b -> p (a b)"), wpa_t.ap()[ch, :, :], [b_wp])
                cast_load(wpb[:, ch, :, :].rearrange("p a b -> p (a b)"), wpb_t.ap()[ch, :, :], [b_wp])
            oaT = [sb("oaT%d" % i, [128, 16, 512], BF16) for i in range(2)]
            obT2 = [sb("obT2%d" % i, [128, 16, 512], BF16) for i in range(2)]
            b_oT2 = k.bufs(2, "oT2")
            ga = [sb("ga%d" % i, [128, 512]) for i in range(2)]
            gb_ = [sb("gb%d" % i, [128, 512]) for i in range(2)]
            b_gg = k.bufs(2, "gg")
            t1 = [sb("t1%d" % i, [128, 512]) for i in range(2)]
            t2 = [sb("t2%d" % i, [128, 512]) for i in range(2)]
            mgo = [sb("mgo%d" % i, [128, 512], BF16) for i in range(2)]
            mgof = [sb("mgof%d" % i, [128, 512]) for i in range(2)] if mg_dbg is not None else None
            b_mgo = k.bufs(2, "mgo")
            pa = [ps("pa%d" % i, [128, 512]) for i in range(2)]
            pb2 = [ps("pb2%d" % i, [128, 512]) for i in range(2)]
            b_pab = k.bufs(2, "pab")
            b_mgb = k.buf("mgb")
            it = 0
            for tt_i in range(S // 512):
                t0 = tt_i * 512
                a = tt_i % 2
                oa_v = oa_f[t0 // CW].ap().rearrange("(kc p) t -> p kc t", p=128)
                ob_v = ob_f[t0 // CW].ap().rearrange("(kc p) t -> p kc t", p=128)
                k.dma("sp", oaT[a][:], oa_v[:, :, t0 % CW:t0 % CW + 512], W=[b_oT2[a]])
                k.dma("sp", obT2[a][:], ob_v[:, :, t0 % CW:t0 % CW + 512], W=[b_oT2[a]])
                for ch in range(8):
                    p = it % 2
                    it += 1
                    k.dma("sp", ga[p][:], projT_t.ap()[(34 + ch) * 128:(35 + ch) * 128, t0:t0 + 512], W=[b_gg[p]])
                    k.dma("sp", gb_[p][:], projT_t.ap()[(42 + ch) * 128:(43 + ch) * 128, t0:t0 + 512], W=[b_gg[p]])
                    for kc in range(16):
                        mm(pa[p][:], wpa[:, ch, kc, :], oaT[a][:, kc, :], [b_wp, b_oT2[a]], [b_pab[p]],
                           start=(kc == 0), stop=(kc == 15))
                    for kc in range(16):
                        mm(pb2[p][:], wpb[:, ch, kc, :], obT2[a][:, kc, :], [b_wp, b_oT2[a]], [b_pab[p]],
                           start=(kc == 0), stop=(kc == 15))
                    tt(t1[p][:], pa[p][:], ga[p][:], ALU.mult, [b_pab[p], b_gg[p]], [b_mgo[p]])
                    tt(t2[p][:], pb2[p][:], gb_[p][:], ALU.mult, [b_pab[p], b_gg[p]], [b_mgo[p]])
                    tt(mgo[p][:], t1[p][:], t2[p][:], ALU.add, [b_mgo[p]], [b_mgo[p]], eng="pool")
                    k.dma("sp", mg_b[tt_i].ap()[ch * 128:(ch + 1) * 128, :], mgo[p][:], R=[b_mgo[p]], W=[])
                    if mg_dbg is not None:
                        tt(mgof[p][:], t1[p][:], t2[p][:], ALU.add, [b_mgo[p]], [b_mgo[p]])
                        k.dma("sp", mg_dbg.ap()[ch * 128:(ch + 1) * 128, t0:t0 + 512], mgof[p][:], R=[b_mgo[p]], W=[])
            k.barrier()
            es_2.close()
            b_g2 = k.buf("gath2")
            for i in range(S // 512):
                k.allgather(mg_b[i], mg_f[i])
            k.barrier()

            es_3 = ExitStack()
            cur[0] = es_3
            wo = sb("wo", [128, 8, 32, 128], BF16)
            b_wo = k.buf("wo")
            es_3s = ExitStack()
            cur[0] = es_3s
            stg_alloc(2)
            for ch in range(8):
                cast_load(wo[:, ch, :, :].rearrange("p a b -> p (a b)"), wo_t.ap()[ch, :, :], [b_wo])
            k.barrier()
            es_3s.close()
            cur[0] = es_3
            mgT = [sb("mgT%d" % i, [128, 32, 512], BF16) for i in range(2)]
            xcs = [sb("xcs%d" % i, [128, 4, 1024]) for i in range(2)]
            b_in7 = k.bufs(2, "in7")
            y1 = [sb("y1%d" % i, [128, 512]) for i in range(2)]
            b_y1 = k.bufs(2, "y1")
            ssq = sb("ssq", [128, S // 128])
            b_ssq = k.buf("ssq")
            junk2 = sb("junk2", [128, 1024], BF16)
            b_j2 = k.buf("j2")
            pm2 = [ps("pm2%d" % i, [128, 512]) for i in range(2)]
            b_pm2 = k.bufs(2, "pm2")
            pt2 = [ps("pt2%d" % i, [128, 512]) for i in range(2)]
            b_pt2 = k.bufs(2, "pt2")
            b_yb_ = k.buf("ybuf")
            it = 0
            for tt_i in range(S // 512):
                t0 = tt_i * 512
                a = tt_i % 2
                k.dma("sp", mgT[a][:], mg_f[tt_i].ap().rearrange("(kc p) t -> p kc t", p=128), W=[b_in7[a]])
                k.dma("sp", xcs[a][:], xc_t.ap()[t0:t0 + 512, :].rearrange("(s p) n -> p s n", p=128), W=[b_in7[a]])
                for ch in range(8):
                    p = it % 2
                    it += 1
                    for kc in range(32):
                        mm(pm2[p][:], wo[:, ch, kc, :], mgT[a][:, kc, :], [b_wo, b_in7[a]], [b_pm2[p]],
                           start=(kc == 0), stop=(kc == 31))
                    act(y1[p][:], pm2[p][:], AF.Identity, [b_pm2[p]], [b_y1[p]], scale=gate_sb[:, ch:ch + 1])
                    for sub in range(4):
                        tr(pt2[p][:, sub * 128:(sub + 1) * 128], y1[p][:, sub * 128:(sub + 1) * 128], ident[:],
                           [b_y1[p]], [b_pt2[p]])
                    xv = xcs[a][:, :, ch * 128:(ch + 1) * 128]
                    tt(xv, pt2[p][:].rearrange("p (s c) -> p s c", c=128), xv, ALU.add, [b_pt2[p], b_in7[a]], [b_in7[a]])
                for sub in range(4):
                    act(junk2[:], xcs[a][:, sub, :], AF.Square, [b_in7[a]], [b_j2, b_ssq],
                        accum_out=ssq[:, tt_i * 4 + sub:tt_i * 4 + sub + 1])
                k.dma("sp", ybuf_t.ap()[t0:t0 + 512, :].rearrange("(s p) n -> p s n", p=128), xcs[a][:],
                      R=[b_in7[a]], W=[])
            b_ssb = k.buf("ssb")
            k.dma("sp", ss_b.ap()[:, :], ssq[:], R=[b_ssq], W=[b_ssb])
            k.barrier()
            k.allgather(ss_b, ss_f, W=[b_ssb])
            k.barrier()
            ssf = sb("ssf", [128, 4, S // 128])
            rstd = sb("rstd", [128, S // 128])
            fg = sb("fg", [128, 1024])
            b_fin = k.buf("fin")
            k.dma("sp", ssf[:], ss_f.ap().rearrange("(r p) n -> p r n", p=128), W=[b_fin])
            k.dma("sp", fg[:], fg_t.ap()[:, :], W=[b_fin])
            tt(rstd[:], ssf[:, 0, :], ssf[:, 1, :], ALU.add, [b_fin], [b_fin])
            tt(rstd[:], rstd[:], ssf[:, 2, :], ALU.add, [b_fin], [b_fin])
            tt(rstd[:], rstd[:], ssf[:, 3, :], ALU.add, [b_fin], [b_fin])
            ts(rstd[:], rstd[:], 1.0 / D, EPS, ALU.mult, ALU.add, [b_fin], [b_fin])
            act(rstd[:], rstd[:], AF.Sqrt, [b_fin], [b_fin])
            k.op("dve", "reciprocal", R=[b_fin], W=[b_fin], out=rstd[:], in_=rstd[:])
            yt = [sb("yt%d" % i, [128, 1024]) for i in range(2)]
            b_yt = k.bufs(2, "yt")
            for tile in range(S // 128):
                a = tile % 2
                k.dma("sp", yt[a][:], ybuf_t.ap()[tile * 128:(tile + 1) * 128, :], W=[b_yt[a]])
                stt(yt[a][:], yt[a][:], rstd[:, tile:tile + 1], fg[:], ALU.mult, ALU.mult, [b_yt[a], b_fin], [b_yt[a]])
                k.dma("sp", out_t.ap()[tile * 128:(tile + 1) * 128, :], yt[a][:], R=[b_yt[a]], W=[])
            finish()
            es_3.close()
            es_c.close()
    return nc


def _col_index(j):
    g, half = j // 2, j % 2
    o_q, o_kv, o_g, o_z = 0, 2048, 2048 + 1536, 2048 + 1536 + 48
    o_dn = o_z + 2048
    o_a = o_dn + 6144
    o_b = o_a + 16
    o_dz = o_b + 16
    o_mg = o_dz + 2048
    cols = []
    my_heads = [8 * g + 4 * half + i for i in range(4)]
    ot_heads = [8 * g + 4 * (1 - half) + i for i in range(4)]
    for h in my_heads + ot_heads:
        cols += list(range(o_q + h * 128, o_q + (h + 1) * 128))
    for t in range(6):
        cols += list(range(o_kv + t * 256 + g * 128, o_kv + t * 256 + (g + 1) * 128))
    for h in my_heads:
        cols += list(range(o_z + h * 128, o_z + (h + 1) * 128))
    dn_heads = [4 * j + i for i in range(4)]
    for t in range(3):
        for h in dn_heads:
            cols += list(range(o_dn + t * 2048 + h * 128, o_dn + t * 2048 + (h + 1) * 128))
    for h in dn_heads:
        cols += list(range(o_dz + h * 128, o_dz + (h + 1) * 128))
    cols += list(range(o_mg + j * 1024, o_mg + (j + 1) * 1024))
    cols += list(range(o_mg + 4096 + j * 1024, o_mg + 4096 + (j + 1) * 1024))
    small = []
    for br in range(3):
        small += [o_g + br * 16 + h for h in my_heads]
    small += [o_a + h for h in dn_heads]
    small += [o_b + h for h in dn_heads]
    return np.array(cols), np.array(small)


def _pk(v):
    return np.ascontiguousarray(v.reshape(NKC, 128).T)


def _wlayout(w, nkc):
    w = w.reshape(nkc, 128, 8, 128).transpose(2, 1, 0, 3)
    return np.ascontiguousarray(w).reshape(8, 128, nkc * 128)


def _consts():
    f32 = np.float32
    idx = np.arange(64)
    tri_le = (idx[:, None] <= idx[None, :]).astype(f32)
    lowst = (idx[:, None] > idx[None, :]).astype(f32)
    bd = (idx[:, None] // 32 == idx[None, :] // 32).astype(f32)
    c64 = np.concatenate([tri_le, lowst, lowst * bd, lowst * (1 - bd)], 1)
    p = np.arange(128)[:, None]
    c = np.arange(256)[None, :]
    cval = (16 * c + 31 - p).astype(f32)
    cval[:, 255] = 1e9
    cmp_start = np.arange(255) * 16
    slc_start = np.arange(64) * 64
    ov = ((cmp_start[:, None] <= slc_start[None, :] + 63) & (cmp_start[:, None] + 31 >= slc_start[None, :])).astype(f32)
    ov = np.concatenate([ov, np.zeros((1, 64), f32)], 0)
    ovl = ov.reshape(2, 128, 64).transpose(1, 0, 2).reshape(128, 128)
    d = np.arange(128)
    invf = np.where(d < 32, 500000.0 ** (-(d % 16) / 16.0), 0.0).astype(f32)[:, None]
    cc = np.arange(128)[None, :]
    tri128 = (cc <= p).astype(f32)
    triw = (cc > p).astype(f32)
    c128 = np.concatenate([cval, ovl, invf, tri128, triw], 1).astype(f32)
    rotT = np.zeros((32, 32), f32)
    for m in range(16):
        rotT[m + 16, m] = -1.0
        rotT[m, m + 16] = 1.0
    vmask = np.zeros((32, 128, 64), f32)
    fbias = np.zeros((32, 128, 64), f32)
    j = np.arange(64)[None, :]
    for qi in range(32):
        t = (128 * qi + np.arange(128))[:, None]
        tb = t // 64
        valid = (j * 64 <= t)
        forced = (j == 0) | (j == tb) | (j == tb - 1)
        vmask[qi] = (valid & ~forced)
        fbias[qi] = np.where(forced, 1e9, np.where(valid, 0.0, -1.0))
    return dict(ident=np.eye(128, dtype=f32), c64=c64, c128=c128, rotT=rotT, vmask=vmask, fbias=fbias)


def make_in_maps(inp):
    f32 = np.float32
    maps = []
    cst = _consts()
    w_in = np.asarray(inp["w_in"][0])
    w_ada = np.asarray(inp["w_ada"][0])
    conv_w = np.asarray(inp["conv_w"][0])
    for core in range(8):
        b, j = core // 4, core % 4
        cols, small = _col_index(j)
        wj = w_in[:, cols]
        wj = wj.reshape(NKC, 128, NCHUNK, 128).transpose(2, 1, 0, 3)
        wj = np.ascontiguousarray(wj).reshape(NCHUNK, 128, NKC * 128)
        ws = w_in[:, small].reshape(NKC, 128, NSMALL).transpose(1, 0, 2)
        ws = np.ascontiguousarray(ws).reshape(128, NKC * NSMALL)
        gsel = np.zeros((96, 8), f32)
        for ch in range(8):
            gsel[64 + 8 * j + ch, ch] = 1.0
        dn_heads = [4 * j + i for i in range(4)]
        cw = np.zeros((128, 12, 4), f32)
        for ci in range(12):
            t, h = ci // 4, dn_heads[ci % 4]
            cw[:, ci, :] = conv_w[:, t * 2048 + h * 128:t * 2048 + (h + 1) * 128].T
        dnc = np.zeros((64, 136), f32)
        dnc[:, 0:4] = np.asarray(inp["dt_bias"][0])[dn_heads][None, :]
        dnc[:, 4:8] = np.asarray(inp["a_log"][0])[dn_heads][None, :]
        dnc[:, 8:136] = np.asarray(inp["dn_norm_gain"][0])[None, :]
        cs = slice(j * 1024, (j + 1) * 1024)
        m = {
            "x": np.ascontiguousarray(inp["x"][b]),
            "xcols": np.ascontiguousarray(inp["x"][b][:, cs]),
            "c_pk": _pk(np.asarray(inp["c"][b])),
            "w_ada": np.ascontiguousarray(w_ada[:, j * 3072:(j + 1) * 3072]),
            "b_ada": np.ascontiguousarray(inp["b_ada"][0][j * 3072:(j + 1) * 3072]).reshape(1, 3072),
            "ngain_pk": _pk(np.asarray(inp["norm_gain"][0])),
            "gsel": gsel,
            "w_in": wj,
            "w_small": ws,
            "pos": np.ascontiguousarray(inp["positions"][b]).reshape(1, S).astype(np.int32),
            "w1k": np.ascontiguousarray(inp["w_cmp_k1"][0]),
            "w1v": np.ascontiguousarray(inp["w_cmp_v1"][0]),
            "w2k": np.ascontiguousarray(inp["w_cmp_k2"][0]),
            "w2v": np.ascontiguousarray(inp["w_cmp_v2"][0]),
            "cposkT": np.ascontiguousarray(inp["cmp_pos_k"][0].T),
            "cposvT": np.ascontiguousarray(inp["cmp_pos_v"][0].T),
            "convw": cw.reshape(128, 48),
            "dnc": dnc,
            "wpa": _wlayout(np.asarray(inp["w_proj_a"][0])[:, cs], 16),
            "wpb": _wlayout(np.asarray(inp["w_proj_b"][0])[:, cs], 16),
            "wo": _wlayout(np.asarray(inp["w_out"][0])[:, cs], 32),
            "fgain": np.ascontiguousarray(np.broadcast_to(np.asarray(inp["final_gain"])[cs][None, :], (128, 1024))),
        }
        m.update(cst)
        maps.append(m)
    return maps


def kernel(**inputs):
    inp = {k_: np.asarray(v) for k_, v in inputs.items()}
    nc = build_program()
    maps = make_in_maps(inp)
    res = run_bass_kernel_spmd(nc, maps, core_ids=list(range(8)))
    out = np.zeros((2, S, D), np.float32)
    for core in range(8):
        b, j = core // 4, core % 4
        out[b][:, j * 1024:(j + 1) * 1024] = res.results[core]["out"]
    return out
```

```python
import numpy as np
from contextlib import ExitStack
import concourse.bass as bass
import concourse.mybir as mybir
from concourse.bass_utils import run_bass_kernel_spmd

F32 = mybir.dt.float32
BF16 = mybir.dt.bfloat16
I32 = mybir.dt.int32
AF = mybir.ActivationFunctionType
ALU = mybir.AluOpType
AX = mybir.AxisListType

D = 4096
S = 4096
NKC = 32
NCHUNK = 50
NSMALL = 20
EPS = 1e-6
GROUPS = [[0, 1, 2, 3], [4, 5, 6, 7]]

DEBUG = None
STOP_AFTER = None


class Buf:
    __slots__ = ("name", "w", "r", "excl")

    def __init__(self, name):
        self.name = name
        self.w = None
        self.r = {}
        self.excl = False


class K:
    ENG = ("pe", "act", "dve", "pool", "sp")

    def __init__(self, nc, stack):
        self.nc = nc
        self.e = {"pe": nc.tensor, "act": nc.scalar, "dve": nc.vector, "pool": nc.gpsimd, "sp": nc.sync}
        self.stack = stack
        self.sems = {}
        self.cnt = {}
        for en in self.ENG:
            self.sems[en] = stack.enter_context(nc.semaphore("s_" + en))
            self.cnt[en] = 0
        self.ring = [[stack.enter_context(nc.semaphore("d%d" % i)), 0] for i in range(40)]
        self.ring_i = 0
        self.cc_sem = stack.enter_context(nc.semaphore("cc"))
        self.cc_cnt = 0
        self.waited = {en: {} for en in self.ENG}
        self.nbuf = 0

    def buf(self, name=None):
        self.nbuf += 1
        return Buf(name or "b%d" % self.nbuf)

    def bufs(self, n, name="b"):
        return [self.buf("%s%d" % (name, i)) for i in range(n)]

    def _wait(self, eng, sem, val):
        key = id(sem)
        if self.waited[eng].get(key, -1) >= val:
            return
        self.waited[eng][key] = val
        self.e[eng].wait_ge(sem, val)

    def _deps(self, eng, R, W, is_dma):
        need = {}

        def add(ev):
            if ev is None:
                return
            sem, val, src = ev
            if src == "pe" and eng == "pe" and not is_dma:
                return
            k = id(sem)
            if k not in need or need[k][1] < val:
                need[k] = (sem, val)

        for b in R:
            add(b.w)
            if b.excl:
                for ev in b.r.values():
                    add(ev)
        for b in W:
            add(b.w)
            for ev in b.r.values():
                add(ev)
        for sem, val in need.values():
            self._wait(eng, sem, val)

    def _record(self, ev, R, W):
        for b in R:
            if b.excl:
                b.w = ev
                b.r = {}
                continue
            k = (ev[2], id(ev[0]))
            b.r[k] = ev
        for b in W:
            b.w = ev
            b.r = {}

    def op(self, eng, meth, R=(), W=(), **kw):
        self._deps(eng, R, W, False)
        ins = getattr(self.e[eng], meth)(**kw)
        self.cnt[eng] += 1
        ins.then_inc(self.sems[eng], 1)
        ev = (self.sems[eng], self.cnt[eng], eng)
        self._record(ev, R, W)
        return ev

    def dma(self, eng, out, in_, R=(), W=(), **kw):
        self._deps(eng, R, W, True)
        slot = self.ring[self.ring_i]
        self.ring_i = (self.ring_i + 1) % len(self.ring)
        if slot[1] > 0:
            self._wait(eng, slot[0], slot[1])
        ins = self.e[eng].dma_start(out=out, in_=in_, **kw)
        slot[1] += 16
        ins.then_inc(slot[0], 16)
        ev = (slot[0], slot[1], "dma")
        self._record(ev, R, W)
        return ev

    def allgather(self, in_t, out_t, R=(), W=()):
        self._deps("pool", R, W, True)
        ins = self.nc.gpsimd.collective_compute(
            "AllGather", ALU.bypass, replica_groups=GROUPS,
            ins=[in_t.ap().opt()], outs=[out_t.ap().opt()])
        self.cc_cnt += 1
        ins.then_inc(self.cc_sem)
        ev = (self.cc_sem, self.cc_cnt, "dma")
        self._record(ev, R, W)
        return ev

    def barrier(self):
        for en in self.ENG:
            for src in self.ENG:
                if self.cnt[src] > 0 and src != en:
                    self._wait(en, self.sems[src], self.cnt[src])
            for slot in self.ring:
                if slot[1] > 0:
                    self._wait(en, slot[0], slot[1])
            if self.cc_cnt:
                self._wait(en, self.cc_sem, self.cc_cnt)
        for en in self.ENG:
            if self.cnt[en] > 0 and en != "pe":
                self._wait(en, self.sems[en], self.cnt[en])

    def finish(self, out_bufs):
        self.barrier()


SCALE = 128.0 ** -0.5
TWO_PI = 2.0 * np.pi


def build_program():
    nc = bass.Bass("TRN2", target_bir_lowering=False)
    dbg = DEBUG or set()

    def din(name, shape, dt=F32):
        return nc.dram_tensor(name, list(shape), dt, kind="ExternalInput")

    dbg_pairs = []

    def dscr(name, shape, dt=F32):
        if name in dbg and name in ("projT", "small", "dnq", "ybuf"):
            t = nc.dram_tensor(name + "_i", list(shape), dt)
            o = nc.dram_tensor(name, list(shape), dt, kind="ExternalOutput")
            dbg_pairs.append((t, o))
            return t
        if name in dbg:
            return nc.dram_tensor(name, list(shape), dt, kind="ExternalOutput")
        return nc.dram_tensor(name, list(shape), dt)

    x_t = din("x", [S, D])
    xc_t = din("xcols", [S, 1024])
    c_t = din("c_pk", [128, NKC])
    wada_t = din("w_ada", [D, 3072])
    bada_t = din("b_ada", [1, 3072])
    ngain_t = din("ngain_pk", [128, NKC])
    gsel_t = din("gsel", [96, 8])
    win_t = din("w_in", [NCHUNK, 128, NKC * 128])
    wsm_t = din("w_small", [128, NKC * NSMALL])
    pos_t = din("pos", [1, S], I32)
    w1k_t = din("w1k", [D, 256])
    w1v_t = din("w1v", [D, 256])
    w2k_t = din("w2k", [256, 128])
    w2v_t = din("w2v", [256, 128])
    cposk_t = din("cposkT", [128, 32])
    cposv_t = din("cposvT", [128, 32])
    convw_t = din("convw", [128, 12 * 4])
    dnc_t = din("dnc", [64, 8 + 128])
    wpa_t = din("wpa", [8, 128, 16 * 128])
    wpb_t = din("wpb", [8, 128, 16 * 128])
    wo_t = din("wo", [8, 128, 32 * 128])
    fg_t = din("fgain", [128, 1024])
    ident_t = din("ident", [128, 128])
    c64_t = din("c64", [64, 256])
    c128_t = din("c128", [128, 256 + 128 + 1 + 128 + 128])
    rot_t = din("rotT", [32, 32])
    vmask_t = din("vmask", [32, 128, 64])
    fbias_t = din("fbias", [32, 128, 64])
    out_t = nc.dram_tensor("out", [S, 1024], F32, kind="ExternalOutput")

    modb_t = nc.dram_tensor("mod_b", [1, 3072], F32)
    modf_t = nc.dram_tensor("mod_f", [4, 3072], F32)
    projT_t = dscr("projT", [NCHUNK * 128, S])
    small_t = dscr("small", [S, NSMALL])
    dnq_t = dscr("dnq", [3 * 512, S])
    CW = min(1024, S)
    NCW = S // CW
    oa_b = [nc.dram_tensor("oa_b%d" % i, [512, CW], BF16) for i in range(NCW)]
    oa_f = [nc.dram_tensor("oa_f%d" % i, [2048, CW], BF16) for i in range(NCW)]
    ob_b = [nc.dram_tensor("ob_b%d" % i, [512, CW], BF16) for i in range(NCW)]
    ob_f = [nc.dram_tensor("ob_f%d" % i, [2048, CW], BF16) for i in range(NCW)]
    mg_b = [nc.dram_tensor("mg_b%d" % i, [1024, 512], BF16) for i in range(S // 512)]
    mg_f = [nc.dram_tensor("mg_f%d" % i, [4096, 512], BF16) for i in range(S // 512)]
    ybuf_t = dscr("ybuf", [S, 1024])
    ss_b = nc.dram_tensor("ss_b", [128, S // 128], F32)
    ss_f = nc.dram_tensor("ss_f", [512, S // 128], F32)
    oa_dbg = dscr("oa_dbg", [512, S]) if "oa_dbg" in dbg else None
    ob_dbg = dscr("ob_dbg", [512, S]) if "ob_dbg" in dbg else None
    qr_dbg = dscr("qr_dbg", [128, S]) if "qr_dbg" in dbg else None
    kc_dbg = dscr("kc_dbg", [128, 512]) if "kc_dbg" in dbg else None
    mg_dbg = dscr("mg_dbg", [1024, S]) if "mg_dbg" in dbg else None

    es = ExitStack()
    with es:
        k = K(nc, es)
        blk = es.enter_context(nc.Block())
        cur = [None]
        b_out = k.buf("out")

        def sb(name, shape, dt=F32):
            return cur[0].enter_context(nc.sbuf_tensor("sb_" + name, list(shape), dt))

        def ps(name, shape, dt=F32):
            return cur[0].enter_context(nc.psum_tensor("ps_" + name, list(shape), dt))

        def finish():
            k.barrier()
            for t, o in dbg_pairs:
                n = t.ap().shape[0]
                step = 128 if n % 128 == 0 else n
                for r0 in range(0, n, step):
                    k.dma("sp", o.ap()[r0:r0 + step, :], t.ap()[r0:r0 + step, :])
            k.barrier()

        stg = {}

        def stg_alloc(n=2, size=4096):
            stg["t"] = [sb("stg%d_%d" % (k.nbuf, i), [128, size]) for i in range(n)]
            stg["b"] = k.bufs(n, "stg")
            stg["i"] = 0

        def cast_load(dst, src, W, inner=None):
            i = stg["i"] % len(stg["t"])
            stg["i"] += 1
            n = 1
            for d_ in dst.shape[1:]:
                n *= d_
            sv = stg["t"][i][:, 0:n]
            if inner is not None:
                sv = sv.rearrange("p (a b) -> p a b", b=inner)
            k.dma("sp", sv, src, W=[stg["b"][i]])
            ce = ("dve", "act", "dve", "act", "dve", "act", "pool")[stg["i"] % 7]
            cp(dst, sv, [stg["b"][i]], W, eng=ce)

        def mm(out, lhsT, rhs, R, W, start=True, stop=True):
            return k.op("pe", "matmul", R=R, W=W, out=out, lhsT=lhsT, rhs=rhs, start=start, stop=stop)

        def tr(out, in_, idn, R, W):
            return k.op("pe", "transpose", R=R, W=W, out=out, in_=in_, identity=idn)

        def act(out, in_, func, R, W, **kw):
            return k.op("act", "activation", R=R, W=W, out=out, in_=in_, func=func, **kw)

        def ts(out, in0, s1, s2, op0, op1, R, W, eng="dve", **kw):
            if op1 is None:
                return k.op(eng, "tensor_scalar", R=R, W=W, out=out, in0=in0, scalar1=s1, scalar2=None, op0=op0, **kw)
            return k.op(eng, "tensor_scalar", R=R, W=W, out=out, in0=in0, scalar1=s1, scalar2=s2, op0=op0, op1=op1, **kw)

        def tt(out, in0, in1, op, R, W, eng="dve"):
            return k.op(eng, "tensor_tensor", R=R, W=W, out=out, in0=in0, in1=in1, op=op)

        def stt(out, in0, scalar, in1, op0, op1, R, W, **kw):
            return k.op("dve", "scalar_tensor_tensor", R=R, W=W, out=out, in0=in0, scalar=scalar, in1=in1,
                        op0=op0, op1=op1, **kw)

        def cp(out, in_, R, W, eng="dve"):
            if eng == "act":
                return k.op("act", "activation", R=R, W=W, out=out, in_=in_, func=AF.Copy)
            return k.op(eng, "tensor_copy", R=R, W=W, out=out, in_=in_)

        @blk.sync
        def _(sync_eng):
            es_c = ExitStack()
            cur[0] = es_c
            ident = sb("ident", [128, 128])
            identb = sb("identb", [128, 128], BF16)
            ones = sb("ones", [128, 128])
            c64 = sb("c64", [64, 256])
            c128 = sb("c128", [128, 641])
            tri_b = sb("tri_b", [128, 128], BF16)
            triw_b = sb("triw_b", [128, 128], BF16)
            ones_b = sb("ones_b", [128, 512], BF16)
            rotT = sb("rotT", [32, 32])
            a_sb = sb("a_sb", [128, NKC])
            shift_sb = sb("shift_sb", [128, NKC])
            gate_sb = sb("gate_sb", [128, 8])
            b_cst = k.buf("cst")
            k.dma("sp", ident[:], ident_t.ap()[:, :], W=[b_cst])
            k.dma("sp", c64[:], c64_t.ap()[:, :], W=[b_cst])
            k.dma("sp", c128[:], c128_t.ap()[:, :], W=[b_cst])
            k.dma("sp", rotT[:], rot_t.ap()[:, :], W=[b_cst])
            cp(identb[:], ident[:], [b_cst], [b_cst])
            k.op("dve", "memset", W=[b_cst], ap=ones[:], constant=1.0)
            k.op("dve", "memset", W=[b_cst], ap=ones_b[:], constant=1.0)
            cp(tri_b[:], c128[:, 385:513], [b_cst], [b_cst])
            cp(triw_b[:], c128[:, 513:641], [b_cst], [b_cst])
            tri_le = c64[:, 0:64]
            lowst = c64[:, 64:128]
            lowd = c64[:, 128:192]
            lowo = c64[:, 192:256]
            cval = c128[:, 0:256]
            ovl = c128[:, 256:384].rearrange("p (a b) -> p a b", b=64)
            invf = c128[:, 384:385]
            b_mod = k.buf("modvecs")
            k.barrier()

            es_p = ExitStack()
            cur[0] = es_p
            c_sb = sb("c_sb", [128, NKC])
            bada_sb = sb("bada_sb", [1, 3072])
            mod_sb = sb("mod_sb", [1, 3072])
            wa = [sb("wa%d" % i, [128, NKC, 256]) for i in range(2)]
            b_wa = k.bufs(2, "wa")
            pm = [ps("pm%d" % i, [128, 512]) for i in range(2)]
            b_pm = k.bufs(2, "pm")
            b_c = k.buf("c")
            b_modsb = k.buf("modsb")
            k.dma("sp", c_sb[:], c_t.ap()[:, :], W=[b_c])
            k.dma("sp", bada_sb[:], bada_t.ap()[:, :], W=[b_c])
            wada_v = wada_t.ap().rearrange("(kc p) n -> p kc n", p=128)
            for ct in range(12):
                i = ct % 2
                k.dma("sp", wa[i][:], wada_v[:, :, ct * 256:(ct + 1) * 256], W=[b_wa[i]])
                for kc in range(NKC):
                    mm(pm[i][0:1, 0:256], c_sb[:, kc:kc + 1], wa[i][:, kc, :], [b_c, b_wa[i]], [b_pm[i]],
                       start=(kc == 0), stop=(kc == NKC - 1))
                tt(mod_sb[0:1, ct * 256:(ct + 1) * 256], pm[i][0:1, 0:256],
                   bada_sb[0:1, ct * 256:(ct + 1) * 256], ALU.add, [b_pm[i], b_c], [b_modsb])
            b_modb = k.buf("modb")
            b_modf = k.buf("modf")
            k.dma("sp", modb_t.ap()[:, :], mod_sb[:], R=[b_modsb], W=[b_modb])
            k.allgather(modb_t, modf_t, R=[b_modb], W=[b_modf])
            rows = sb("modrows", [96, 128])
            b_rows = k.buf("rows")
            modf_rows = modf_t.ap().rearrange("r (a p) -> (r a) p", p=128)
            k.dma("sp", rows[:], modf_rows, R=[b_modf], W=[b_rows])
            tr(pm[0][:, 0:96], rows[:], ident[0:96, 0:96], [b_rows], [b_pm[0]])
            ng_sb = sb("ng_sb", [128, NKC])
            k.dma("sp", ng_sb[:], ngain_t.ap()[:, :], W=[b_c])
            cp(shift_sb[:], pm[0][:, 0:32], [b_pm[0]], [b_mod])
            stt(a_sb[:], pm[0][:, 32:64], 1.0, ng_sb[:], ALU.add, ALU.mult, [b_pm[0], b_c], [b_mod])
            gsel = sb("gsel", [96, 8])
            k.dma("sp", gsel[:], gsel_t.ap()[:, :], W=[b_c])
            mm(pm[1][:, 0:8], rows[:], gsel[:], [b_rows, b_c], [b_pm[1]])
            cp(gate_sb[:], pm[1][:, 0:8], [b_pm[1]], [b_mod])
            k.barrier()
            es_p.close()

            es_p = ExitStack()
            cur[0] = es_p
            TT = 512
            xs = [sb("xs%d" % i, [128, D]) for i in range(4)]
            b_xs = k.bufs(4, "xs")
            junk = sb("junk", [128, D], BF16)
            b_junk = k.buf("junk")
            hT = sb("hT", [128, NKC, TT], BF16)
            b_hT = k.buf("hT")
            wb = [sb("wb%d" % i, [128, NKC, 128], BF16) for i in range(3)]
            b_wb = k.bufs(3, "wb")
            ob = [sb("ob%d" % i, [128, TT]) for i in range(2)]
            b_ob = k.bufs(2, "ob")
            wsm = sb("wsm", [128, NKC, NSMALL], BF16)
            b_wsm = k.buf("wsm")
            osm = sb("osm", [128, 4, NSMALL])
            b_osm = k.buf("osm")
            st = sb("stats", [128, 8])
            b_st = k.buf("st")
            ptr = [ps("ptr%d" % i, [128, 512]) for i in range(2)]
            b_ptr = k.bufs(2, "ptr")
            pp = [ps("pp%d" % i, [128, 512]) for i in range(2)]
            b_pp = k.bufs(2, "pp")
            psm = ps("psm", [128, 512])
            b_psm = k.buf("psm")
            b_projT = k.buf("projT")
            b_small = k.buf("small")
            stg_alloc(2)
            cast_load(wsm[:].rearrange("p a b -> p (a b)"), wsm_t.ap()[:, :], [b_wsm])
            n_tt = S // TT

            def chunk_func(ch):
                if 14 <= ch < 18 or 30 <= ch < 34:
                    return AF.Silu
                if ch >= 34:
                    return AF.Sigmoid
                return AF.Identity
            epi = 0
            for tt_i in range(n_tt):
                t0 = tt_i * TT
                for sub in range(4):
                    k.dma("sp", xs[sub][:], x_t.ap()[t0 + sub * 128:t0 + (sub + 1) * 128, :], W=[b_xs[sub]])
                    act(junk[:], xs[sub][:], AF.Square, [b_xs[sub]], [b_junk, b_st], accum_out=st[:, sub:sub + 1])
                ts(st[:, 0:4], st[:, 0:4], 1.0 / D, EPS, ALU.mult, ALU.add, [b_st], [b_st])
                act(st[:, 0:4], st[:, 0:4], AF.Sqrt, [b_st], [b_st])
                k.op("dve", "reciprocal", R=[b_st], W=[b_st], out=st[:, 4:8], in_=st[:, 0:4])
                for sub in range(4):
                    ts(xs[sub][:], xs[sub][:], st[:, 4 + sub:5 + sub], None, ALU.mult, None,
                       [b_xs[sub], b_st], [b_xs[sub]])
                for kc in range(NKC):
                    i = kc % 2
                    for sub in range(4):
                        tr(ptr[i][:, sub * 128:(sub + 1) * 128], xs[sub][:, kc * 128:(kc + 1) * 128], ident[:],
                           [b_xs[sub]], [b_ptr[i]])
                    act(hT[:, kc, :], ptr[i][:], AF.Identity, [b_ptr[i]], [b_hT],
                        scale=a_sb[:, kc:kc + 1], bias=shift_sb[:, kc:kc + 1])
                for sub in range(4):
                    for kc in range(NKC):
                        mm(psm[:, sub * 32:sub * 32 + NSMALL], hT[:, kc, sub * 128:(sub + 1) * 128], wsm[:, kc, :],
                           [b_hT, b_wsm], [b_psm], start=(kc == 0), stop=(kc == NKC - 1))
                cp(osm[:], psm[:, 0:128].rearrange("p (a b) -> p a b", b=32)[:, :, 0:NSMALL], [b_psm], [b_osm])
                k.dma("act", small_t.ap()[t0:t0 + TT, :].rearrange("(a p) n -> p a n", p=128), osm[:],
                      R=[b_osm], W=[])
                for ch in range(NCHUNK):
                    wi = epi % 3
                    pi = epi % 2
                    epi += 1
                    cast_load(wb[wi][:].rearrange("p a b -> p (a b)"), win_t.ap()[ch, :, :], [b_wb[wi]])
                    for kc in range(NKC):
                        mm(pp[pi][:], wb[wi][:, kc, :], hT[:, kc, :], [b_hT, b_wb[wi]], [b_pp[pi]],
                           start=(kc == 0), stop=(kc == NKC - 1))
                    act(ob[pi][:], pp[pi][:], chunk_func(ch), [b_pp[pi]], [b_ob[pi]])
                    k.dma("act", projT_t.ap()[ch * 128:(ch + 1) * 128, t0:t0 + TT], ob[pi][:],
                          R=[b_ob[pi]], W=[])
            k.barrier()
            es_p.close()
            if STOP_AFTER == "proj":
                finish()
                return

            es_n = ExitStack()
            cur[0] = es_n
            qT = sb("qT", [128, 8, S], BF16)
            kTs = sb("kTs", [128, S], BF16)
            kTw = sb("kTw", [128, S], BF16)
            Vs = sb("Vs", [128, S // 128, 128], BF16)
            Vw = sb("Vw", [128, S // 128, 128], BF16)
            kcmpT = sb("kcmpT", [128, 256], BF16)
            vcmp = sb("vcmp", [128, 2, 128], BF16)
            es_q = ExitStack()
            cur[0] = es_q
            kcT = sb("kcT", [128, S], BF16)
            vcT = sb("vcT", [128, S], BF16)
            cosT = sb("cosT", [32, S])
            sinT = sb("sinT", [32, S])
            HS = S // 2
            es_r = ExitStack()
            cur[0] = es_r
            posi = sb("posi", [32, HS], I32)
            ang = sb("ang", [32, HS])
            rtmp = sb("rtmp", [32, HS])
            rtmp2 = sb("rtmp2", [32, HS])
            rtmpi = sb("rtmpi", [32, HS], I32)
            b_r = k.buf("rope")
            RW = dict(R=[b_r], W=[b_r])
            for hf in range(2):
                hs = slice(hf * HS, (hf + 1) * HS)
                k.dma("sp", posi[:], pos_t.ap()[0:1, hs].to_broadcast([32, HS]), W=[b_r])
                cp(ang[:], posi[:], [b_r], [b_r])
                ts(ang[:], ang[:], invf[0:32, :], None, ALU.mult, None, [b_r], [b_r])
                for tab, off in ((sinT, 0.0), (cosT, float(np.pi / 2))):
                    ts(rtmp[:], ang[:], 1.0 / TWO_PI, off / TWO_PI, ALU.mult, ALU.add, [b_r], [b_r])
                    cp(rtmpi[:], rtmp[:], [b_r], [b_r])
                    cp(rtmp[:], rtmpi[:], [b_r], [b_r])
                    stt(rtmp[:], rtmp[:], -TWO_PI, ang[:], ALU.mult, ALU.add, [b_r], [b_r])
                    if off:
                        ts(rtmp[:], rtmp[:], off, None, ALU.add, None, [b_r], [b_r])
                    ts(rtmp2[:], rtmp[:], float(np.pi), -TWO_PI, ALU.is_gt, ALU.mult, [b_r], [b_r])
                    tt(rtmp[:], rtmp[:], rtmp2[:], ALU.add, [b_r], [b_r])
                    ts(rtmp2[:], rtmp[:], -float(np.pi), TWO_PI, ALU.is_lt, ALU.mult, [b_r], [b_r])
                    tt(rtmp[:], rtmp[:], rtmp2[:], ALU.add, [b_r], [b_r])
                    ts(rtmp[:], rtmp[:], float(np.pi), -float(np.pi), ALU.min, ALU.max, [b_r], [b_r])
                    act(tab[:, hs], rtmp[:], AF.Sin, [b_r], [b_r])
            k.barrier()
            es_r.close()
            cur[0] = es_q
            lb = [sb("lb%d" % i, [128, 512]) for i in range(3)]
            b_lb = k.bufs(3, "lb")
            rt1 = [sb("rt1_%d" % i, [32, 512]) for i in range(2)]
            rt2 = [sb("rt2_%d" % i, [32, 512]) for i in range(2)]
            b_rt = k.bufs(2, "rt")
            pr = [ps("pr%d" % i, [128, 512]) for i in range(2)]
            b_pr = k.bufs(2, "pr")
            b_dst = k.buf("nsadst")
            b_qdbg = k.buf("qdbg")
            li = 0
            plan = [(h, "rope", qT[:, h, :]) for h in range(8)]
            plan += [(8, "cast", kcT[:]), (9, "cast", vcT[:]), (10, "rope", kTs[:]), (11, "vT", Vs),
                     (12, "rope", kTw[:]), (13, "vT", Vw)]
            for ch, kind, dst in plan:
                for tt_i in range(S // 512):
                    t0 = tt_i * 512
                    a = li % 3
                    r = li % 2
                    li += 1
                    k.dma("sp", lb[a][:], projT_t.ap()[ch * 128:(ch + 1) * 128, t0:t0 + 512], W=[b_lb[a]])
                    if kind == "cast":
                        cp(dst[:, t0:t0 + 512], lb[a][:], [b_lb[a]], [b_dst], eng="act")
                    elif kind == "rope":
                        mm(pr[r][0:32, :], rotT[:, :], lb[a][0:32, :], [b_lb[a]], [b_pr[r]])
                        cp(dst[:, t0:t0 + 512], lb[a][:], [b_lb[a]], [b_dst], eng="act")
                        tt(rt1[r][:], lb[a][0:32, :], cosT[:, t0:t0 + 512], ALU.mult, [b_lb[a]], [b_rt[r]])
                        tt(rt2[r][:], pr[r][0:32, :], sinT[:, t0:t0 + 512], ALU.mult, [b_pr[r]], [b_rt[r]])
                        tt(dst[0:32, t0:t0 + 512], rt1[r][:], rt2[r][:], ALU.add, [b_rt[r]], [b_dst])
                    else:
                        for sub in range(4):
                            tr(pr[r][:, sub * 128:(sub + 1) * 128], lb[a][:, sub * 128:(sub + 1) * 128], ident[:],
                               [b_lb[a]], [b_pr[r]])
                        cp(dst[:, tt_i * 4:(tt_i + 1) * 4, :], pr[r][:].rearrange("p (a b) -> p a b", b=128),
                           [b_pr[r]], [b_dst])
            if qr_dbg is not None:
                qf = sb("qf_dbg", [128, S])
                cp(qf[:], qT[:, 0, :], [b_dst], [b_qdbg])
                k.dma("sp", qr_dbg.ap()[:, :], qf[:], R=[b_qdbg], W=[])
            k.barrier()

            NCMP = (S - 32) // 16 + 1
            stg_alloc(1, 2048)
            w1 = sb("w1", [128, 32, 256], BF16)
            w2 = sb("w2", [128, 2, 128], BF16)
            posT = sb("posT", [128, 32])
            posTb = sb("posTb", [128, 32], BF16)
            hid = sb("hid", [128, 2, 256], BF16)
            pbs = sb("pbs", [128, 2])
            kf = sb("kf", [128, 256])
            b_w = k.buf("cmpw")
            b_hid = k.buf("hid")
            b_kf = k.buf("kf")
            b_cmp = k.buf("cmpout")
            ph = pr[0]
            pb_ = pr[1]
            b_ph, b_pb = b_pr
            k.op("dve", "memset", W=[b_hid], ap=hid[:], constant=0.0)
            k.op("dve", "memset", W=[b_cmp], ap=kcmpT[:], constant=0.0)
            for which in range(2):
                w1_t = (w1k_t, w1v_t)[which]
                w2_t = (w2k_t, w2v_t)[which]
                cps_t = (cposk_t, cposv_t)[which]
                src = (kcT, vcT)[which]
                srcv = src[:].rearrange("p (n s) -> p n s", s=16)
                w1v = w1_t.ap().rearrange("(l d) h -> d l h", d=128)
                for a in range(4):
                    cast_load(w1[:, 8 * a:8 * a + 8, :], w1v[:, 8 * a:8 * a + 8, :], [b_w], inner=256)
                cast_load(w2[:], w2_t.ap().rearrange("(hc p) d -> p hc d", p=128), [b_w], inner=128)
                k.dma("sp", posT[:], cps_t.ap()[:, :], W=[b_w])
                cp(posTb[:], posT[:], [b_w], [b_w])
                for hc in range(2):
                    for l in range(32):
                        mm(pb_[:, 0:1], w1[:, l, hc * 128:(hc + 1) * 128], posTb[:, l:l + 1], [b_w], [b_pb],
                           start=(l == 0), stop=(l == 31))
                    cp(pbs[:, hc:hc + 1], pb_[:, 0:1], [b_pb], [b_hid])
                    for l in range(32):
                        rhs = srcv[:, 0:NCMP, l] if l < 16 else srcv[:, 1:NCMP + 1, l - 16]
                        mm(ph[:, 0:NCMP], w1[:, l, hc * 128:(hc + 1) * 128], rhs, [b_w], [b_ph],
                           start=(l == 0), stop=(l == 31))
                    act(hid[:, hc, 0:NCMP], ph[:, 0:NCMP], AF.Silu, [b_ph, b_hid], [b_hid], bias=pbs[:, hc:hc + 1])
                if which == 0:
                    for hc in range(2):
                        mm(ph[:, 0:NCMP], w2[:, hc, :], hid[:, hc, 0:NCMP], [b_w, b_hid], [b_ph],
                           start=(hc == 0), stop=(hc == 1))
                    cp(kf[:, 0:NCMP], ph[:, 0:NCMP], [b_ph], [b_kf])
                    mm(pb_[0:32, 0:NCMP], rotT[:, :], kf[0:32, 0:NCMP], [b_kf], [b_pb])
                    cosv = cosT[:].rearrange("p (n s) -> p n s", s=16)[:, 1:NCMP + 1, 15]
                    sinv = sinT[:].rearrange("p (n s) -> p n s", s=16)[:, 1:NCMP + 1, 15]
                    cp(kcmpT[:, 0:NCMP], kf[:, 0:NCMP], [b_kf], [b_cmp], eng="act")
                    tt(rt1[0][:, 0:NCMP], kf[0:32, 0:NCMP], cosv, ALU.mult, [b_kf], [b_rt[0]])
                    tt(rt2[0][:, 0:NCMP], pb_[0:32, 0:NCMP], sinv, ALU.mult, [b_pb], [b_rt[0]])
                    tt(kcmpT[0:32, 0:NCMP], rt1[0][:, 0:NCMP], rt2[0][:, 0:NCMP], ALU.add, [b_rt[0]], [b_cmp])
                    if kc_dbg is not None:
                        kfd = sb("kfd", [128, 512])
                        k.op("dve", "memset", W=[b_kf], ap=kfd[:], constant=0.0)
                        cp(kfd[:, 0:256], kcmpT[:], [b_cmp], [b_kf])
                else:
                    for nt in range(2):
                        for hc in range(2):
                            mm(ph[:, 0:128], hid[:, hc, nt * 128:(nt + 1) * 128], w2[:, hc, :], [b_w, b_hid], [b_ph],
                               start=(hc == 0), stop=(hc == 1))
                        cp(vcmp[:, nt, :], ph[:, 0:128], [b_ph], [b_cmp])
                        if kc_dbg is not None:
                            cp(kfd[:, 256 + nt * 128:256 + (nt + 1) * 128], ph[:, 0:128], [b_ph], [b_kf])
            if kc_dbg is not None:
                k.dma("sp", kc_dbg.ap()[:, :], kfd[:], R=[b_kf], W=[])
            k.barrier()
            es_q.close()

            es_a = ExitStack()
            cur[0] = es_a
            pS = [ps("pS%d" % i, [128, 512]) for i in range(2)]
            b_pS = k.bufs(2, "pS")
            pT = [ps("pT%d" % i, [128, 512], BF16) for i in range(2)]
            b_pT = k.bufs(2, "pT")
            pO = [ps("pO%d" % i, [128, 512]) for i in range(2)]
            b_pO = k.bufs(2, "pO")
            pI = ps("pI", [128, 512])
            b_pI = k.buf("pI")
            pX = ps("pX", [128, 512])
            b_pX = k.buf("pX")
            Pf = [sb("Pf%d" % i, [128, 512], BF16) for i in range(2)]
            b_Pf = k.bufs(2, "Pf")
            PTs = [sb("PTs%d" % i, [128, 512], BF16) for i in range(2)]
            b_PTs = k.bufs(2, "PTs")
            acc = sb("acc", [128, 256])
            b_acc = k.buf("acc")
            accT = sb("accT", [128, 2, 128])
            b_accT = k.buf("accT")
            cm = sb("cm", [128, 256], BF16)
            b_cm = k.buf("cm")
            vm = sb("vm", [128, 64])
            fb = sb("fb", [128, 64])
            b_vf = k.buf("vf")
            score = sb("score", [128, 64])
            sc2 = sb("sc2", [128, 64])
            m8 = sb("m8", [128, 8])
            m8b = sb("m8b", [128, 8])
            sel = sb("sel", [128, 64], BF16)
            b_sel = k.buf("sel")
            gts = sb("gts", [128, 12])
            b_gts = k.buf("gts")
            oacc = [sb("oacc%d" % i, [128, 128]) for i in range(4)]
            b_oacc = k.bufs(4, "oacc")
            zt = sb("zt", [128, 4, 128])
            b_zt = k.buf("zt")
            oT = sb("oT", [128, 4, 128], BF16)
            b_oT = k.buf("oT")
            oTf = sb("oTf", [128, 4, 128]) if oa_dbg is not None else None
            stt_ = sb("stt", [128, 256])
            b_stc = k.bufs(256, "stc")
            stc_i = [0]
            mxt = [sb("mxt%d" % i, [128, 16]) for i in range(4)]
            b_mxt = k.bufs(4, "mxt")
            mxt_i = [0]
            b_oab = k.buf("oab")
            tog = {"s": 0, "o": 0}

            def stat():
                c = stc_i[0] % 256
                stc_i[0] += 1
                return stt_[:, c:c + 1], b_stc[c]

            def finish_branch(h, o, rs_ap, b_rs, gcol, first):
                ri, b_ri = stat()
                ts(ri, rs_ap, 1e-30, None, ALU.max, None, [b_rs], [b_ri])
                k.op("dve", "reciprocal", R=[b_ri], W=[b_ri], out=ri, in_=ri)
                cf, b_cf = stat()
                tt(cf, ri, gts[:, gcol:gcol + 1], ALU.mult, [b_ri, b_gts], [b_cf])
                if first:
                    ts(oacc[h][:], pO[o][:, 0:128], cf, None, ALU.mult, None, [b_pO[o], b_cf], [b_oacc[h]])
                else:
                    stt(oacc[h][:], pO[o][:, 0:128], cf, oacc[h][:], ALU.mult, ALU.add,
                        [b_pO[o], b_cf, b_oacc[h]], [b_oacc[h]])
                return ri, b_ri

            def branch(qi, h, blocks, kT_, V_, use_sel, wfirst, gcol):
                t0 = qi * 128
                tiles = [blocks[a:a + 4] for a in range(0, len(blocks), 4)]
                mi = mxt_i[0] % 4
                mxt_i[0] += 1
                for ti, tl in enumerate(tiles):
                    w = 128 * len(tl)
                    k0 = tl[0] * 128
                    s = tog["s"]
                    tog["s"] ^= 1
                    mm(pS[s][:, 0:w], qT[:, h, t0:t0 + 128], kT_[:, k0:k0 + w], [], [b_pS[s]])
                    k.op("dve", "reduce_max", R=[b_pS[s]], W=[b_mxt[mi]], out=mxt[mi][:, ti:ti + 1],
                         in_=pS[s][:, 0:w], axis=AX.X)
                nm, b_nm = stat()
                k.op("dve", "reduce_max", R=[b_mxt[mi]], W=[b_nm], out=nm, in_=mxt[mi][:, 0:len(tiles)], axis=AX.X)
                ts(nm, nm, -SCALE, None, ALU.mult, None, [b_nm], [b_nm])
                o = tog["o"]
                tog["o"] ^= 1
                nblk = len(blocks)
                bdone = 0
                for ti, tl in enumerate(tiles):
                    w = 128 * len(tl)
                    k0 = tl[0] * 128
                    s = tog["s"]
                    tog["s"] ^= 1
                    mm(pS[s][:, 0:w], qT[:, h, t0:t0 + 128], kT_[:, k0:k0 + w], [], [b_pS[s]])
                    act(Pf[s][:, 0:w], pS[s][:, 0:w], AF.Exp, [b_pS[s], b_nm], [b_Pf[s]], scale=SCALE, bias=nm)
                    for bi, blk_ in enumerate(tl):
                        sl = slice(bi * 128, (bi + 1) * 128)
                        if blk_ == qi:
                            tt(Pf[s][:, sl], Pf[s][:, sl], tri_b[:], ALU.mult, [b_Pf[s]], [b_Pf[s]])
                        elif wfirst is not None and blk_ == wfirst:
                            tt(Pf[s][:, sl], Pf[s][:, sl], triw_b[:], ALU.mult, [b_Pf[s]], [b_Pf[s]])
                    nb2 = 2 * len(tl)
                    if use_sel:
                        in1 = sel[:, 2 * tl[0]:2 * tl[0] + nb2].unsqueeze(2).to_broadcast([128, nb2, 64])
                        pv_ = Pf[s][:, 0:w].rearrange("p (a b) -> p a b", b=64)
                        stt(pv_, pv_, 1.0, in1, ALU.mult, ALU.mult, [b_Pf[s], b_sel], [b_Pf[s], b_mxt[mi]],
                            accum_out=mxt[mi][:, 8 + ti:9 + ti])
                    else:
                        stt(Pf[s][:, 0:w], Pf[s][:, 0:w], 1.0, ones_b[:, 0:w], ALU.mult, ALU.mult,
                            [b_Pf[s]], [b_Pf[s], b_mxt[mi]], accum_out=mxt[mi][:, 8 + ti:9 + ti])
                    for bi in range(len(tl)):
                        sl = slice(bi * 128, (bi + 1) * 128)
                        tr(pT[s][:, sl], Pf[s][:, sl], identb[:], [b_Pf[s]], [b_pT[s]])
                    cp(PTs[s][:, 0:w], pT[s][:, 0:w], [b_pT[s]], [b_PTs[s]], eng="act")
                    for bi, blk_ in enumerate(tl):
                        sl = slice(bi * 128, (bi + 1) * 128)
                        mm(pO[o][:, 0:128], PTs[s][:, sl], V_[:, blk_, :], [b_PTs[s]], [b_pO[o]],
                           start=(bdone == 0), stop=(bdone == nblk - 1))
                        bdone += 1
                rs, b_rs = stat()
                k.op("dve", "reduce_sum", R=[b_mxt[mi]], W=[b_rs], out=rs, in_=mxt[mi][:, 8:8 + len(tiles)], axis=AX.X)
                finish_branch(h, o, rs, b_rs, gcol, False)

            for qi in range(S // 128):
                t0 = qi * 128
                k.dma("sp", gts[:], small_t.ap()[t0:t0 + 128, 0:12], W=[b_gts])
                act(gts[:], gts[:], AF.Sigmoid, [b_gts], [b_gts])
                k.dma("sp", vm[:], vmask_t.ap()[qi, :, :], W=[b_vf])
                k.dma("sp", fb[:], fbias_t.ap()[qi, :, :], W=[b_vf])
                k.dma("sp", zt[:], projT_t.ap()[14 * 128:18 * 128, t0:t0 + 128].rearrange("(h p) t -> p h t", p=128),
                      W=[b_zt])
                ts(cm[:], cval, float(128 * qi), None, ALU.is_le, None, [], [b_cm])
                for h in range(8):
                    s = tog["s"]
                    tog["s"] ^= 1
                    mm(pS[s][:, 0:256], qT[:, h, t0:t0 + 128], kcmpT[:, :], [], [b_pS[s]])
                    nm, b_nm = stat()
                    k.op("dve", "reduce_max", R=[b_pS[s]], W=[b_nm], out=nm, in_=pS[s][:, 0:256], axis=AX.X)
                    ts(nm, nm, -SCALE, None, ALU.mult, None, [b_nm], [b_nm])
                    act(Pf[s][:, 0:256], pS[s][:, 0:256], AF.Exp, [b_pS[s], b_nm], [b_Pf[s]], scale=SCALE, bias=nm)
                    rs, b_rs = stat()
                    stt(Pf[s][:, 0:256], Pf[s][:, 0:256], 1.0, cm[:], ALU.mult, ALU.mult, [b_Pf[s], b_cm],
                        [b_Pf[s], b_rs], accum_out=rs)
                    if h < 4:
                        for ct in range(2):
                            sl = slice(ct * 128, (ct + 1) * 128)
                            tr(pT[s][:, sl], Pf[s][:, sl], identb[:], [b_Pf[s]], [b_pT[s]])
                        cp(PTs[s][:, 0:256], pT[s][:, 0:256], [b_pT[s]], [b_PTs[s]], eng="act")
                        o = tog["o"]
                        tog["o"] ^= 1
                        for ct in range(2):
                            sl = slice(ct * 128, (ct + 1) * 128)
                            mm(pO[o][:, 0:128], PTs[s][:, sl], vcmp[:, ct, :], [b_PTs[s]], [b_pO[o]],
                               start=(ct == 0), stop=(ct == 1))
                        ri, b_ri = finish_branch(h, o, rs, b_rs, h, True)
                    else:
                        ri, b_ri = stat()
                        ts(ri, rs, 1e-30, None, ALU.max, None, [b_rs], [b_ri])
                        k.op("dve", "reciprocal", R=[b_ri], W=[b_ri], out=ri, in_=ri)
                    if h == 0:
                        ts(acc[:], Pf[s][:, 0:256], ri, None, ALU.mult, None, [b_Pf[s], b_ri], [b_acc])
                    else:
                        stt(acc[:], Pf[s][:, 0:256], ri, acc[:], ALU.mult, ALU.add, [b_Pf[s], b_ri, b_acc], [b_acc])
                for ct in range(2):
                    sl = slice(ct * 128, (ct + 1) * 128)
                    tr(pX[:, sl], acc[:, sl], ident[:], [b_acc], [b_pX])
                cp(accT[:], pX[:, 0:256].rearrange("p (a b) -> p a b", b=128), [b_pX], [b_accT])
                for ct in range(2):
                    mm(pI[:, 0:64], accT[:, ct, :], ovl[:, ct, :], [b_accT], [b_pI], start=(ct == 0), stop=(ct == 1))
                tt(score[:], pI[:, 0:64], vm[:], ALU.mult, [b_pI, b_vf], [b_sel])
                tt(score[:], score[:], fb[:], ALU.add, [b_sel, b_vf], [b_sel])
                k.op("dve", "max", R=[b_sel], W=[b_sel], out=m8[:], in_=score[:])
                k.op("dve", "match_replace", R=[b_sel], W=[b_sel], out=sc2[:], in_to_replace=m8[:],
                     in_values=score[:], imm_value=-2.0)
                k.op("dve", "max", R=[b_sel], W=[b_sel], out=m8b[:], in_=sc2[:])
                ts(sel[:], score[:], m8b[:, 7:8], None, ALU.is_ge, None, [b_sel], [b_sel])
                for h in range(4):
                    branch(qi, h, list(range(0, qi + 1)), kTs, Vs, True, None, 4 + h)
                    branch(qi, h, list(range(max(0, qi - 4), qi + 1)), kTw, Vw, False,
                           (qi - 4) if qi >= 4 else None, 8 + h)
                for h in range(4):
                    tr(pX[:, h * 128:(h + 1) * 128], oacc[h][:], ident[:], [b_oacc[h]], [b_pX])
                tt(oT[:], pX[:].rearrange("p (a b) -> p a b", b=128), zt[:], ALU.mult, [b_pX, b_zt], [b_oT])
                k.dma("sp", oa_b[t0 // CW].ap().rearrange("(h p) t -> p h t", p=128)[:, :, t0 % CW:t0 % CW + 128], oT[:],
                      R=[b_oT], W=[])
                if oa_dbg is not None:
                    tt(oTf[:], pX[:].rearrange("p (a b) -> p a b", b=128), zt[:], ALU.mult, [b_pX, b_zt], [b_oT])
                    k.dma("sp", oa_dbg.ap().rearrange("(h p) t -> p h t", p=128)[:, :, t0:t0 + 128], oTf[:],
                          R=[b_oT], W=[])
            k.barrier()
            es_a.close()
            es_n.close()
            if STOP_AFTER == "nsa":
                finish()
                return

            es_d = ExitStack()
            cur[0] = es_d
            cw = sb("cw", [128, 12, 4])
            dnc = sb("dnc", [64, 136])
            b_dc = k.buf("dnconst")
            k.dma("sp", cw[:].rearrange("p a b -> p (a b)"), convw_t.ap()[:, :], W=[b_dc])
            k.dma("sp", dnc[:], dnc_t.ap()[:, :], W=[b_dc])
            g_sb = sb("g_sb", [64, S // 64, 4])
            beta_sb = sb("beta_sb", [64, S // 64, 4])
            gb_raw = sb("gb_raw", [64, S // 64, 8])
            nea = sb("nea", [64, 4])
            es_dp = ExitStack()
            cur[0] = es_dp
            xb = [sb("xb%d" % i, [128, 515]) for i in range(2)]
            b_xb = k.bufs(2, "xb")
            yb = [sb("yb%d" % i, [128, 512]) for i in range(2)]
            b_yb = k.bufs(2, "yb")
            sq = sb("sq", [128, 512])
            rn = sb("rn", [128, 512])
            b_sq = k.buf("sq")
            pss = ps("pss", [128, 512])
            b_pss = k.buf("pss")
            b_dnq = k.buf("dnq")
            li = 0
            for ci in range(12):
                ch = 18 + ci
                for tt_i in range(S // 512):
                    t0 = tt_i * 512
                    a = li % 2
                    li += 1
                    if tt_i == 0:
                        k.op("dve", "memset", W=[b_xb[a]], ap=xb[a][:, 0:3], constant=0.0)
                        k.dma("sp", xb[a][:, 3:515], projT_t.ap()[ch * 128:(ch + 1) * 128, 0:512], W=[b_xb[a]])
                    else:
                        k.dma("sp", xb[a][:, 0:515], projT_t.ap()[ch * 128:(ch + 1) * 128, t0 - 3:t0 + 512],
                              W=[b_xb[a]])
                    ts(yb[a][:], xb[a][:, 3:515], cw[:, ci, 3:4], None, ALU.mult, None, [b_xb[a], b_dc], [b_yb[a]])
                    for i in (2, 1, 0):
                        stt(yb[a][:], xb[a][:, i:i + 512], cw[:, ci, i:i + 1], yb[a][:], ALU.mult, ALU.add,
                            [b_xb[a], b_dc, b_yb[a]], [b_yb[a]])
                    act(yb[a][:], yb[a][:], AF.Silu, [b_yb[a]], [b_yb[a]])
                    if ci < 8:
                        act(sq[:], yb[a][:], AF.Square, [b_yb[a]], [b_sq])
                        mm(pss[:], ones[:, :], sq[:], [b_sq], [b_pss])
                        ts(rn[:], pss[:], EPS, None, ALU.add, None, [b_pss], [b_sq])
                        act(rn[:], rn[:], AF.Sqrt, [b_sq], [b_sq])
                        k.op("dve", "reciprocal", R=[b_sq], W=[b_sq], out=rn[:], in_=rn[:])
                        if ci < 4:
                            stt(yb[a][:], yb[a][:], SCALE, rn[:], ALU.mult, ALU.mult, [b_yb[a], b_sq], [b_yb[a]])
                        else:
                            tt(yb[a][:], yb[a][:], rn[:], ALU.mult, [b_yb[a], b_sq], [b_yb[a]])
                    k.dma("sp", dnq_t.ap()[ci * 128:(ci + 1) * 128, t0:t0 + 512], yb[a][:], R=[b_yb[a]], W=[])
            b_g = k.buf("g")
            k.dma("sp", gb_raw[:], small_t.ap().rearrange("(c p) n -> p c n", p=64)[:, :, 12:20], W=[b_g])
            tt(g_sb[:], gb_raw[:, :, 0:4], dnc[:, 0:4].unsqueeze(1).to_broadcast([64, S // 64, 4]), ALU.add,
               [b_g, b_dc], [b_g])
            act(g_sb[:], g_sb[:], AF.Exp, [b_g], [b_g])
            ts(g_sb[:], g_sb[:], 1.0, None, ALU.add, None, [b_g], [b_g])
            act(g_sb[:], g_sb[:], AF.Ln, [b_g], [b_g])
            act(nea[:], dnc[:, 4:8], AF.Exp, [b_dc], [b_g])
            ts(nea[:], nea[:], -1.0, None, ALU.mult, None, [b_g], [b_g])
            tt(g_sb[:], g_sb[:], nea[:].unsqueeze(1).to_broadcast([64, S // 64, 4]), ALU.mult, [b_g], [b_g])
            act(beta_sb[:], gb_raw[:, :, 4:8], AF.Sigmoid, [b_g], [b_g])
            k.barrier()
            es_dp.close()
            cur[0] = es_d
            St = [sb("St%d" % i, [128, 128]) for i in range(4)]
            b_St = k.bufs(4, "St")
            for i in range(4):
                k.op("dve", "memset", W=[b_St[i]], ap=St[i][:], constant=0.0)
            qc = [sb("qc%d" % i, [128, 4, 64]) for i in range(2)]
            kc_ = [sb("kc%d" % i, [128, 4, 64]) for i in range(2)]
            vc_ = [sb("vc%d" % i, [128, 4, 64]) for i in range(2)]
            dzt = [sb("dzt%d" % i, [128, 4, 64]) for i in range(2)]
            b_ld = k.bufs(2, "dnld")
            obT = [sb("obT%d" % i, [128, 4, 64], BF16) for i in range(2)]
            b_obT = k.bufs(2, "obT")
            obTf = [sb("obTf%d" % i, [128, 4, 64]) for i in range(2)] if ob_dbg is not None else None
            NTMP = 176
            tmps = [sb("tmp%d" % i, [128, 128]) for i in range(NTMP)]
            b_tmps = k.bufs(NTMP, "tmp")
            tmp_i = [0]
            banks = [ps("dnp%d" % i, [128, 512]) for i in range(7)]
            b_banks = k.bufs(7, "dnbank")
            for b_ in b_banks:
                b_.excl = True
            b_slots = [b_banks[i // 4] for i in range(28)]
            slot_i = [0]
            b_obb = k.buf("obb")

            def tmp():
                i = tmp_i[0] % NTMP
                tmp_i[0] += 1
                return tmps[i], b_tmps[i]

            def pslot():
                i = slot_i[0] % 28
                slot_i[0] += 1
                return banks[i // 4][:, (i % 4) * 128:(i % 4 + 1) * 128], b_slots[i]

            I64 = ident[0:64, 0:64]
            H = slice(0, 64)
            dnq_v = dnq_t.ap().rearrange("(m h p) t -> m p h t", m=3, p=128)
            def dn_load(c_):
                a_ = c_ % 2
                cs_ = slice(c_ * 64, (c_ + 1) * 64)
                k.dma("sp", qc[a_][:], dnq_v[0][:, :, cs_], W=[b_ld[a_]])
                k.dma("sp", kc_[a_][:], dnq_v[1][:, :, cs_], W=[b_ld[a_]])
                k.dma("sp", vc_[a_][:], dnq_v[2][:, :, cs_], W=[b_ld[a_]])
                k.dma("sp", dzt[a_][:], projT_t.ap()[30 * 128:34 * 128, cs_].rearrange("(h p) t -> p h t", p=128),
                      W=[b_ld[a_]])

            dn_load(0)
            for c in range(S // 64):
                a = c % 2
                cs = slice(c * 64, (c + 1) * 64)
                if c + 1 < S // 64:
                    dn_load(c + 1)
                def head_body(hh, c=c, a=a, cs=cs):
                    L = [b_ld[a]]
                    gc = g_sb[:, c, hh:hh + 1]
                    bc = beta_sb[:, c, hh:hh + 1]
                    kT_h = kc_[a][:, hh, :]
                    qT_h = qc[a][:, hh, :]
                    vT_h = vc_[a][:, hh, :]
                    Gm, bGm = tmp()
                    ts(Gm[H, 0:64], lowst, gc, None, ALU.mult, None, [], [bGm])
                    cp(Gm[H, 64:65], gc, [], [bGm])
                    pDT, bDT = pslot()
                    mm(pDT[H, 0:64], Gm[H, 0:64], tri_le, [bGm], [bDT])
                    pDN, bDN = pslot()
                    mm(pDN[H, 0:65], tri_le, Gm[H, 0:65], [bGm], [bDN])
                    pgl, bgl = pslot()
                    mm(pgl[:, 0:1], ones[H, :], Gm[H, 64:65], [bGm], [bgl])
                    yield
                    ET, bET = tmp()
                    act(ET[H, 0:64], pDT[H, 0:64], AF.Exp, [bDT], [bET])
                    tt(ET[H, 0:64], ET[H, 0:64], tri_le, ALU.mult, [bET], [bET])
                    EN, bEN = tmp()
                    act(EN[H, 0:64], pDN[H, 0:64], AF.Exp, [bDN], [bEN])
                    ENo, bENo = tmp()
                    tt(ENo[H, 0:64], EN[H, 0:64], lowo, ALU.mult, [bEN], [bENo])
                    tt(EN[H, 0:64], EN[H, 0:64], lowd, ALU.mult, [bEN], [bEN])
                    yield
                    sc, bsc = tmp()
                    act(sc[H, 0:1], pDN[H, 64:65], AF.Exp, [bDN], [bsc])
                    cp(sc[H, 6:7], pDN[H, 64:65], [bDN], [bsc])
                    tt(sc[H, 5:6], pgl[H, 0:1], sc[H, 6:7], ALU.subtract, [bgl, bsc], [bsc])
                    act(sc[H, 1:2], sc[H, 5:6], AF.Exp, [bsc], [bsc])
                    tt(sc[H, 2:3], sc[H, 0:1], bc, ALU.mult, [bsc], [bsc])
                    ts(sc[H, 3:4], bc, -1.0, None, ALU.mult, None, [bsc], [bsc])
                    act(sc[:, 4:5], pgl[:, 0:1], AF.Exp, [bgl, bsc], [bsc])
                    yield
                    pk, bpk = pslot()
                    tr(pk[H, :], kT_h, ident[:], L, [bpk])
                    kbd, bkbd = tmp()
                    ts(kbd[H, :], pk[H, :], sc[H, 2:3], None, ALU.mult, None, [bpk, bsc], [bkbd])
                    kd, bkd = tmp()
                    ts(kd[H, :], pk[H, :], sc[H, 1:2], None, ALU.mult, None, [bpk, bsc], [bkd])
                    pv, bpv = pslot()
                    tr(pv[H, :], vT_h, ident[:], L, [bpv])
                    vb, bvb = tmp()
                    ts(vb[H, :], pv[H, :], bc, None, ALU.mult, None, [bpv], [bvb])
                    yield
                    pG, bpG = pslot()
                    mm(pG[H, 0:64], kT_h, kT_h, L, [bpG])
                    B, bB = tmp()
                    stt(B[H, 0:64], pG[H, 0:64], sc[H, 3:4], EN[H, 0:64], ALU.mult, ALU.mult, [bpG, bsc, bEN], [bB])
                    Bo, bBo = tmp()
                    stt(Bo[H, 0:64], pG[H, 0:64], sc[H, 3:4], ENo[H, 0:64], ALU.mult, ALU.mult, [bpG, bsc, bENo], [bBo])
                    pA, bpA = pslot()
                    mm(pA[H, 0:64], kT_h, qT_h, L, [bpA])
                    At, bAt = tmp()
                    tt(At[H, 0:64], pA[H, 0:64], ET[H, 0:64], ALU.mult, [bpA, bET], [bAt])
                    yield
                    pC, bpC = pslot()
                    tr(pC[H, 0:64], B[H, 0:64], I64, [bB], [bpC])
                    C, bC = tmp()
                    cp(C[H, 0:64], pC[H, 0:64], [bpC], [bC], eng="act")
                    X, bX = tmp()
                    tt(X[H, 0:64], pC[H, 0:64], I64, ALU.add, [bpC], [bX])
                    Bp, bBp, Cp, bCp = B, bB, C, bC
                    for lev in range(1, 5):
                        yield
                        pB2, bpB2 = pslot()
                        mm(pB2[H, 0:64], Cp[H, 0:64], Bp[H, 0:64], [bCp, bBp], [bpB2])
                        nB, bnB = tmp()
                        cp(nB[H, 0:64], pB2[H, 0:64], [bpB2], [bnB], eng="act")
                        if lev < 4:
                            pC2, bpC2 = pslot()
                            mm(pC2[H, 0:64], Bp[H, 0:64], Cp[H, 0:64], [bCp, bBp], [bpC2])
                            nC, bnC = tmp()
                            cp(nC[H, 0:64], pC2[H, 0:64], [bpC2], [bnC])
                        yield
                        pX2, bpX2 = pslot()
                        mm(pX2[H, 0:64], nB[H, 0:64], X[H, 0:64], [bnB, bX], [bpX2])
                        nX, bnX = tmp()
                        tt(nX[H, 0:64], pX2[H, 0:64], X[H, 0:64], ALU.add, [bpX2, bX], [bnX])
                        Bp, bBp, X, bX = nB, bnB, nX, bnX
                        if lev < 4:
                            Cp, bCp = nC, bnC
                    yield
                    pM1, bpM1 = pslot()
                    mm(pM1[H, 0:64], Bo[H, 0:64], X[H, 0:64], [bBo, bX], [bpM1])
                    M1, bM1 = tmp()
                    cp(M1[H, 0:64], pM1[H, 0:64], [bpM1], [bM1], eng="act")
                    pTd, bpTd = pslot()
                    tr(pTd[H, 0:64], X[H, 0:64], I64, [bX], [bpTd])
                    Td, bTd = tmp()
                    cp(Td[H, 0:64], pTd[H, 0:64], [bpTd], [bTd])
                    yield
                    pM2, bpM2 = pslot()
                    mm(pM2[H, 0:64], Td[H, 0:64], M1[H, 0:64], [bTd, bM1], [bpM2])
                    Xf, bXf = tmp()
                    tt(Xf[H, 0:64], pM2[H, 0:64], X[H, 0:64], ALU.add, [bpM2, bX], [bXf])
                    X, bX = Xf, bXf
                    yield
                    pu, bpu = pslot()
                    mm(pu[H, :], X[H, 0:64], vb[H, :], [bX, bvb], [bpu])
                    u, bu = tmp()
                    cp(u[H, :], pu[H, :], [bpu], [bu], eng="act")
                    pw, bpw = pslot()
                    mm(pw[:, 0:64], kbd[H, :], X[H, 0:64], [bX, bkbd], [bpw])
                    wT, bwT = tmp()
                    cp(wT[:, 0:64], pw[:, 0:64], [bpw], [bwT])
                    yield
                    ppv, bppv = pslot()
                    mm(ppv[H, :], wT[:, 0:64], St[hh][:], [bwT, b_St[hh]], [bppv])
                    vn, bvn = tmp()
                    tt(vn[H, :], u[H, :], ppv[H, :], ALU.subtract, [bu, bppv], [bvn])
                    po1, bpo1 = pslot()
                    mm(po1[H, :], qT_h, St[hh][:], L + [b_St[hh]], [bpo1])
                    o1, bo1 = tmp()
                    ts(o1[H, :], po1[H, :], sc[H, 0:1], None, ALU.mult, None, [bpo1, bsc], [bo1])
                    yield
                    po2, bpo2 = pslot()
                    mm(po2[H, :], At[H, 0:64], vn[H, :], [bAt, bvn], [bpo2])
                    o_, bo = tmp()
                    tt(o_[H, :], po2[H, :], o1[H, :], ALU.add, [bpo2, bo1], [bo])
                    yield
                    pSn, bpSn = pslot()
                    mm(pSn[:, :], kd[H, :], vn[H, :], [bkd, bvn], [bpSn])
                    stt(St[hh][:], St[hh][:], sc[:, 4:5], pSn[:, :], ALU.mult, ALU.add,
                        [b_St[hh], bsc, bpSn], [b_St[hh]])
                    yield
                    jk, bjk = tmp()
                    act(jk[H, :], o_[H, :], AF.Square, [bo], [bjk, bsc], accum_out=sc[H, 7:8])
                    ts(sc[H, 7:8], sc[H, 7:8], 1.0 / 128, EPS, ALU.mult, ALU.add, [bsc], [bsc])
                    act(sc[H, 7:8], sc[H, 7:8], AF.Sqrt, [bsc], [bsc])
                    k.op("dve", "reciprocal", R=[bsc], W=[bsc], out=sc[H, 7:8], in_=sc[H, 7:8])
                    on, bon = tmp()
                    stt(on[H, :], o_[H, :], sc[H, 7:8], dnc[:, 8:136], ALU.mult, ALU.mult, [bo, bsc], [bon])
                    yield
                    pot, bpot = pslot()
                    tr(pot[:, 0:64], on[H, :], I64, [bon], [bpot])
                    tt(obT[a][:, hh, :], pot[:, 0:64], dzt[a][:, hh, :], ALU.mult, [bpot] + L, [b_obT[a]])
                    if ob_dbg is not None:
                        tt(obTf[a][:, hh, :], pot[:, 0:64], dzt[a][:, hh, :], ALU.mult, [bpot] + L, [b_obT[a]])
                gens = [head_body(hh) for hh in range(4)]
                while gens:
                    for g_ in list(gens):
                        try:
                            next(g_)
                        except StopIteration:
                            gens.remove(g_)
                k.dma("sp", ob_b[(c * 64) // CW].ap().rearrange("(h p) t -> p h t", p=128)[:, :, (c * 64) % CW:(c * 64) % CW + 64],
                      obT[a][:], R=[b_obT[a]], W=[])
                if ob_dbg is not None:
                    k.dma("sp", ob_dbg.ap().rearrange("(h p) t -> p h t", p=128)[:, :, cs], obTf[a][:],
                          R=[b_obT[a]], W=[])
            k.barrier()
            es_d.close()
            if STOP_AFTER == "dn":
                finish()
                return

            b_g1 = k.buf("gath1")
            for i in range(NCW):
                k.allgather(oa_b[i], oa_f[i])
                k.allgather(ob_b[i], ob_f[i])
            k.barrier()
            es_2 = ExitStack()
            cur[0] = es_2
            wpa = sb("wpa", [128, 8, 16, 128], BF16)
            wpb = sb("wpb", [128, 8, 16, 128], BF16)
            b_wp = k.buf("wp")
            stg_alloc(2)
            for ch in range(8):
                cast_load(wpa[:, ch, :, :].rearrange("p a b -> p (a b)"), wpa_t.ap()[ch, :, :], [b_wp])
                cast_load(wpb[:, ch, :, :].rearrange("p a b -> p (a b)"), wpb_t.ap()[ch, :, :], [b_wp])
            oaT = [sb("oaT%d" % i, [128, 16, 512], BF16) for i in range(2)]
            obT2 = [sb("obT2%d" % i, [128, 16, 512], BF16) for i in range(2)]
            b_oT2 = k.bufs(2, "oT2")
            ga = [sb("ga%d" % i, [128, 512]) for i in range(2)]
            gb_ = [sb("gb%d" % i, [128, 512]) for i in range(2)]
            b_gg = k.bufs(2, "gg")
            t1 = [sb("t1%d" % i, [128, 512]) for i in range(2)]
            t2 = [sb("t2%d" % i, [128, 512]) for i in range(2)]
            mgo = [sb("mgo%d" % i, [128, 512], BF16) for i in range(2)]
            mgof = [sb("mgof%d" % i, [128, 512]) for i in range(2)] if mg_dbg is not None else None
            b_mgo = k.bufs(2, "mgo")
            pa = [ps("pa%d" % i, [128, 512]) for i in range(2)]
            pb2 = [ps("pb2%d" % i, [128, 512]) for i in range(2)]
            b_pab = k.bufs(2, "pab")
            b_mgb = k.buf("mgb")
            it = 0
            for tt_i in range(S // 512):
                t0 = tt_i * 512
                a = tt_i % 2
                oa_v = oa_f[t0 // CW].ap().rearrange("(kc p) t -> p kc t", p=128)
                ob_v = ob_f[t0 // CW].ap().rearrange("(kc p) t -> p kc t", p=128)
                k.dma("sp", oaT[a][:], oa_v[:, :, t0 % CW:t0 % CW + 512], W=[b_oT2[a]])
                k.dma("sp", obT2[a][:], ob_v[:, :, t0 % CW:t0 % CW + 512], W=[b_oT2[a]])
                for ch in range(8):
                    p = it % 2
                    it += 1
                    k.dma("sp", ga[p][:], projT_t.ap()[(34 + ch) * 128:(35 + ch) * 128, t0:t0 + 512], W=[b_gg[p]])
                    k.dma("sp", gb_[p][:], projT_t.ap()[(42 + ch) * 128:(43 + ch) * 128, t0:t0 + 512], W=[b_gg[p]])
                    for kc in range(16):
                        mm(pa[p][:], wpa[:, ch, kc, :], oaT[a][:, kc, :], [b_wp, b_oT2[a]], [b_pab[p]],
                           start=(kc == 0), stop=(kc == 15))
                    for kc in range(16):
                        mm(pb2[p][:], wpb[:, ch, kc, :], obT2[a][:, kc, :], [b_wp, b_oT2[a]], [b_pab[p]],
                           start=(kc == 0), stop=(kc == 15))
                    tt(t1[p][:], pa[p][:], ga[p][:], ALU.mult, [b_pab[p], b_gg[p]], [b_mgo[p]])
                    tt(t2[p][:], pb2[p][:], gb_[p][:], ALU.mult, [b_pab[p], b_gg[p]], [b_mgo[p]])
                    tt(mgo[p][:], t1[p][:], t2[p][:], ALU.add, [b_mgo[p]], [b_mgo[p]], eng="pool")
                    k.dma("act", mg_b[tt_i].ap()[ch * 128:(ch + 1) * 128, :], mgo[p][:], R=[b_mgo[p]], W=[])
                    if mg_dbg is not None:
                        tt(mgof[p][:], t1[p][:], t2[p][:], ALU.add, [b_mgo[p]], [b_mgo[p]])
                        k.dma("sp", mg_dbg.ap()[ch * 128:(ch + 1) * 128, t0:t0 + 512], mgof[p][:], R=[b_mgo[p]], W=[])
            k.barrier()
            es_2.close()
            b_g2 = k.buf("gath2")
            for i in range(S // 512):
                k.allgather(mg_b[i], mg_f[i])
            k.barrier()

            es_3 = ExitStack()
            cur[0] = es_3
            wo = sb("wo", [128, 8, 32, 128], BF16)
            b_wo = k.buf("wo")
            es_3s = ExitStack()
            cur[0] = es_3s
            stg_alloc(2)
            for ch in range(8):
                cast_load(wo[:, ch, :, :].rearrange("p a b -> p (a b)"), wo_t.ap()[ch, :, :], [b_wo])
            k.barrier()
            es_3s.close()
            cur[0] = es_3
            mgT = [sb("mgT%d" % i, [128, 32, 512], BF16) for i in range(2)]
            xcs = [sb("xcs%d" % i, [128, 4, 1024]) for i in range(2)]
            b_in7 = k.bufs(2, "in7")
            y1 = [sb("y1%d" % i, [128, 512]) for i in range(2)]
            b_y1 = k.bufs(2, "y1")
            ssq = sb("ssq", [128, S // 128])
            b_ssq = k.buf("ssq")
            junk2 = sb("junk2", [128, 1024], BF16)
            b_j2 = k.buf("j2")
            pm2 = [ps("pm2%d" % i, [128, 512]) for i in range(2)]
            b_pm2 = k.bufs(2, "pm2")
            pt2 = [ps("pt2%d" % i, [128, 512]) for i in range(2)]
            b_pt2 = k.bufs(2, "pt2")
            b_yb_ = k.buf("ybuf")
            it = 0
            for tt_i in range(S // 512):
                t0 = tt_i * 512
                a = tt_i % 2
                k.dma("sp", mgT[a][:], mg_f[tt_i].ap().rearrange("(kc p) t -> p kc t", p=128), W=[b_in7[a]])
                k.dma("sp", xcs[a][:], xc_t.ap()[t0:t0 + 512, :].rearrange("(s p) n -> p s n", p=128), W=[b_in7[a]])
                for ch in range(8):
                    p = it % 2
                    it += 1
                    for kc in range(32):
                        mm(pm2[p][:], wo[:, ch, kc, :], mgT[a][:, kc, :], [b_wo, b_in7[a]], [b_pm2[p]],
                           start=(kc == 0), stop=(kc == 31))
                    act(y1[p][:], pm2[p][:], AF.Identity, [b_pm2[p]], [b_y1[p]], scale=gate_sb[:, ch:ch + 1])
                    for sub in range(4):
                        tr(pt2[p][:, sub * 128:(sub + 1) * 128], y1[p][:, sub * 128:(sub + 1) * 128], ident[:],
                           [b_y1[p]], [b_pt2[p]])
                    xv = xcs[a][:, :, ch * 128:(ch + 1) * 128]
                    tt(xv, pt2[p][:].rearrange("p (s c) -> p s c", c=128), xv, ALU.add, [b_pt2[p], b_in7[a]], [b_in7[a]])
                for sub in range(4):
                    act(junk2[:], xcs[a][:, sub, :], AF.Square, [b_in7[a]], [b_j2, b_ssq],
                        accum_out=ssq[:, tt_i * 4 + sub:tt_i * 4 + sub + 1])
                k.dma("act", ybuf_t.ap()[t0:t0 + 512, :].rearrange("(s p) n -> p s n", p=128), xcs[a][:],
                      R=[b_in7[a]], W=[])
            b_ssb = k.buf("ssb")
            k.dma("sp", ss_b.ap()[:, :], ssq[:], R=[b_ssq], W=[b_ssb])
            k.barrier()
            k.allgather(ss_b, ss_f, W=[b_ssb])
            k.barrier()
            ssf = sb("ssf", [128, 4, S // 128])
            rstd = sb("rstd", [128, S // 128])
            fg = sb("fg", [128, 1024])
            b_fin = k.buf("fin")
            k.dma("sp", ssf[:], ss_f.ap().rearrange("(r p) n -> p r n", p=128), W=[b_fin])
            k.dma("sp", fg[:], fg_t.ap()[:, :], W=[b_fin])
            tt(rstd[:], ssf[:, 0, :], ssf[:, 1, :], ALU.add, [b_fin], [b_fin])
            tt(rstd[:], rstd[:], ssf[:, 2, :], ALU.add, [b_fin], [b_fin])
            tt(rstd[:], rstd[:], ssf[:, 3, :], ALU.add, [b_fin], [b_fin])
            ts(rstd[:], rstd[:], 1.0 / D, EPS, ALU.mult, ALU.add, [b_fin], [b_fin])
            act(rstd[:], rstd[:], AF.Sqrt, [b_fin], [b_fin])
            k.op("dve", "reciprocal", R=[b_fin], W=[b_fin], out=rstd[:], in_=rstd[:])
            yt = [sb("yt%d" % i, [128, 1024]) for i in range(2)]
            b_yt = k.bufs(2, "yt")
            for tile in range(S // 128):
                a = tile % 2
                k.dma("sp", yt[a][:], ybuf_t.ap()[tile * 128:(tile + 1) * 128, :], W=[b_yt[a]])
                stt(yt[a][:], yt[a][:], rstd[:, tile:tile + 1], fg[:], ALU.mult, ALU.mult, [b_yt[a], b_fin], [b_yt[a]])
                k.dma("act", out_t.ap()[tile * 128:(tile + 1) * 128, :], yt[a][:], R=[b_yt[a]], W=[])
            finish()
            es_3.close()
            es_c.close()
    return nc


def _col_index(j):
    g, half = j // 2, j % 2
    o_q, o_kv, o_g, o_z = 0, 2048, 2048 + 1536, 2048 + 1536 + 48
    o_dn = o_z + 2048
    o_a = o_dn + 6144
    o_b = o_a + 16
    o_dz = o_b + 16
    o_mg = o_dz + 2048
    cols = []
    my_heads = [8 * g + 4 * half + i for i in range(4)]
    ot_heads = [8 * g + 4 * (1 - half) + i for i in range(4)]
    for h in my_heads + ot_heads:
        cols += list(range(o_q + h * 128, o_q + (h + 1) * 128))
    for t in range(6):
        cols += list(range(o_kv + t * 256 + g * 128, o_kv + t * 256 + (g + 1) * 128))
    for h in my_heads:
        cols += list(range(o_z + h * 128, o_z + (h + 1) * 128))
    dn_heads = [4 * j + i for i in range(4)]
    for t in range(3):
        for h in dn_heads:
            cols += list(range(o_dn + t * 2048 + h * 128, o_dn + t * 2048 + (h + 1) * 128))
    for h in dn_heads:
        cols += list(range(o_dz + h * 128, o_dz + (h + 1) * 128))
    cols += list(range(o_mg + j * 1024, o_mg + (j + 1) * 1024))
    cols += list(range(o_mg + 4096 + j * 1024, o_mg + 4096 + (j + 1) * 1024))
    small = []
    for br in range(3):
        small += [o_g + br * 16 + h for h in my_heads]
    small += [o_a + h for h in dn_heads]
    small += [o_b + h for h in dn_heads]
    return np.array(cols), np.array(small)


def _pk(v):
    return np.ascontiguousarray(v.reshape(NKC, 128).T)


def _wlayout(w, nkc):
    w = w.reshape(nkc, 128, 8, 128).transpose(2, 1, 0, 3)
    return np.ascontiguousarray(w).reshape(8, 128, nkc * 128)


def _consts():
    f32 = np.float32
    idx = np.arange(64)
    tri_le = (idx[:, None] <= idx[None, :]).astype(f32)
    lowst = (idx[:, None] > idx[None, :]).astype(f32)
    bd = (idx[:, None] // 32 == idx[None, :] // 32).astype(f32)
    c64 = np.concatenate([tri_le, lowst, lowst * bd, lowst * (1 - bd)], 1)
    p = np.arange(128)[:, None]
    c = np.arange(256)[None, :]
    cval = (16 * c + 31 - p).astype(f32)
    cval[:, 255] = 1e9
    cmp_start = np.arange(255) * 16
    slc_start = np.arange(64) * 64
    ov = ((cmp_start[:, None] <= slc_start[None, :] + 63) & (cmp_start[:, None] + 31 >= slc_start[None, :])).astype(f32)
    ov = np.concatenate([ov, np.zeros((1, 64), f32)], 0)
    ovl = ov.reshape(2, 128, 64).transpose(1, 0, 2).reshape(128, 128)
    d = np.arange(128)
    invf = np.where(d < 32, 500000.0 ** (-(d % 16) / 16.0), 0.0).astype(f32)[:, None]
    cc = np.arange(128)[None, :]
    tri128 = (cc <= p).astype(f32)
    triw = (cc > p).astype(f32)
    c128 = np.concatenate([cval, ovl, invf, tri128, triw], 1).astype(f32)
    rotT = np.zeros((32, 32), f32)
    for m in range(16):
        rotT[m + 16, m] = -1.0
        rotT[m, m + 16] = 1.0
    vmask = np.zeros((32, 128, 64), f32)
    fbias = np.zeros((32, 128, 64), f32)
    j = np.arange(64)[None, :]
    for qi in range(32):
        t = (128 * qi + np.arange(128))[:, None]
        tb = t // 64
        valid = (j * 64 <= t)
        forced = (j == 0) | (j == tb) | (j == tb - 1)
        vmask[qi] = (valid & ~forced)
        fbias[qi] = np.where(forced, 1e9, np.where(valid, 0.0, -1.0))
    return dict(ident=np.eye(128, dtype=f32), c64=c64, c128=c128, rotT=rotT, vmask=vmask, fbias=fbias)


def make_in_maps(inp):
    f32 = np.float32
    maps = []
    cst = _consts()
    w_in = np.asarray(inp["w_in"][0])
    w_ada = np.asarray(inp["w_ada"][0])
    conv_w = np.asarray(inp["conv_w"][0])
    for core in range(8):
        b, j = core // 4, core % 4
        cols, small = _col_index(j)
        wj = w_in[:, cols]
        wj = wj.reshape(NKC, 128, NCHUNK, 128).transpose(2, 1, 0, 3)
        wj = np.ascontiguousarray(wj).reshape(NCHUNK, 128, NKC * 128)
        ws = w_in[:, small].reshape(NKC, 128, NSMALL).transpose(1, 0, 2)
        ws = np.ascontiguousarray(ws).reshape(128, NKC * NSMALL)
        gsel = np.zeros((96, 8), f32)
        for ch in range(8):
            gsel[64 + 8 * j + ch, ch] = 1.0
        dn_heads = [4 * j + i for i in range(4)]
        cw = np.zeros((128, 12, 4), f32)
        for ci in range(12):
            t, h = ci // 4, dn_heads[ci % 4]
            cw[:, ci, :] = conv_w[:, t * 2048 + h * 128:t * 2048 + (h + 1) * 128].T
        dnc = np.zeros((64, 136), f32)
        dnc[:, 0:4] = np.asarray(inp["dt_bias"][0])[dn_heads][None, :]
        dnc[:, 4:8] = np.asarray(inp["a_log"][0])[dn_heads][None, :]
        dnc[:, 8:136] = np.asarray(inp["dn_norm_gain"][0])[None, :]
        cs = slice(j * 1024, (j + 1) * 1024)
        m = {
            "x": np.ascontiguousarray(inp["x"][b]),
            "xcols": np.ascontiguousarray(inp["x"][b][:, cs]),
            "c_pk": _pk(np.asarray(inp["c"][b])),
            "w_ada": np.ascontiguousarray(w_ada[:, j * 3072:(j + 1) * 3072]),
            "b_ada": np.ascontiguousarray(inp["b_ada"][0][j * 3072:(j + 1) * 3072]).reshape(1, 3072),
            "ngain_pk": _pk(np.asarray(inp["norm_gain"][0])),
            "gsel": gsel,
            "w_in": wj,
            "w_small": ws,
            "pos": np.ascontiguousarray(inp["positions"][b]).reshape(1, S).astype(np.int32),
            "w1k": np.ascontiguousarray(inp["w_cmp_k1"][0]),
            "w1v": np.ascontiguousarray(inp["w_cmp_v1"][0]),
            "w2k": np.ascontiguousarray(inp["w_cmp_k2"][0]),
            "w2v": np.ascontiguousarray(inp["w_cmp_v2"][0]),
            "cposkT": np.ascontiguousarray(inp["cmp_pos_k"][0].T),
            "cposvT": np.ascontiguousarray(inp["cmp_pos_v"][0].T),
            "convw": cw.reshape(128, 48),
            "dnc": dnc,
            "wpa": _wlayout(np.asarray(inp["w_proj_a"][0])[:, cs], 16),
            "wpb": _wlayout(np.asarray(inp["w_proj_b"][0])[:, cs], 16),
            "wo": _wlayout(np.asarray(inp["w_out"][0])[:, cs], 32),
            "fgain": np.ascontiguousarray(np.broadcast_to(np.asarray(inp["final_gain"])[cs][None, :], (128, 1024))),
        }
        m.update(cst)
        maps.append(m)
    return maps


def kernel(**inputs):
    inp = {k_: np.asarray(v) for k_, v in inputs.items()}
    nc = build_program()
    maps = make_in_maps(inp)
    res = run_bass_kernel_spmd(nc, maps, core_ids=list(range(8)))
    out = np.zeros((2, S, D), np.float32)
    for core in range(8):
        b, j = core // 4, core % 4
        out[b][:, j * 1024:(j + 1) * 1024] = res.results[core]["out"]
    return out
```

```python
import numpy as np
from contextlib import ExitStack
import concourse.bass as bass
import concourse.mybir as mybir
from concourse.bass_utils import run_bass_kernel_spmd

F32 = mybir.dt.float32
BF16 = mybir.dt.bfloat16
I32 = mybir.dt.int32
AF = mybir.ActivationFunctionType
ALU = mybir.AluOpType
AX = mybir.AxisListType

D = 4096
S = 4096
NKC = 32
NCHUNK = 50
NSMALL = 20
EPS = 1e-6
GROUPS = [[0, 1, 2, 3], [4, 5, 6, 7]]

DEBUG = None
STOP_AFTER = None


class Buf:
    __slots__ = ("name", "w", "r", "excl")

    def __init__(self, name):
        self.name = name
        self.w = None
        self.r = {}
        self.excl = False


class K:
    ENG = ("pe", "act", "dve", "pool", "sp")

    def __init__(self, nc, stack):
        self.nc = nc
        self.e = {"pe": nc.tensor, "act": nc.scalar, "dve": nc.vector, "pool": nc.gpsimd, "sp": nc.sync}
        self.stack = stack
        self.sems = {}
        self.cnt = {}
        for en in self.ENG:
            self.sems[en] = stack.enter_context(nc.semaphore("s_" + en))
            self.cnt[en] = 0
        self.ring = [[stack.enter_context(nc.semaphore("d%d" % i)), 0] for i in range(40)]
        self.ring_i = 0
        self.cc_sem = stack.enter_context(nc.semaphore("cc"))
        self.cc_cnt = 0
        self.waited = {en: {} for en in self.ENG}
        self.nbuf = 0

    def buf(self, name=None):
        self.nbuf += 1
        return Buf(name or "b%d" % self.nbuf)

    def bufs(self, n, name="b"):
        return [self.buf("%s%d" % (name, i)) for i in range(n)]

    def _wait(self, eng, sem, val):
        key = id(sem)
        if self.waited[eng].get(key, -1) >= val:
            return
        self.waited[eng][key] = val
        self.e[eng].wait_ge(sem, val)

    def _deps(self, eng, R, W, is_dma):
        need = {}

        def add(ev):
            if ev is None:
                return
            sem, val, src = ev
            if src == "pe" and eng == "pe" and not is_dma:
                return
            k = id(sem)
            if k not in need or need[k][1] < val:
                need[k] = (sem, val)

        for b in R:
            add(b.w)
            if b.excl:
                for ev in b.r.values():
                    add(ev)
        for b in W:
            add(b.w)
            for ev in b.r.values():
                add(ev)
        for sem, val in need.values():
            self._wait(eng, sem, val)

    def _record(self, ev, R, W):
        for b in R:
            if b.excl:
                b.w = ev
                b.r = {}
                continue
            k = (ev[2], id(ev[0]))
            b.r[k] = ev
        for b in W:
            b.w = ev
            b.r = {}

    def op(self, eng, meth, R=(), W=(), **kw):
        self._deps(eng, R, W, False)
        ins = getattr(self.e[eng], meth)(**kw)
        self.cnt[eng] += 1
        ins.then_inc(self.sems[eng], 1)
        ev = (self.sems[eng], self.cnt[eng], eng)
        self._record(ev, R, W)
        return ev

    def dma(self, eng, out, in_, R=(), W=(), **kw):
        self._deps(eng, R, W, True)
        slot = self.ring[self.ring_i]
        self.ring_i = (self.ring_i + 1) % len(self.ring)
        if slot[1] > 0:
            self._wait(eng, slot[0], slot[1])
        ins = self.e[eng].dma_start(out=out, in_=in_, **kw)
        slot[1] += 16
        ins.then_inc(slot[0], 16)
        ev = (slot[0], slot[1], "dma")
        self._record(ev, R, W)
        return ev

    def allgather(self, in_t, out_t, R=(), W=()):
        self._deps("pool", R, W, True)
        ins = self.nc.gpsimd.collective_compute(
            "AllGather", ALU.bypass, replica_groups=GROUPS,
            ins=[in_t.ap().opt()], outs=[out_t.ap().opt()])
        self.cc_cnt += 1
        ins.then_inc(self.cc_sem)
        ev = (self.cc_sem, self.cc_cnt, "dma")
        self._record(ev, R, W)
        return ev

    def barrier(self):
        for en in self.ENG:
            for src in self.ENG:
                if self.cnt[src] > 0 and src != en:
                    self._wait(en, self.sems[src], self.cnt[src])
            for slot in self.ring:
                if slot[1] > 0:
                    self._wait(en, slot[0], slot[1])
            if self.cc_cnt:
                self._wait(en, self.cc_sem, self.cc_cnt)
        for en in self.ENG:
            if self.cnt[en] > 0 and en != "pe":
                self._wait(en, self.sems[en], self.cnt[en])

    def finish(self, out_bufs):
        self.barrier()


SCALE = 128.0 ** -0.5
TWO_PI = 2.0 * np.pi


def build_program():
    nc = bass.Bass("TRN2", target_bir_lowering=False)
    dbg = DEBUG or set()

    def din(name, shape, dt=F32):
        return nc.dram_tensor(name, list(shape), dt, kind="ExternalInput")

    dbg_pairs = []

    def dscr(name, shape, dt=F32):
        if name in dbg and name in ("projT", "small", "dnq", "ybuf"):
            t = nc.dram_tensor(name + "_i", list(shape), dt)
            o = nc.dram_tensor(name, list(shape), dt, kind="ExternalOutput")
            dbg_pairs.append((t, o))
            return t
        if name in dbg:
            return nc.dram_tensor(name, list(shape), dt, kind="ExternalOutput")
        return nc.dram_tensor(name, list(shape), dt)

    x_t = din("x", [S, D])
    xc_t = din("xcols", [S, 1024])
    c_t = din("c_pk", [128, NKC])
    wada_t = din("w_ada", [D, 3072])
    bada_t = din("b_ada", [1, 3072])
    ngain_t = din("ngain_pk", [128, NKC])
    gsel_t = din("gsel", [96, 8])
    win_t = din("w_in", [NCHUNK, 128, NKC * 128])
    wsm_t = din("w_small", [128, NKC * NSMALL])
    pos_t = din("pos", [1, S], I32)
    w1k_t = din("w1k", [D, 256])
    w1v_t = din("w1v", [D, 256])
    w2k_t = din("w2k", [256, 128])
    w2v_t = din("w2v", [256, 128])
    cposk_t = din("cposkT", [128, 32])
    cposv_t = din("cposvT", [128, 32])
    convw_t = din("convw", [128, 12 * 4])
    dnc_t = din("dnc", [64, 8 + 128])
    wpa_t = din("wpa", [8, 128, 16 * 128])
    wpb_t = din("wpb", [8, 128, 16 * 128])
    wo_t = din("wo", [8, 128, 32 * 128])
    fg_t = din("fgain", [128, 1024])
    ident_t = din("ident", [128, 128])
    c64_t = din("c64", [64, 256])
    c128_t = din("c128", [128, 256 + 128 + 1 + 128 + 128])
    rot_t = din("rotT", [32, 32])
    vmask_t = din("vmask", [32, 128, 64])
    fbias_t = din("fbias", [32, 128, 64])
    out_t = nc.dram_tensor("out", [S, 1024], F32, kind="ExternalOutput")

    modb_t = nc.dram_tensor("mod_b", [1, 3072], F32)
    modf_t = nc.dram_tensor("mod_f", [4, 3072], F32)
    projT_t = dscr("projT", [NCHUNK * 128, S])
    small_t = dscr("small", [S, NSMALL])
    dnq_t = dscr("dnq", [3 * 512, S])
    CW = min(1024, S)
    NCW = S // CW
    oa_b = [nc.dram_tensor("oa_b%d" % i, [512, CW], BF16) for i in range(NCW)]
    oa_f = [nc.dram_tensor("oa_f%d" % i, [2048, CW], BF16) for i in range(NCW)]
    ob_b = [nc.dram_tensor("ob_b%d" % i, [512, CW], BF16) for i in range(NCW)]
    ob_f = [nc.dram_tensor("ob_f%d" % i, [2048, CW], BF16) for i in range(NCW)]
    mg_b = [nc.dram_tensor("mg_b%d" % i, [1024, 512], BF16) for i in range(S // 512)]
    mg_f = [nc.dram_tensor("mg_f%d" % i, [4096, 512], BF16) for i in range(S // 512)]
    ybuf_t = dscr("ybuf", [S, 1024])
    ss_b = nc.dram_tensor("ss_b", [128, S // 128], F32)
    ss_f = nc.dram_tensor("ss_f", [512, S // 128], F32)
    oa_dbg = dscr("oa_dbg", [512, S]) if "oa_dbg" in dbg else None
    ob_dbg = dscr("ob_dbg", [512, S]) if "ob_dbg" in dbg else None
    qr_dbg = dscr("qr_dbg", [128, S]) if "qr_dbg" in dbg else None
    kc_dbg = dscr("kc_dbg", [128, 512]) if "kc_dbg" in dbg else None
    mg_dbg = dscr("mg_dbg", [1024, S]) if "mg_dbg" in dbg else None

    es = ExitStack()
    with es:
        k = K(nc, es)
        blk = es.enter_context(nc.Block())
        cur = [None]
        b_out = k.buf("out")

        def sb(name, shape, dt=F32):
            return cur[0].enter_context(nc.sbuf_tensor("sb_" + name, list(shape), dt))

        def ps(name, shape, dt=F32):
            return cur[0].enter_context(nc.psum_tensor("ps_" + name, list(shape), dt))

        def finish():
            k.barrier()
            for t, o in dbg_pairs:
                n = t.ap().shape[0]
                step = 128 if n % 128 == 0 else n
                for r0 in range(0, n, step):
                    k.dma("sp", o.ap()[r0:r0 + step, :], t.ap()[r0:r0 + step, :])
            k.barrier()

        stg = {}

        def stg_alloc(n=2, size=4096):
            stg["t"] = [sb("stg%d_%d" % (k.nbuf, i), [128, size]) for i in range(n)]
            stg["b"] = k.bufs(n, "stg")
            stg["i"] = 0

        def cast_load(dst, src, W, inner=None):
            i = stg["i"] % len(stg["t"])
            stg["i"] += 1
            n = 1
            for d_ in dst.shape[1:]:
                n *= d_
            sv = stg["t"][i][:, 0:n]
            if inner is not None:
                sv = sv.rearrange("p (a b) -> p a b", b=inner)
            k.dma("sp", sv, src, W=[stg["b"][i]])
            ce = ("dve", "act", "dve", "act", "dve", "act", "pool")[stg["i"] % 7]
            cp(dst, sv, [stg["b"][i]], W, eng=ce)

        def mm(out, lhsT, rhs, R, W, start=True, stop=True):
            return k.op("pe", "matmul", R=R, W=W, out=out, lhsT=lhsT, rhs=rhs, start=start, stop=stop)

        def tr(out, in_, idn, R, W):
            return k.op("pe", "transpose", R=R, W=W, out=out, in_=in_, identity=idn)

        def act(out, in_, func, R, W, **kw):
            return k.op("act", "activation", R=R, W=W, out=out, in_=in_, func=func, **kw)

        def ts(out, in0, s1, s2, op0, op1, R, W, eng="dve", **kw):
            if op1 is None:
                return k.op(eng, "tensor_scalar", R=R, W=W, out=out, in0=in0, scalar1=s1, scalar2=None, op0=op0, **kw)
            return k.op(eng, "tensor_scalar", R=R, W=W, out=out, in0=in0, scalar1=s1, scalar2=s2, op0=op0, op1=op1, **kw)

        def tt(out, in0, in1, op, R, W, eng="dve"):
            return k.op(eng, "tensor_tensor", R=R, W=W, out=out, in0=in0, in1=in1, op=op)

        def stt(out, in0, scalar, in1, op0, op1, R, W, **kw):
            return k.op("dve", "scalar_tensor_tensor", R=R, W=W, out=out, in0=in0, scalar=scalar, in1=in1,
                        op0=op0, op1=op1, **kw)

        def cp(out, in_, R, W, eng="dve"):
            if eng == "act":
                return k.op("act", "activation", R=R, W=W, out=out, in_=in_, func=AF.Copy)
            return k.op(eng, "tensor_copy", R=R, W=W, out=out, in_=in_)

        @blk.sync
        def _(sync_eng):
            es_c = ExitStack()
            cur[0] = es_c
            ident = sb("ident", [128, 128])
            identb = sb("identb", [128, 128], BF16)
            ones = sb("ones", [128, 128])
            c64 = sb("c64", [64, 256])
            c128 = sb("c128", [128, 641])
            tri_b = sb("tri_b", [128, 128], BF16)
            triw_b = sb("triw_b", [128, 128], BF16)
            ones_b = sb("ones_b", [128, 512], BF16)
            rotT = sb("rotT", [32, 32])
            a_sb = sb("a_sb", [128, NKC])
            shift_sb = sb("shift_sb", [128, NKC])
            gate_sb = sb("gate_sb", [128, 8])
            b_cst = k.buf("cst")
            k.dma("sp", ident[:], ident_t.ap()[:, :], W=[b_cst])
            k.dma("sp", c64[:], c64_t.ap()[:, :], W=[b_cst])
            k.dma("sp", c128[:], c128_t.ap()[:, :], W=[b_cst])
            k.dma("sp", rotT[:], rot_t.ap()[:, :], W=[b_cst])
            cp(identb[:], ident[:], [b_cst], [b_cst])
            k.op("dve", "memset", W=[b_cst], ap=ones[:], constant=1.0)
            k.op("dve", "memset", W=[b_cst], ap=ones_b[:], constant=1.0)
            cp(tri_b[:], c128[:, 385:513], [b_cst], [b_cst])
            cp(triw_b[:], c128[:, 513:641], [b_cst], [b_cst])
            tri_le = c64[:, 0:64]
            lowst = c64[:, 64:128]
            lowd = c64[:, 128:192]
            lowo = c64[:, 192:256]
            cval = c128[:, 0:256]
            ovl = c128[:, 256:384].rearrange("p (a b) -> p a b", b=64)
            invf = c128[:, 384:385]
            b_mod = k.buf("modvecs")
            k.barrier()

            es_p = ExitStack()
            cur[0] = es_p
            c_sb = sb("c_sb", [128, NKC])
            bada_sb = sb("bada_sb", [1, 3072])
            mod_sb = sb("mod_sb", [1, 3072])
            wa = [sb("wa%d" % i, [128, NKC, 256]) for i in range(2)]
            b_wa = k.bufs(2, "wa")
            pm = [ps("pm%d" % i, [128, 512]) for i in range(2)]
            b_pm = k.bufs(2, "pm")
            b_c = k.buf("c")
            b_modsb = k.buf("modsb")
            k.dma("sp", c_sb[:], c_t.ap()[:, :], W=[b_c])
            k.dma("sp", bada_sb[:], bada_t.ap()[:, :], W=[b_c])
            wada_v = wada_t.ap().rearrange("(kc p) n -> p kc n", p=128)
            for ct in range(12):
                i = ct % 2
                k.dma("sp", wa[i][:], wada_v[:, :, ct * 256:(ct + 1) * 256], W=[b_wa[i]])
                for kc in range(NKC):
                    mm(pm[i][0:1, 0:256], c_sb[:, kc:kc + 1], wa[i][:, kc, :], [b_c, b_wa[i]], [b_pm[i]],
                       start=(kc == 0), stop=(kc == NKC - 1))
                tt(mod_sb[0:1, ct * 256:(ct + 1) * 256], pm[i][0:1, 0:256],
                   bada_sb[0:1, ct * 256:(ct + 1) * 256], ALU.add, [b_pm[i], b_c], [b_modsb])
            b_modb = k.buf("modb")
            b_modf = k.buf("modf")
            k.dma("sp", modb_t.ap()[:, :], mod_sb[:], R=[b_modsb], W=[b_modb])
            k.allgather(modb_t, modf_t, R=[b_modb], W=[b_modf])
            rows = sb("modrows", [96, 128])
            b_rows = k.buf("rows")
            modf_rows = modf_t.ap().rearrange("r (a p) -> (r a) p", p=128)
            k.dma("sp", rows[:], modf_rows, R=[b_modf], W=[b_rows])
            tr(pm[0][:, 0:96], rows[:], ident[0:96, 0:96], [b_rows], [b_pm[0]])
            ng_sb = sb("ng_sb", [128, NKC])
            k.dma("sp", ng_sb[:], ngain_t.ap()[:, :], W=[b_c])
            cp(shift_sb[:], pm[0][:, 0:32], [b_pm[0]], [b_mod])
            stt(a_sb[:], pm[0][:, 32:64], 1.0, ng_sb[:], ALU.add, ALU.mult, [b_pm[0], b_c], [b_mod])
            gsel = sb("gsel", [96, 8])
            k.dma("sp", gsel[:], gsel_t.ap()[:, :], W=[b_c])
            mm(pm[1][:, 0:8], rows[:], gsel[:], [b_rows, b_c], [b_pm[1]])
            cp(gate_sb[:], pm[1][:, 0:8], [b_pm[1]], [b_mod])
            k.barrier()
            es_p.close()

            es_p = ExitStack()
            cur[0] = es_p
            TT = 512
            xs = [sb("xs%d" % i, [128, D]) for i in range(4)]
            b_xs = k.bufs(4, "xs")
            junk = sb("junk", [128, D], BF16)
            b_junk = k.buf("junk")
            hT = sb("hT", [128, NKC, TT], BF16)
            b_hT = k.buf("hT")
            wb = [sb("wb%d" % i, [128, NKC, 128], BF16) for i in range(3)]
            b_wb = k.bufs(3, "wb")
            ob = [sb("ob%d" % i, [128, TT]) for i in range(2)]
            b_ob = k.bufs(2, "ob")
            wsm = sb("wsm", [128, NKC, NSMALL], BF16)
            b_wsm = k.buf("wsm")
            osm = sb("osm", [128, 4, NSMALL])
            b_osm = k.buf("osm")
            st = sb("stats", [128, 8])
            b_st = k.buf("st")
            ptr = [ps("ptr%d" % i, [128, 512]) for i in range(2)]
            b_ptr = k.bufs(2, "ptr")
            pp = [ps("pp%d" % i, [128, 512]) for i in range(2)]
            b_pp = k.bufs(2, "pp")
            psm = ps("psm", [128, 512])
            b_psm = k.buf("psm")
            b_projT = k.buf("projT")
            b_small = k.buf("small")
            stg_alloc(2)
            cast_load(wsm[:].rearrange("p a b -> p (a b)"), wsm_t.ap()[:, :], [b_wsm])
            n_tt = S // TT

            def chunk_func(ch):
                if 14 <= ch < 18 or 30 <= ch < 34:
                    return AF.Silu
                if ch >= 34:
                    return AF.Sigmoid
                return AF.Identity
            epi = 0
            for tt_i in range(n_tt):
                t0 = tt_i * TT
                for sub in range(4):
                    k.dma("sp", xs[sub][:], x_t.ap()[t0 + sub * 128:t0 + (sub + 1) * 128, :], W=[b_xs[sub]])
                    act(junk[:], xs[sub][:], AF.Square, [b_xs[sub]], [b_junk, b_st], accum_out=st[:, sub:sub + 1])
                ts(st[:, 0:4], st[:, 0:4], 1.0 / D, EPS, ALU.mult, ALU.add, [b_st], [b_st])
                act(st[:, 0:4], st[:, 0:4], AF.Sqrt, [b_st], [b_st])
                k.op("dve", "reciprocal", R=[b_st], W=[b_st], out=st[:, 4:8], in_=st[:, 0:4])
                for sub in range(4):
                    ts(xs[sub][:], xs[sub][:], st[:, 4 + sub:5 + sub], None, ALU.mult, None,
                       [b_xs[sub], b_st], [b_xs[sub]])
                for kc in range(NKC):
                    i = kc % 2
                    for sub in range(4):
                        tr(ptr[i][:, sub * 128:(sub + 1) * 128], xs[sub][:, kc * 128:(kc + 1) * 128], ident[:],
                           [b_xs[sub]], [b_ptr[i]])
                    act(hT[:, kc, :], ptr[i][:], AF.Identity, [b_ptr[i]], [b_hT],
                        scale=a_sb[:, kc:kc + 1], bias=shift_sb[:, kc:kc + 1])
                for sub in range(4):
                    for kc in range(NKC):
                        mm(psm[:, sub * 32:sub * 32 + NSMALL], hT[:, kc, sub * 128:(sub + 1) * 128], wsm[:, kc, :],
                           [b_hT, b_wsm], [b_psm], start=(kc == 0), stop=(kc == NKC - 1))
                cp(osm[:], psm[:, 0:128].rearrange("p (a b) -> p a b", b=32)[:, :, 0:NSMALL], [b_psm], [b_osm])
                k.dma("act", small_t.ap()[t0:t0 + TT, :].rearrange("(a p) n -> p a n", p=128), osm[:],
                      R=[b_osm], W=[])
                for ch in range(NCHUNK):
                    wi = epi % 3
                    pi = epi % 2
                    epi += 1
                    cast_load(wb[wi][:].rearrange("p a b -> p (a b)"), win_t.ap()[ch, :, :], [b_wb[wi]])
                    for kc in range(NKC):
                        mm(pp[pi][:], wb[wi][:, kc, :], hT[:, kc, :], [b_hT, b_wb[wi]], [b_pp[pi]],
                           start=(kc == 0), stop=(kc == NKC - 1))
                    act(ob[pi][:], pp[pi][:], chunk_func(ch), [b_pp[pi]], [b_ob[pi]])
                    k.dma("act", projT_t.ap()[ch * 128:(ch + 1) * 128, t0:t0 + TT], ob[pi][:],
                          R=[b_ob[pi]], W=[])
            k.barrier()
            es_p.close()
            if STOP_AFTER == "proj":
                finish()
                return

            es_n = ExitStack()
            cur[0] = es_n
            qT = sb("qT", [128, 8, S], BF16)
            kTs = sb("kTs", [128, S], BF16)
            kTw = sb("kTw", [128, S], BF16)
            Vs = sb("Vs", [128, S // 128, 128], BF16)
            Vw = sb("Vw", [128, S // 128, 128], BF16)
            kcmpT = sb("kcmpT", [128, 256], BF16)
            vcmp = sb("vcmp", [128, 2, 128], BF16)
            es_q = ExitStack()
            cur[0] = es_q
            kcT = sb("kcT", [128, S], BF16)
            vcT = sb("vcT", [128, S], BF16)
            cosT = sb("cosT", [32, S])
            sinT = sb("sinT", [32, S])
            HS = S // 2
            es_r = ExitStack()
            cur[0] = es_r
            posi = sb("posi", [32, HS], I32)
            ang = sb("ang", [32, HS])
            rtmp = sb("rtmp", [32, HS])
            rtmp2 = sb("rtmp2", [32, HS])
            rtmpi = sb("rtmpi", [32, HS], I32)
            b_r = k.buf("rope")
            RW = dict(R=[b_r], W=[b_r])
            for hf in range(2):
                hs = slice(hf * HS, (hf + 1) * HS)
                k.dma("sp", posi[:], pos_t.ap()[0:1, hs].to_broadcast([32, HS]), W=[b_r])
                cp(ang[:], posi[:], [b_r], [b_r])
                ts(ang[:], ang[:], invf[0:32, :], None, ALU.mult, None, [b_r], [b_r])
                for tab, off in ((sinT, 0.0), (cosT, float(np.pi / 2))):
                    ts(rtmp[:], ang[:], 1.0 / TWO_PI, off / TWO_PI, ALU.mult, ALU.add, [b_r], [b_r])
                    cp(rtmpi[:], rtmp[:], [b_r], [b_r])
                    cp(rtmp[:], rtmpi[:], [b_r], [b_r])
                    stt(rtmp[:], rtmp[:], -TWO_PI, ang[:], ALU.mult, ALU.add, [b_r], [b_r])
                    if off:
                        ts(rtmp[:], rtmp[:], off, None, ALU.add, None, [b_r], [b_r])
                    ts(rtmp2[:], rtmp[:], float(np.pi), -TWO_PI, ALU.is_gt, ALU.mult, [b_r], [b_r])
                    tt(rtmp[:], rtmp[:], rtmp2[:], ALU.add, [b_r], [b_r])
                    ts(rtmp2[:], rtmp[:], -float(np.pi), TWO_PI, ALU.is_lt, ALU.mult, [b_r], [b_r])
                    tt(rtmp[:], rtmp[:], rtmp2[:], ALU.add, [b_r], [b_r])
                    ts(rtmp[:], rtmp[:], float(np.pi), -float(np.pi), ALU.min, ALU.max, [b_r], [b_r])
                    act(tab[:, hs], rtmp[:], AF.Sin, [b_r], [b_r])
            k.barrier()
            es_r.close()
            cur[0] = es_q
            lb = [sb("lb%d" % i, [128, 512]) for i in range(3)]
            b_lb = k.bufs(3, "lb")
            rt1 = [sb("rt1_%d" % i, [32, 512]) for i in range(2)]
            rt2 = [sb("rt2_%d" % i, [32, 512]) for i in range(2)]
            b_rt = k.bufs(2, "rt")
            pr = [ps("pr%d" % i, [128, 512]) for i in range(2)]
            b_pr = k.bufs(2, "pr")
            b_dst = k.buf("nsadst")
            b_qdbg = k.buf("qdbg")
            li = 0
            plan = [(h, "rope", qT[:, h, :]) for h in range(8)]
            plan += [(8, "cast", kcT[:]), (9, "cast", vcT[:]), (10, "rope", kTs[:]), (11, "vT", Vs),
                     (12, "rope", kTw[:]), (13, "vT", Vw)]
            for ch, kind, dst in plan:
                for tt_i in range(S // 512):
                    t0 = tt_i * 512
                    a = li % 3
                    r = li % 2
                    li += 1
                    k.dma("sp", lb[a][:], projT_t.ap()[ch * 128:(ch + 1) * 128, t0:t0 + 512], W=[b_lb[a]])
                    if kind == "cast":
                        cp(dst[:, t0:t0 + 512], lb[a][:], [b_lb[a]], [b_dst], eng="act")
                    elif kind == "rope":
                        mm(pr[r][0:32, :], rotT[:, :], lb[a][0:32, :], [b_lb[a]], [b_pr[r]])
                        cp(dst[:, t0:t0 + 512], lb[a][:], [b_lb[a]], [b_dst], eng="act")
                        tt(rt1[r][:], lb[a][0:32, :], cosT[:, t0:t0 + 512], ALU.mult, [b_lb[a]], [b_rt[r]])
                        tt(rt2[r][:], pr[r][0:32, :], sinT[:, t0:t0 + 512], ALU.mult, [b_pr[r]], [b_rt[r]])
                        tt(dst[0:32, t0:t0 + 512], rt1[r][:], rt2[r][:], ALU.add, [b_rt[r]], [b_dst])
                    else:
                        for sub in range(4):
                            tr(pr[r][:, sub * 128:(sub + 1) * 128], lb[a][:, sub * 128:(sub + 1) * 128], ident[:],
                               [b_lb[a]], [b_pr[r]])
                        cp(dst[:, tt_i * 4:(tt_i + 1) * 4, :], pr[r][:].rearrange("p (a b) -> p a b", b=128),
                           [b_pr[r]], [b_dst])
            if qr_dbg is not None:
                qf = sb("qf_dbg", [128, S])
                cp(qf[:], qT[:, 0, :], [b_dst], [b_qdbg])
                k.dma("sp", qr_dbg.ap()[:, :], qf[:], R=[b_qdbg], W=[])
            k.barrier()

            NCMP = (S - 32) // 16 + 1
            stg_alloc(1, 2048)
            w1 = sb("w1", [128, 32, 256], BF16)
            w2 = sb("w2", [128, 2, 128], BF16)
            posT = sb("posT", [128, 32])
            posTb = sb("posTb", [128, 32], BF16)
            hid = sb("hid", [128, 2, 256], BF16)
            pbs = sb("pbs", [128, 2])
            kf = sb("kf", [128, 256])
            b_w = k.buf("cmpw")
            b_hid = k.buf("hid")
            b_kf = k.buf("kf")
            b_cmp = k.buf("cmpout")
            ph = pr[0]
            pb_ = pr[1]
            b_ph, b_pb = b_pr
            k.op("dve", "memset", W=[b_hid], ap=hid[:], constant=0.0)
            k.op("dve", "memset", W=[b_cmp], ap=kcmpT[:], constant=0.0)
            for which in range(2):
                w1_t = (w1k_t, w1v_t)[which]
                w2_t = (w2k_t, w2v_t)[which]
                cps_t = (cposk_t, cposv_t)[which]
                src = (kcT, vcT)[which]
                srcv = src[:].rearrange("p (n s) -> p n s", s=16)
                w1v = w1_t.ap().rearrange("(l d) h -> d l h", d=128)
                for a in range(4):
                    cast_load(w1[:, 8 * a:8 * a + 8, :], w1v[:, 8 * a:8 * a + 8, :], [b_w], inner=256)
                cast_load(w2[:], w2_t.ap().rearrange("(hc p) d -> p hc d", p=128), [b_w], inner=128)
                k.dma("sp", posT[:], cps_t.ap()[:, :], W=[b_w])
                cp(posTb[:], posT[:], [b_w], [b_w])
                for hc in range(2):
                    for l in range(32):
                        mm(pb_[:, 0:1], w1[:, l, hc * 128:(hc + 1) * 128], posTb[:, l:l + 1], [b_w], [b_pb],
                           start=(l == 0), stop=(l == 31))
                    cp(pbs[:, hc:hc + 1], pb_[:, 0:1], [b_pb], [b_hid])
                    for l in range(32):
                        rhs = srcv[:, 0:NCMP, l] if l < 16 else srcv[:, 1:NCMP + 1, l - 16]
                        mm(ph[:, 0:NCMP], w1[:, l, hc * 128:(hc + 1) * 128], rhs, [b_w], [b_ph],
                           start=(l == 0), stop=(l == 31))
                    act(hid[:, hc, 0:NCMP], ph[:, 0:NCMP], AF.Silu, [b_ph, b_hid], [b_hid], bias=pbs[:, hc:hc + 1])
                if which == 0:
                    for hc in range(2):
                        mm(ph[:, 0:NCMP], w2[:, hc, :], hid[:, hc, 0:NCMP], [b_w, b_hid], [b_ph],
                           start=(hc == 0), stop=(hc == 1))
                    cp(kf[:, 0:NCMP], ph[:, 0:NCMP], [b_ph], [b_kf])
                    mm(pb_[0:32, 0:NCMP], rotT[:, :], kf[0:32, 0:NCMP], [b_kf], [b_pb])
                    cosv = cosT[:].rearrange("p (n s) -> p n s", s=16)[:, 1:NCMP + 1, 15]
                    sinv = sinT[:].rearrange("p (n s) -> p n s", s=16)[:, 1:NCMP + 1, 15]
                    cp(kcmpT[:, 0:NCMP], kf[:, 0:NCMP], [b_kf], [b_cmp], eng="act")
                    tt(rt1[0][:, 0:NCMP], kf[0:32, 0:NCMP], cosv, ALU.mult, [b_kf], [b_rt[0]])
                    tt(rt2[0][:, 0:NCMP], pb_[0:32, 0:NCMP], sinv, ALU.mult, [b_pb], [b_rt[0]])
                    tt(kcmpT[0:32, 0:NCMP], rt1[0][:, 0:NCMP], rt2[0][:, 0:NCMP], ALU.add, [b_rt[0]], [b_cmp])
                    if kc_dbg is not None:
                        kfd = sb("kfd", [128, 512])
                        k.op("dve", "memset", W=[b_kf], ap=kfd[:], constant=0.0)
                        cp(kfd[:, 0:256], kcmpT[:], [b_cmp], [b_kf])
                else:
                    for nt in range(2):
                        for hc in range(2):
                            mm(ph[:, 0:128], hid[:, hc, nt * 128:(nt + 1) * 128], w2[:, hc, :], [b_w, b_hid], [b_ph],
                               start=(hc == 0), stop=(hc == 1))
                        cp(vcmp[:, nt, :], ph[:, 0:128], [b_ph], [b_cmp])
                        if kc_dbg is not None:
                            cp(kfd[:, 256 + nt * 128:256 + (nt + 1) * 128], ph[:, 0:128], [b_ph], [b_kf])
            if kc_dbg is not None:
                k.dma("sp", kc_dbg.ap()[:, :], kfd[:], R=[b_kf], W=[])
            k.barrier()
            es_q.close()

            es_a = ExitStack()
            cur[0] = es_a
            pS = [ps("pS%d" % i, [128, 512]) for i in range(2)]
            b_pS = k.bufs(2, "pS")
            pT = [ps("pT%d" % i, [128, 512], BF16) for i in range(2)]
            b_pT = k.bufs(2, "pT")
            pO = [ps("pO%d" % i, [128, 512]) for i in range(2)]
            b_pO = k.bufs(2, "pO")
            pI = ps("pI", [128, 512])
            b_pI = k.buf("pI")
            pX = ps("pX", [128, 512])
            b_pX = k.buf("pX")
            Pf = [sb("Pf%d" % i, [128, 512], BF16) for i in range(2)]
            b_Pf = k.bufs(2, "Pf")
            PTs = [sb("PTs%d" % i, [128, 512], BF16) for i in range(2)]
            b_PTs = k.bufs(2, "PTs")
            acc = sb("acc", [128, 256])
            b_acc = k.buf("acc")
            accT = sb("accT", [128, 2, 128])
            b_accT = k.buf("accT")
            cm = sb("cm", [128, 256], BF16)
            b_cm = k.buf("cm")
            vm = sb("vm", [128, 64])
            fb = sb("fb", [128, 64])
            b_vf = k.buf("vf")
            score = sb("score", [128, 64])
            sc2 = sb("sc2", [128, 64])
            m8 = sb("m8", [128, 8])
            m8b = sb("m8b", [128, 8])
            sel = sb("sel", [128, 64], BF16)
            b_sel = k.buf("sel")
            gts = sb("gts", [128, 12])
            b_gts = k.buf("gts")
            oacc = [sb("oacc%d" % i, [128, 128]) for i in range(4)]
            b_oacc = k.bufs(4, "oacc")
            zt = sb("zt", [128, 4, 128])
            b_zt = k.buf("zt")
            oT = sb("oT", [128, 4, 128], BF16)
            b_oT = k.buf("oT")
            oTf = sb("oTf", [128, 4, 128]) if oa_dbg is not None else None
            stt_ = sb("stt", [128, 256])
            b_stc = k.bufs(256, "stc")
            stc_i = [0]
            mxt = [sb("mxt%d" % i, [128, 16]) for i in range(4)]
            b_mxt = k.bufs(4, "mxt")
            mxt_i = [0]
            b_oab = k.buf("oab")
            tog = {"s": 0, "o": 0}

            def stat():
                c = stc_i[0] % 256
                stc_i[0] += 1
                return stt_[:, c:c + 1], b_stc[c]

            def finish_branch(h, o, rs_ap, b_rs, gcol, first):
                ri, b_ri = stat()
                ts(ri, rs_ap, 1e-30, None, ALU.max, None, [b_rs], [b_ri])
                k.op("dve", "reciprocal", R=[b_ri], W=[b_ri], out=ri, in_=ri)
                cf, b_cf = stat()
                tt(cf, ri, gts[:, gcol:gcol + 1], ALU.mult, [b_ri, b_gts], [b_cf])
                if first:
                    ts(oacc[h][:], pO[o][:, 0:128], cf, None, ALU.mult, None, [b_pO[o], b_cf], [b_oacc[h]])
                else:
                    stt(oacc[h][:], pO[o][:, 0:128], cf, oacc[h][:], ALU.mult, ALU.add,
                        [b_pO[o], b_cf, b_oacc[h]], [b_oacc[h]])
                return ri, b_ri

            def branch(qi, h, blocks, kT_, V_, use_sel, wfirst, gcol, slot):
                t0 = qi * 128
                tiles = [blocks[a:a + 4] for a in range(0, len(blocks), 4)]
                mi = mxt_i[0] % 4
                mxt_i[0] += 1
                for ti, tl in enumerate(tiles):
                    w = 128 * len(tl)
                    k0 = tl[0] * 128
                    s = slot
                    mm(pS[s][:, 0:w], qT[:, h, t0:t0 + 128], kT_[:, k0:k0 + w], [], [b_pS[s]])
                    k.op("dve", "reduce_max", R=[b_pS[s]], W=[b_mxt[mi]], out=mxt[mi][:, ti:ti + 1],
                         in_=pS[s][:, 0:w], axis=AX.X)
                    yield
                nm, b_nm = stat()
                k.op("dve", "reduce_max", R=[b_mxt[mi]], W=[b_nm], out=nm, in_=mxt[mi][:, 0:len(tiles)], axis=AX.X)
                ts(nm, nm, -SCALE, None, ALU.mult, None, [b_nm], [b_nm])
                o = slot
                nblk = len(blocks)
                bdone = 0
                for ti, tl in enumerate(tiles):
                    w = 128 * len(tl)
                    k0 = tl[0] * 128
                    s = slot
                    mm(pS[s][:, 0:w], qT[:, h, t0:t0 + 128], kT_[:, k0:k0 + w], [], [b_pS[s]])
                    act(Pf[s][:, 0:w], pS[s][:, 0:w], AF.Exp, [b_pS[s], b_nm], [b_Pf[s]], scale=SCALE, bias=nm)
                    yield
                    for bi, blk_ in enumerate(tl):
                        sl = slice(bi * 128, (bi + 1) * 128)
                        if blk_ == qi:
                            tt(Pf[s][:, sl], Pf[s][:, sl], tri_b[:], ALU.mult, [b_Pf[s]], [b_Pf[s]])
                        elif wfirst is not None and blk_ == wfirst:
                            tt(Pf[s][:, sl], Pf[s][:, sl], triw_b[:], ALU.mult, [b_Pf[s]], [b_Pf[s]])
                    nb2 = 2 * len(tl)
                    if use_sel:
                        in1 = sel[:, 2 * tl[0]:2 * tl[0] + nb2].unsqueeze(2).to_broadcast([128, nb2, 64])
                        pv_ = Pf[s][:, 0:w].rearrange("p (a b) -> p a b", b=64)
                        stt(pv_, pv_, 1.0, in1, ALU.mult, ALU.mult, [b_Pf[s], b_sel], [b_Pf[s], b_mxt[mi]],
                            accum_out=mxt[mi][:, 8 + ti:9 + ti])
                    else:
                        stt(Pf[s][:, 0:w], Pf[s][:, 0:w], 1.0, ones_b[:, 0:w], ALU.mult, ALU.mult,
                            [b_Pf[s]], [b_Pf[s], b_mxt[mi]], accum_out=mxt[mi][:, 8 + ti:9 + ti])
                    yield
                    for bi in range(len(tl)):
                        sl = slice(bi * 128, (bi + 1) * 128)
                        tr(pT[s][:, sl], Pf[s][:, sl], identb[:], [b_Pf[s]], [b_pT[s]])
                    cp(PTs[s][:, 0:w], pT[s][:, 0:w], [b_pT[s]], [b_PTs[s]], eng="act")
                    yield
                    for bi, blk_ in enumerate(tl):
                        sl = slice(bi * 128, (bi + 1) * 128)
                        mm(pO[o][:, 0:128], PTs[s][:, sl], V_[:, blk_, :], [b_PTs[s]], [b_pO[o]],
                           start=(bdone == 0), stop=(bdone == nblk - 1))
                        bdone += 1
                rs, b_rs = stat()
                k.op("dve", "reduce_sum", R=[b_mxt[mi]], W=[b_rs], out=rs, in_=mxt[mi][:, 8:8 + len(tiles)], axis=AX.X)
                finish_branch(h, o, rs, b_rs, gcol, False)
                yield

            for qi in range(S // 128):
                t0 = qi * 128
                k.dma("sp", gts[:], small_t.ap()[t0:t0 + 128, 0:12], W=[b_gts])
                act(gts[:], gts[:], AF.Sigmoid, [b_gts], [b_gts])
                k.dma("sp", vm[:], vmask_t.ap()[qi, :, :], W=[b_vf])
                k.dma("sp", fb[:], fbias_t.ap()[qi, :, :], W=[b_vf])
                k.dma("sp", zt[:], projT_t.ap()[14 * 128:18 * 128, t0:t0 + 128].rearrange("(h p) t -> p h t", p=128),
                      W=[b_zt])
                ts(cm[:], cval, float(128 * qi), None, ALU.is_le, None, [], [b_cm])
                for h in range(8):
                    s = tog["s"]
                    tog["s"] ^= 1
                    mm(pS[s][:, 0:256], qT[:, h, t0:t0 + 128], kcmpT[:, :], [], [b_pS[s]])
                    nm, b_nm = stat()
                    k.op("dve", "reduce_max", R=[b_pS[s]], W=[b_nm], out=nm, in_=pS[s][:, 0:256], axis=AX.X)
                    ts(nm, nm, -SCALE, None, ALU.mult, None, [b_nm], [b_nm])
                    act(Pf[s][:, 0:256], pS[s][:, 0:256], AF.Exp, [b_pS[s], b_nm], [b_Pf[s]], scale=SCALE, bias=nm)
                    rs, b_rs = stat()
                    stt(Pf[s][:, 0:256], Pf[s][:, 0:256], 1.0, cm[:], ALU.mult, ALU.mult, [b_Pf[s], b_cm],
                        [b_Pf[s], b_rs], accum_out=rs)
                    if h < 4:
                        for ct in range(2):
                            sl = slice(ct * 128, (ct + 1) * 128)
                            tr(pT[s][:, sl], Pf[s][:, sl], identb[:], [b_Pf[s]], [b_pT[s]])
                        cp(PTs[s][:, 0:256], pT[s][:, 0:256], [b_pT[s]], [b_PTs[s]], eng="act")
                        o = tog["o"]
                        tog["o"] ^= 1
                        for ct in range(2):
                            sl = slice(ct * 128, (ct + 1) * 128)
                            mm(pO[o][:, 0:128], PTs[s][:, sl], vcmp[:, ct, :], [b_PTs[s]], [b_pO[o]],
                               start=(ct == 0), stop=(ct == 1))
                        ri, b_ri = finish_branch(h, o, rs, b_rs, h, True)
                    else:
                        ri, b_ri = stat()
                        ts(ri, rs, 1e-30, None, ALU.max, None, [b_rs], [b_ri])
                        k.op("dve", "reciprocal", R=[b_ri], W=[b_ri], out=ri, in_=ri)
                    if h == 0:
                        ts(acc[:], Pf[s][:, 0:256], ri, None, ALU.mult, None, [b_Pf[s], b_ri], [b_acc])
                    else:
                        stt(acc[:], Pf[s][:, 0:256], ri, acc[:], ALU.mult, ALU.add, [b_Pf[s], b_ri, b_acc], [b_acc])
                for ct in range(2):
                    sl = slice(ct * 128, (ct + 1) * 128)
                    tr(pX[:, sl], acc[:, sl], ident[:], [b_acc], [b_pX])
                cp(accT[:], pX[:, 0:256].rearrange("p (a b) -> p a b", b=128), [b_pX], [b_accT])
                for ct in range(2):
                    mm(pI[:, 0:64], accT[:, ct, :], ovl[:, ct, :], [b_accT], [b_pI], start=(ct == 0), stop=(ct == 1))
                tt(score[:], pI[:, 0:64], vm[:], ALU.mult, [b_pI, b_vf], [b_sel])
                tt(score[:], score[:], fb[:], ALU.add, [b_sel, b_vf], [b_sel])
                k.op("dve", "max", R=[b_sel], W=[b_sel], out=m8[:], in_=score[:])
                k.op("dve", "match_replace", R=[b_sel], W=[b_sel], out=sc2[:], in_to_replace=m8[:],
                     in_values=score[:], imm_value=-2.0)
                k.op("dve", "max", R=[b_sel], W=[b_sel], out=m8b[:], in_=sc2[:])
                ts(sel[:], score[:], m8b[:, 7:8], None, ALU.is_ge, None, [b_sel], [b_sel])
                tasks = []
                for h in range(4):
                    tasks.append(lambda slot, h=h: branch(qi, h, list(range(0, qi + 1)), kTs, Vs, True, None, 4 + h, slot))
                for h in range(4):
                    tasks.append(lambda slot, h=h: branch(qi, h, list(range(max(0, qi - 4), qi + 1)), kTw, Vw, False,
                                                          (qi - 4) if qi >= 4 else None, 8 + h, slot))
                active = [None, None]
                ti_ = 0
                while True:
                    for sl_ in range(2):
                        if active[sl_] is None and ti_ < len(tasks):
                            active[sl_] = tasks[ti_](sl_)
                            ti_ += 1
                    if active[0] is None and active[1] is None:
                        break
                    for sl_ in range(2):
                        if active[sl_] is not None:
                            try:
                                next(active[sl_])
                            except StopIteration:
                                active[sl_] = None
                for h in range(4):
                    tr(pX[:, h * 128:(h + 1) * 128], oacc[h][:], ident[:], [b_oacc[h]], [b_pX])
                tt(oT[:], pX[:].rearrange("p (a b) -> p a b", b=128), zt[:], ALU.mult, [b_pX, b_zt], [b_oT])
                k.dma("sp", oa_b[t0 // CW].ap().rearrange("(h p) t -> p h t", p=128)[:, :, t0 % CW:t0 % CW + 128], oT[:],
                      R=[b_oT], W=[])
                if oa_dbg is not None:
                    tt(oTf[:], pX[:].rearrange("p (a b) -> p a b", b=128), zt[:], ALU.mult, [b_pX, b_zt], [b_oT])
                    k.dma("sp", oa_dbg.ap().rearrange("(h p) t -> p h t", p=128)[:, :, t0:t0 + 128], oTf[:],
                          R=[b_oT], W=[])
            k.barrier()
            es_a.close()
            es_n.close()
            if STOP_AFTER == "nsa":
                finish()
                return

            es_d = ExitStack()
            cur[0] = es_d
            cw = sb("cw", [128, 12, 4])
            dnc = sb("dnc", [64, 136])
            b_dc = k.buf("dnconst")
            k.dma("sp", cw[:].rearrange("p a b -> p (a b)"), convw_t.ap()[:, :], W=[b_dc])
            k.dma("sp", dnc[:], dnc_t.ap()[:, :], W=[b_dc])
            g_sb = sb("g_sb", [64, S // 64, 4])
            beta_sb = sb("beta_sb", [64, S // 64, 4])
            gb_raw = sb("gb_raw", [64, S // 64, 8])
            nea = sb("nea", [64, 4])
            es_dp = ExitStack()
            cur[0] = es_dp
            xb = [sb("xb%d" % i, [128, 515]) for i in range(2)]
            b_xb = k.bufs(2, "xb")
            yb = [sb("yb%d" % i, [128, 512]) for i in range(2)]
            b_yb = k.bufs(2, "yb")
            sq = sb("sq", [128, 512])
            rn = sb("rn", [128, 512])
            b_sq = k.buf("sq")
            pss = ps("pss", [128, 512])
            b_pss = k.buf("pss")
            b_dnq = k.buf("dnq")
            li = 0
            for ci in range(12):
                ch = 18 + ci
                for tt_i in range(S // 512):
                    t0 = tt_i * 512
                    a = li % 2
                    li += 1
                    if tt_i == 0:
                        k.op("dve", "memset", W=[b_xb[a]], ap=xb[a][:, 0:3], constant=0.0)
                        k.dma("sp", xb[a][:, 3:515], projT_t.ap()[ch * 128:(ch + 1) * 128, 0:512], W=[b_xb[a]])
                    else:
                        k.dma("sp", xb[a][:, 0:515], projT_t.ap()[ch * 128:(ch + 1) * 128, t0 - 3:t0 + 512],
                              W=[b_xb[a]])
                    ts(yb[a][:], xb[a][:, 3:515], cw[:, ci, 3:4], None, ALU.mult, None, [b_xb[a], b_dc], [b_yb[a]])
                    for i in (2, 1, 0):
                        stt(yb[a][:], xb[a][:, i:i + 512], cw[:, ci, i:i + 1], yb[a][:], ALU.mult, ALU.add,
                            [b_xb[a], b_dc, b_yb[a]], [b_yb[a]])
                    act(yb[a][:], yb[a][:], AF.Silu, [b_yb[a]], [b_yb[a]])
                    if ci < 8:
                        act(sq[:], yb[a][:], AF.Square, [b_yb[a]], [b_sq])
                        mm(pss[:], ones[:, :], sq[:], [b_sq], [b_pss])
                        ts(rn[:], pss[:], EPS, None, ALU.add, None, [b_pss], [b_sq])
                        act(rn[:], rn[:], AF.Sqrt, [b_sq], [b_sq])
                        k.op("dve", "reciprocal", R=[b_sq], W=[b_sq], out=rn[:], in_=rn[:])
                        if ci < 4:
                            stt(yb[a][:], yb[a][:], SCALE, rn[:], ALU.mult, ALU.mult, [b_yb[a], b_sq], [b_yb[a]])
                        else:
                            tt(yb[a][:], yb[a][:], rn[:], ALU.mult, [b_yb[a], b_sq], [b_yb[a]])
                    k.dma("sp", dnq_t.ap()[ci * 128:(ci + 1) * 128, t0:t0 + 512], yb[a][:], R=[b_yb[a]], W=[])
            b_g = k.buf("g")
            k.dma("sp", gb_raw[:], small_t.ap().rearrange("(c p) n -> p c n", p=64)[:, :, 12:20], W=[b_g])
            tt(g_sb[:], gb_raw[:, :, 0:4], dnc[:, 0:4].unsqueeze(1).to_broadcast([64, S // 64, 4]), ALU.add,
               [b_g, b_dc], [b_g])
            act(g_sb[:], g_sb[:], AF.Exp, [b_g], [b_g])
            ts(g_sb[:], g_sb[:], 1.0, None, ALU.add, None, [b_g], [b_g])
            act(g_sb[:], g_sb[:], AF.Ln, [b_g], [b_g])
            act(nea[:], dnc[:, 4:8], AF.Exp, [b_dc], [b_g])
            ts(nea[:], nea[:], -1.0, None, ALU.mult, None, [b_g], [b_g])
            tt(g_sb[:], g_sb[:], nea[:].unsqueeze(1).to_broadcast([64, S // 64, 4]), ALU.mult, [b_g], [b_g])
            act(beta_sb[:], gb_raw[:, :, 4:8], AF.Sigmoid, [b_g], [b_g])
            k.barrier()
            es_dp.close()
            cur[0] = es_d
            St = [sb("St%d" % i, [128, 128]) for i in range(4)]
            b_St = k.bufs(4, "St")
            for i in range(4):
                k.op("dve", "memset", W=[b_St[i]], ap=St[i][:], constant=0.0)
            qc = [sb("qc%d" % i, [128, 4, 64]) for i in range(2)]
            kc_ = [sb("kc%d" % i, [128, 4, 64]) for i in range(2)]
            vc_ = [sb("vc%d" % i, [128, 4, 64]) for i in range(2)]
            dzt = [sb("dzt%d" % i, [128, 4, 64]) for i in range(2)]
            b_ld = k.bufs(2, "dnld")
            obT = [sb("obT%d" % i, [128, 4, 64], BF16) for i in range(2)]
            b_obT = k.bufs(2, "obT")
            obTf = [sb("obTf%d" % i, [128, 4, 64]) for i in range(2)] if ob_dbg is not None else None
            NTMP = 176
            tmps = [sb("tmp%d" % i, [128, 128]) for i in range(NTMP)]
            b_tmps = k.bufs(NTMP, "tmp")
            tmp_i = [0]
            banks = [ps("dnp%d" % i, [128, 512]) for i in range(7)]
            b_banks = k.bufs(7, "dnbank")
            for b_ in b_banks:
                b_.excl = True
            b_slots = [b_banks[i // 4] for i in range(28)]
            slot_i = [0]
            b_obb = k.buf("obb")

            def tmp():
                i = tmp_i[0] % NTMP
                tmp_i[0] += 1
                return tmps[i], b_tmps[i]

            def pslot():
                i = slot_i[0] % 28
                slot_i[0] += 1
                return banks[i // 4][:, (i % 4) * 128:(i % 4 + 1) * 128], b_slots[i]

            I64 = ident[0:64, 0:64]
            H = slice(0, 64)
            dnq_v = dnq_t.ap().rearrange("(m h p) t -> m p h t", m=3, p=128)
            def dn_load(c_):
                a_ = c_ % 2
                cs_ = slice(c_ * 64, (c_ + 1) * 64)
                k.dma("sp", qc[a_][:], dnq_v[0][:, :, cs_], W=[b_ld[a_]])
                k.dma("sp", kc_[a_][:], dnq_v[1][:, :, cs_], W=[b_ld[a_]])
                k.dma("sp", vc_[a_][:], dnq_v[2][:, :, cs_], W=[b_ld[a_]])
                k.dma("sp", dzt[a_][:], projT_t.ap()[30 * 128:34 * 128, cs_].rearrange("(h p) t -> p h t", p=128),
                      W=[b_ld[a_]])

            dn_load(0)
            for c in range(S // 64):
                a = c % 2
                cs = slice(c * 64, (c + 1) * 64)
                if c + 1 < S // 64:
                    dn_load(c + 1)
                def head_body(hh, c=c, a=a, cs=cs):
                    L = [b_ld[a]]
                    gc = g_sb[:, c, hh:hh + 1]
                    bc = beta_sb[:, c, hh:hh + 1]
                    kT_h = kc_[a][:, hh, :]
                    qT_h = qc[a][:, hh, :]
                    vT_h = vc_[a][:, hh, :]
                    Gm, bGm = tmp()
                    ts(Gm[H, 0:64], lowst, gc, None, ALU.mult, None, [], [bGm])
                    cp(Gm[H, 64:65], gc, [], [bGm])
                    pDT, bDT = pslot()
                    mm(pDT[H, 0:64], Gm[H, 0:64], tri_le, [bGm], [bDT])
                    pDN, bDN = pslot()
                    mm(pDN[H, 0:65], tri_le, Gm[H, 0:65], [bGm], [bDN])
                    pgl, bgl = pslot()
                    mm(pgl[:, 0:1], ones[H, :], Gm[H, 64:65], [bGm], [bgl])
                    yield
                    ET, bET = tmp()
                    act(ET[H, 0:64], pDT[H, 0:64], AF.Exp, [bDT], [bET])
                    tt(ET[H, 0:64], ET[H, 0:64], tri_le, ALU.mult, [bET], [bET])
                    EN, bEN = tmp()
                    act(EN[H, 0:64], pDN[H, 0:64], AF.Exp, [bDN], [bEN])
                    ENo, bENo = tmp()
                    tt(ENo[H, 0:64], EN[H, 0:64], lowo, ALU.mult, [bEN], [bENo])
                    tt(EN[H, 0:64], EN[H, 0:64], lowd, ALU.mult, [bEN], [bEN])
                    yield
                    sc, bsc = tmp()
                    act(sc[H, 0:1], pDN[H, 64:65], AF.Exp, [bDN], [bsc])
                    cp(sc[H, 6:7], pDN[H, 64:65], [bDN], [bsc])
                    tt(sc[H, 5:6], pgl[H, 0:1], sc[H, 6:7], ALU.subtract, [bgl, bsc], [bsc])
                    act(sc[H, 1:2], sc[H, 5:6], AF.Exp, [bsc], [bsc])
                    tt(sc[H, 2:3], sc[H, 0:1], bc, ALU.mult, [bsc], [bsc])
                    ts(sc[H, 3:4], bc, -1.0, None, ALU.mult, None, [bsc], [bsc])
                    act(sc[:, 4:5], pgl[:, 0:1], AF.Exp, [bgl, bsc], [bsc])
                    yield
                    pk, bpk = pslot()
                    tr(pk[H, :], kT_h, ident[:], L, [bpk])
                    kbd, bkbd = tmp()
                    ts(kbd[H, :], pk[H, :], sc[H, 2:3], None, ALU.mult, None, [bpk, bsc], [bkbd])
                    kd, bkd = tmp()
                    ts(kd[H, :], pk[H, :], sc[H, 1:2], None, ALU.mult, None, [bpk, bsc], [bkd])
                    pv, bpv = pslot()
                    tr(pv[H, :], vT_h, ident[:], L, [bpv])
                    vb, bvb = tmp()
                    ts(vb[H, :], pv[H, :], bc, None, ALU.mult, None, [bpv], [bvb])
                    yield
                    pG, bpG = pslot()
                    mm(pG[H, 0:64], kT_h, kT_h, L, [bpG])
                    B, bB = tmp()
                    stt(B[H, 0:64], pG[H, 0:64], sc[H, 3:4], EN[H, 0:64], ALU.mult, ALU.mult, [bpG, bsc, bEN], [bB])
                    Bo, bBo = tmp()
                    stt(Bo[H, 0:64], pG[H, 0:64], sc[H, 3:4], ENo[H, 0:64], ALU.mult, ALU.mult, [bpG, bsc, bENo], [bBo])
                    pA, bpA = pslot()
                    mm(pA[H, 0:64], kT_h, qT_h, L, [bpA])
                    At, bAt = tmp()
                    tt(At[H, 0:64], pA[H, 0:64], ET[H, 0:64], ALU.mult, [bpA, bET], [bAt])
                    yield
                    pC, bpC = pslot()
                    tr(pC[H, 0:64], B[H, 0:64], I64, [bB], [bpC])
                    C, bC = tmp()
                    cp(C[H, 0:64], pC[H, 0:64], [bpC], [bC], eng="act")
                    X, bX = tmp()
                    tt(X[H, 0:64], pC[H, 0:64], I64, ALU.add, [bpC], [bX])
                    Bp, bBp, Cp, bCp = B, bB, C, bC
                    for lev in range(1, 5):
                        yield
                        pB2, bpB2 = pslot()
                        mm(pB2[H, 0:64], Cp[H, 0:64], Bp[H, 0:64], [bCp, bBp], [bpB2])
                        nB, bnB = tmp()
                        cp(nB[H, 0:64], pB2[H, 0:64], [bpB2], [bnB], eng="act")
                        if lev < 4:
                            pC2, bpC2 = pslot()
                            mm(pC2[H, 0:64], Bp[H, 0:64], Cp[H, 0:64], [bCp, bBp], [bpC2])
                            nC, bnC = tmp()
                            cp(nC[H, 0:64], pC2[H, 0:64], [bpC2], [bnC])
                        yield
                        pX2, bpX2 = pslot()
                        mm(pX2[H, 0:64], nB[H, 0:64], X[H, 0:64], [bnB, bX], [bpX2])
                        nX, bnX = tmp()
                        tt(nX[H, 0:64], pX2[H, 0:64], X[H, 0:64], ALU.add, [bpX2, bX], [bnX])
                        Bp, bBp, X, bX = nB, bnB, nX, bnX
                        if lev < 4:
                            Cp, bCp = nC, bnC
                    yield
                    pM1, bpM1 = pslot()
                    mm(pM1[H, 0:64], Bo[H, 0:64], X[H, 0:64], [bBo, bX], [bpM1])
                    M1, bM1 = tmp()
                    cp(M1[H, 0:64], pM1[H, 0:64], [bpM1], [bM1], eng="act")
                    pTd, bpTd = pslot()
                    tr(pTd[H, 0:64], X[H, 0:64], I64, [bX], [bpTd])
                    Td, bTd = tmp()
                    cp(Td[H, 0:64], pTd[H, 0:64], [bpTd], [bTd])
                    yield
                    pM2, bpM2 = pslot()
                    mm(pM2[H, 0:64], Td[H, 0:64], M1[H, 0:64], [bTd, bM1], [bpM2])
                    Xf, bXf = tmp()
                    tt(Xf[H, 0:64], pM2[H, 0:64], X[H, 0:64], ALU.add, [bpM2, bX], [bXf])
                    X, bX = Xf, bXf
                    yield
                    pu, bpu = pslot()
                    mm(pu[H, :], X[H, 0:64], vb[H, :], [bX, bvb], [bpu])
                    u, bu = tmp()
                    cp(u[H, :], pu[H, :], [bpu], [bu], eng="act")
                    pw, bpw = pslot()
                    mm(pw[:, 0:64], kbd[H, :], X[H, 0:64], [bX, bkbd], [bpw])
                    wT, bwT = tmp()
                    cp(wT[:, 0:64], pw[:, 0:64], [bpw], [bwT])
                    yield
                    ppv, bppv = pslot()
                    mm(ppv[H, :], wT[:, 0:64], St[hh][:], [bwT, b_St[hh]], [bppv])
                    vn, bvn = tmp()
                    tt(vn[H, :], u[H, :], ppv[H, :], ALU.subtract, [bu, bppv], [bvn])
                    po1, bpo1 = pslot()
                    mm(po1[H, :], qT_h, St[hh][:], L + [b_St[hh]], [bpo1])
                    o1, bo1 = tmp()
                    ts(o1[H, :], po1[H, :], sc[H, 0:1], None, ALU.mult, None, [bpo1, bsc], [bo1])
                    yield
                    po2, bpo2 = pslot()
                    mm(po2[H, :], At[H, 0:64], vn[H, :], [bAt, bvn], [bpo2])
                    o_, bo = tmp()
                    tt(o_[H, :], po2[H, :], o1[H, :], ALU.add, [bpo2, bo1], [bo])
                    yield
                    pSn, bpSn = pslot()
                    mm(pSn[:, :], kd[H, :], vn[H, :], [bkd, bvn], [bpSn])
                    stt(St[hh][:], St[hh][:], sc[:, 4:5], pSn[:, :], ALU.mult, ALU.add,
                        [b_St[hh], bsc, bpSn], [b_St[hh]])
                    yield
                    jk, bjk = tmp()
                    act(jk[H, :], o_[H, :], AF.Square, [bo], [bjk, bsc], accum_out=sc[H, 7:8])
                    ts(sc[H, 7:8], sc[H, 7:8], 1.0 / 128, EPS, ALU.mult, ALU.add, [bsc], [bsc])
                    act(sc[H, 7:8], sc[H, 7:8], AF.Sqrt, [bsc], [bsc])
                    k.op("dve", "reciprocal", R=[bsc], W=[bsc], out=sc[H, 7:8], in_=sc[H, 7:8])
                    on, bon = tmp()
                    stt(on[H, :], o_[H, :], sc[H, 7:8], dnc[:, 8:136], ALU.mult, ALU.mult, [bo, bsc], [bon])
                    yield
                    pot, bpot = pslot()
                    tr(pot[:, 0:64], on[H, :], I64, [bon], [bpot])
                    tt(obT[a][:, hh, :], pot[:, 0:64], dzt[a][:, hh, :], ALU.mult, [bpot] + L, [b_obT[a]])
                    if ob_dbg is not None:
                        tt(obTf[a][:, hh, :], pot[:, 0:64], dzt[a][:, hh, :], ALU.mult, [bpot] + L, [b_obT[a]])
                gens = [head_body(hh) for hh in range(4)]
                while gens:
                    for g_ in list(gens):
                        try:
                            next(g_)
                        except StopIteration:
                            gens.remove(g_)
                k.dma("sp", ob_b[(c * 64) // CW].ap().rearrange("(h p) t -> p h t", p=128)[:, :, (c * 64) % CW:(c * 64) % CW + 64],
                      obT[a][:], R=[b_obT[a]], W=[])
                if ob_dbg is not None:
                    k.dma("sp", ob_dbg.ap().rearrange("(h p) t -> p h t", p=128)[:, :, cs], obTf[a][:],
                          R=[b_obT[a]], W=[])
            k.barrier()
            es_d.close()
            if STOP_AFTER == "dn":
                finish()
                return

            b_g1 = k.buf("gath1")
            for i in range(NCW):
                k.allgather(oa_b[i], oa_f[i])
                k.allgather(ob_b[i], ob_f[i])
            k.barrier()
            es_2 = ExitStack()
            cur[0] = es_2
            wpa = sb("wpa", [128, 8, 16, 128], BF16)
            wpb = sb("wpb", [128, 8, 16, 128], BF16)
            b_wp = k.buf("wp")
            stg_alloc(2)
            for ch in range(8):
                cast_load(wpa[:, ch, :, :].rearrange("p a b -> p (a b)"), wpa_t.ap()[ch, :, :], [b_wp])
                cast_load(wpb[:, ch, :, :].rearrange("p a b -> p (a b)"), wpb_t.ap()[ch, :, :], [b_wp])
            oaT = [sb("oaT%d" % i, [128, 16, 512], BF16) for i in range(2)]
            obT2 = [sb("obT2%d" % i, [128, 16, 512], BF16) for i in range(2)]
            b_oT2 = k.bufs(2, "oT2")
            ga = [sb("ga%d" % i, [128, 512]) for i in range(2)]
            gb_ = [sb("gb%d" % i, [128, 512]) for i in range(2)]
            b_gg = k.bufs(2, "gg")
            t1 = [sb("t1%d" % i, [128, 512]) for i in range(2)]
            t2 = [sb("t2%d" % i, [128, 512]) for i in range(2)]
            mgo = [sb("mgo%d" % i, [128, 512], BF16) for i in range(2)]
            mgof = [sb("mgof%d" % i, [128, 512]) for i in range(2)] if mg_dbg is not None else None
            b_mgo = k.bufs(2, "mgo")
            pa = [ps("pa%d" % i, [128, 512]) for i in range(2)]
            pb2 = [ps("pb2%d" % i, [128, 512]) for i in range(2)]
            b_pab = k.bufs(2, "pab")
            b_mgb = k.buf("mgb")
            it = 0
            for tt_i in range(S // 512):
                t0 = tt_i * 512
                a = tt_i % 2
                oa_v = oa_f[t0 // CW].ap().rearrange("(kc p) t -> p kc t", p=128)
                ob_v = ob_f[t0 // CW].ap().rearrange("(kc p) t -> p kc t", p=128)
                k.dma("sp", oaT[a][:], oa_v[:, :, t0 % CW:t0 % CW + 512], W=[b_oT2[a]])
                k.dma("sp", obT2[a][:], ob_v[:, :, t0 % CW:t0 % CW + 512], W=[b_oT2[a]])
                for ch in range(8):
                    p = it % 2
                    it += 1
                    k.dma("sp", ga[p][:], projT_t.ap()[(34 + ch) * 128:(35 + ch) * 128, t0:t0 + 512], W=[b_gg[p]])
                    k.dma("sp", gb_[p][:], projT_t.ap()[(42 + ch) * 128:(43 + ch) * 128, t0:t0 + 512], W=[b_gg[p]])
                    for kc in range(16):
                        mm(pa[p][:], wpa[:, ch, kc, :], oaT[a][:, kc, :], [b_wp, b_oT2[a]], [b_pab[p]],
                           start=(kc == 0), stop=(kc == 15))
                    for kc in range(16):
                        mm(pb2[p][:], wpb[:, ch, kc, :], obT2[a][:, kc, :], [b_wp, b_oT2[a]], [b_pab[p]],
                           start=(kc == 0), stop=(kc == 15))
                    tt(t1[p][:], pa[p][:], ga[p][:], ALU.mult, [b_pab[p], b_gg[p]], [b_mgo[p]])
                    tt(t2[p][:], pb2[p][:], gb_[p][:], ALU.mult, [b_pab[p], b_gg[p]], [b_mgo[p]])
                    tt(mgo[p][:], t1[p][:], t2[p][:], ALU.add, [b_mgo[p]], [b_mgo[p]], eng="pool")
                    k.dma("act", mg_b[tt_i].ap()[ch * 128:(ch + 1) * 128, :], mgo[p][:], R=[b_mgo[p]], W=[])
                    if mg_dbg is not None:
                        tt(mgof[p][:], t1[p][:], t2[p][:], ALU.add, [b_mgo[p]], [b_mgo[p]])
                        k.dma("sp", mg_dbg.ap()[ch * 128:(ch + 1) * 128, t0:t0 + 512], mgof[p][:], R=[b_mgo[p]], W=[])
            k.barrier()
            es_2.close()
            b_g2 = k.buf("gath2")
            for i in range(S // 512):
                k.allgather(mg_b[i], mg_f[i])
            k.barrier()

            es_3 = ExitStack()
            cur[0] = es_3
            wo = sb("wo", [128, 8, 32, 128], BF16)
            b_wo = k.buf("wo")
            es_3s = ExitStack()
            cur[0] = es_3s
            stg_alloc(2)
            for ch in range(8):
                cast_load(wo[:, ch, :, :].rearrange("p a b -> p (a b)"), wo_t.ap()[ch, :, :], [b_wo])
            k.barrier()
            es_3s.close()
            cur[0] = es_3
            mgT = [sb("mgT%d" % i, [128, 32, 512], BF16) for i in range(2)]
            xcs = [sb("xcs%d" % i, [128, 4, 1024]) for i in range(2)]
            b_in7 = k.bufs(2, "in7")
            y1 = [sb("y1%d" % i, [128, 512]) for i in range(2)]
            b_y1 = k.bufs(2, "y1")
            ssq = sb("ssq", [128, S // 128])
            b_ssq = k.buf("ssq")
            junk2 = sb("junk2", [128, 1024], BF16)
            b_j2 = k.buf("j2")
            pm2 = [ps("pm2%d" % i, [128, 512]) for i in range(2)]
            b_pm2 = k.bufs(2, "pm2")
            pt2 = [ps("pt2%d" % i, [128, 512]) for i in range(2)]
            b_pt2 = k.bufs(2, "pt2")
            b_yb_ = k.buf("ybuf")
            it = 0
            for tt_i in range(S // 512):
                t0 = tt_i * 512
                a = tt_i % 2
                k.dma("sp", mgT[a][:], mg_f[tt_i].ap().rearrange("(kc p) t -> p kc t", p=128), W=[b_in7[a]])
                k.dma("sp", xcs[a][:], xc_t.ap()[t0:t0 + 512, :].rearrange("(s p) n -> p s n", p=128), W=[b_in7[a]])
                for ch in range(8):
                    p = it % 2
                    it += 1
                    for kc in range(32):
                        mm(pm2[p][:], wo[:, ch, kc, :], mgT[a][:, kc, :], [b_wo, b_in7[a]], [b_pm2[p]],
                           start=(kc == 0), stop=(kc == 31))
                    act(y1[p][:], pm2[p][:], AF.Identity, [b_pm2[p]], [b_y1[p]], scale=gate_sb[:, ch:ch + 1])
                    for sub in range(4):
                        tr(pt2[p][:, sub * 128:(sub + 1) * 128], y1[p][:, sub * 128:(sub + 1) * 128], ident[:],
                           [b_y1[p]], [b_pt2[p]])
                    xv = xcs[a][:, :, ch * 128:(ch + 1) * 128]
                    tt(xv, pt2[p][:].rearrange("p (s c) -> p s c", c=128), xv, ALU.add, [b_pt2[p], b_in7[a]], [b_in7[a]])
                for sub in range(4):
                    act(junk2[:], xcs[a][:, sub, :], AF.Square, [b_in7[a]], [b_j2, b_ssq],
                        accum_out=ssq[:, tt_i * 4 + sub:tt_i * 4 + sub + 1])
                k.dma("act", ybuf_t.ap()[t0:t0 + 512, :].rearrange("(s p) n -> p s n", p=128), xcs[a][:],
                      R=[b_in7[a]], W=[])
            b_ssb = k.buf("ssb")
            k.dma("sp", ss_b.ap()[:, :], ssq[:], R=[b_ssq], W=[b_ssb])
            k.barrier()
            k.allgather(ss_b, ss_f, W=[b_ssb])
            k.barrier()
            ssf = sb("ssf", [128, 4, S // 128])
            rstd = sb("rstd", [128, S // 128])
            fg = sb("fg", [128, 1024])
            b_fin = k.buf("fin")
            k.dma("sp", ssf[:], ss_f.ap().rearrange("(r p) n -> p r n", p=128), W=[b_fin])
            k.dma("sp", fg[:], fg_t.ap()[:, :], W=[b_fin])
            tt(rstd[:], ssf[:, 0, :], ssf[:, 1, :], ALU.add, [b_fin], [b_fin])
            tt(rstd[:], rstd[:], ssf[:, 2, :], ALU.add, [b_fin], [b_fin])
            tt(rstd[:], rstd[:], ssf[:, 3, :], ALU.add, [b_fin], [b_fin])
            ts(rstd[:], rstd[:], 1.0 / D, EPS, ALU.mult, ALU.add, [b_fin], [b_fin])
            act(rstd[:], rstd[:], AF.Sqrt, [b_fin], [b_fin])
            k.op("dve", "reciprocal", R=[b_fin], W=[b_fin], out=rstd[:], in_=rstd[:])
            yt = [sb("yt%d" % i, [128, 1024]) for i in range(2)]
            b_yt = k.bufs(2, "yt")
            for tile in range(S // 128):
                a = tile % 2
                k.dma("sp", yt[a][:], ybuf_t.ap()[tile * 128:(tile + 1) * 128, :], W=[b_yt[a]])
                stt(yt[a][:], yt[a][:], rstd[:, tile:tile + 1], fg[:], ALU.mult, ALU.mult, [b_yt[a], b_fin], [b_yt[a]])
                k.dma("act", out_t.ap()[tile * 128:(tile + 1) * 128, :], yt[a][:], R=[b_yt[a]], W=[])
            finish()
            es_3.close()
            es_c.close()
    return nc


def _col_index(j):
    g, half = j // 2, j % 2
    o_q, o_kv, o_g, o_z = 0, 2048, 2048 + 1536, 2048 + 1536 + 48
    o_dn = o_z + 2048
    o_a = o_dn + 6144
    o_b = o_a + 16
    o_dz = o_b + 16
    o_mg = o_dz + 2048
    cols = []
    my_heads = [8 * g + 4 * half + i for i in range(4)]
    ot_heads = [8 * g + 4 * (1 - half) + i for i in range(4)]
    for h in my_heads + ot_heads:
        cols += list(range(o_q + h * 128, o_q + (h + 1) * 128))
    for t in range(6):
        cols += list(range(o_kv + t * 256 + g * 128, o_kv + t * 256 + (g + 1) * 128))
    for h in my_heads:
        cols += list(range(o_z + h * 128, o_z + (h + 1) * 128))
    dn_heads = [4 * j + i for i in range(4)]
    for t in range(3):
        for h in dn_heads:
            cols += list(range(o_dn + t * 2048 + h * 128, o_dn + t * 2048 + (h + 1) * 128))
    for h in dn_heads:
        cols += list(range(o_dz + h * 128, o_dz + (h + 1) * 128))
    cols += list(range(o_mg + j * 1024, o_mg + (j + 1) * 1024))
    cols += list(range(o_mg + 4096 + j * 1024, o_mg + 4096 + (j + 1) * 1024))
    small = []
    for br in range(3):
        small += [o_g + br * 16 + h for h in my_heads]
    small += [o_a + h for h in dn_heads]
    small += [o_b + h for h in dn_heads]
    return np.array(cols), np.array(small)


def _pk(v):
    return np.ascontiguousarray(v.reshape(NKC, 128).T)


def _wlayout(w, nkc):
    w = w.reshape(nkc, 128, 8, 128).transpose(2, 1, 0, 3)
    return np.ascontiguousarray(w).reshape(8, 128, nkc * 128)


def _consts():
    f32 = np.float32
    idx = np.arange(64)
    tri_le = (idx[:, None] <= idx[None, :]).astype(f32)
    lowst = (idx[:, None] > idx[None, :]).astype(f32)
    bd = (idx[:, None] // 32 == idx[None, :] // 32).astype(f32)
    c64 = np.concatenate([tri_le, lowst, lowst * bd, lowst * (1 - bd)], 1)
    p = np.arange(128)[:, None]
    c = np.arange(256)[None, :]
    cval = (16 * c + 31 - p).astype(f32)
    cval[:, 255] = 1e9
    cmp_start = np.arange(255) * 16
    slc_start = np.arange(64) * 64
    ov = ((cmp_start[:, None] <= slc_start[None, :] + 63) & (cmp_start[:, None] + 31 >= slc_start[None, :])).astype(f32)
    ov = np.concatenate([ov, np.zeros((1, 64), f32)], 0)
    ovl = ov.reshape(2, 128, 64).transpose(1, 0, 2).reshape(128, 128)
    d = np.arange(128)
    invf = np.where(d < 32, 500000.0 ** (-(d % 16) / 16.0), 0.0).astype(f32)[:, None]
    cc = np.arange(128)[None, :]
    tri128 = (cc <= p).astype(f32)
    triw = (cc > p).astype(f32)
    c128 = np.concatenate([cval, ovl, invf, tri128, triw], 1).astype(f32)
    rotT = np.zeros((32, 32), f32)
    for m in range(16):
        rotT[m + 16, m] = -1.0
        rotT[m, m + 16] = 1.0
    vmask = np.zeros((32, 128, 64), f32)
    fbias = np.zeros((32, 128, 64), f32)
    j = np.arange(64)[None, :]
    for qi in range(32):
        t = (128 * qi + np.arange(128))[:, None]
        tb = t // 64
        valid = (j * 64 <= t)
        forced = (j == 0) | (j == tb) | (j == tb - 1)
        vmask[qi] = (valid & ~forced)
        fbias[qi] = np.where(forced, 1e9, np.where(valid, 0.0, -1.0))
    return dict(ident=np.eye(128, dtype=f32), c64=c64, c128=c128, rotT=rotT, vmask=vmask, fbias=fbias)


def make_in_maps(inp):
    f32 = np.float32
    maps = []
    cst = _consts()
    w_in = np.asarray(inp["w_in"][0])
    w_ada = np.asarray(inp["w_ada"][0])
    conv_w = np.asarray(inp["conv_w"][0])
    for core in range(8):
        b, j = core // 4, core % 4
        cols, small = _col_index(j)
        wj = w_in[:, cols]
        wj = wj.reshape(NKC, 128, NCHUNK, 128).transpose(2, 1, 0, 3)
        wj = np.ascontiguousarray(wj).reshape(NCHUNK, 128, NKC * 128)
        ws = w_in[:, small].reshape(NKC, 128, NSMALL).transpose(1, 0, 2)
        ws = np.ascontiguousarray(ws).reshape(128, NKC * NSMALL)
        gsel = np.zeros((96, 8), f32)
        for ch in range(8):
            gsel[64 + 8 * j + ch, ch] = 1.0
        dn_heads = [4 * j + i for i in range(4)]
        cw = np.zeros((128, 12, 4), f32)
        for ci in range(12):
            t, h = ci // 4, dn_heads[ci % 4]
            cw[:, ci, :] = conv_w[:, t * 2048 + h * 128:t * 2048 + (h + 1) * 128].T
        dnc = np.zeros((64, 136), f32)
        dnc[:, 0:4] = np.asarray(inp["dt_bias"][0])[dn_heads][None, :]
        dnc[:, 4:8] = np.asarray(inp["a_log"][0])[dn_heads][None, :]
        dnc[:, 8:136] = np.asarray(inp["dn_norm_gain"][0])[None, :]
        cs = slice(j * 1024, (j + 1) * 1024)
        m = {
            "x": np.ascontiguousarray(inp["x"][b]),
            "xcols": np.ascontiguousarray(inp["x"][b][:, cs]),
            "c_pk": _pk(np.asarray(inp["c"][b])),
            "w_ada": np.ascontiguousarray(w_ada[:, j * 3072:(j + 1) * 3072]),
            "b_ada": np.ascontiguousarray(inp["b_ada"][0][j * 3072:(j + 1) * 3072]).reshape(1, 3072),
            "ngain_pk": _pk(np.asarray(inp["norm_gain"][0])),
            "gsel": gsel,
            "w_in": wj,
            "w_small": ws,
            "pos": np.ascontiguousarray(inp["positions"][b]).reshape(1, S).astype(np.int32),
            "w1k": np.ascontiguousarray(inp["w_cmp_k1"][0]),
            "w1v": np.ascontiguousarray(inp["w_cmp_v1"][0]),
            "w2k": np.ascontiguousarray(inp["w_cmp_k2"][0]),
            "w2v": np.ascontiguousarray(inp["w_cmp_v2"][0]),
            "cposkT": np.ascontiguousarray(inp["cmp_pos_k"][0].T),
            "cposvT": np.ascontiguousarray(inp["cmp_pos_v"][0].T),
            "convw": cw.reshape(128, 48),
            "dnc": dnc,
            "wpa": _wlayout(np.asarray(inp["w_proj_a"][0])[:, cs], 16),
            "wpb": _wlayout(np.asarray(inp["w_proj_b"][0])[:, cs], 16),
            "wo": _wlayout(np.asarray(inp["w_out"][0])[:, cs], 32),
            "fgain": np.ascontiguousarray(np.broadcast_to(np.asarray(inp["final_gain"])[cs][None, :], (128, 1024))),
        }
        m.update(cst)
        maps.append(m)
    return maps


def kernel(**inputs):
    inp = {k_: np.asarray(v) for k_, v in inputs.items()}
    nc = build_program()
    maps = make_in_maps(inp)
    res = run_bass_kernel_spmd(nc, maps, core_ids=list(range(8)))
    out = np.zeros((2, S, D), np.float32)
    for core in range(8):
        b, j = core // 4, core % 4
        out[b][:, j * 1024:(j + 1) * 1024] = res.results[core]["out"]
    return out
```
